# Optimizing a Trainium2 kernel written in Bass

```python
import math
import jax
import jax.numpy as jnp
from jax import lax
import numpy as np

D_MODEL = 2048
BATCH = 16
SEQ = 2048
DEPTH = 2

BRANCH_WIDTH = 1024
N_BRANCH = 3
M_HEADS = 4
M_HEAD_DIM = BRANCH_WIDTH // M_HEADS
M_WIDTH = M_HEADS * M_HEAD_DIM
M_CONV = 4
M_CHUNK = 64
DA_HEADS = 4
DA_HEAD_DIM = 128
DA_V_DIM = 2 * DA_HEAD_DIM
DA_QK_WIDTH = DA_HEADS * 2 * DA_HEAD_DIM
DA_WIDTH = DA_HEADS * DA_V_DIM
Q_BLOCK = 128
REL_BUCKETS = 32
REL_MAX_DIST = 128
S5_WIDTH = BRANCH_WIDTH
S5_GROUP = 16
S5_GROUPS = S5_WIDTH // S5_GROUP
S5_STATE = 64
D_FF = 5632
FFN_CONV = 3
EPS = 1e-6
IN_SIZES = (M_WIDTH, M_WIDTH, M_WIDTH, M_WIDTH, M_HEADS, M_HEADS,
            DA_QK_WIDTH, DA_QK_WIDTH, DA_WIDTH, S5_WIDTH, N_BRANCH * D_MODEL)
N_IN = sum(IN_SIZES)

kernel_name = 'hybrid_gated_mlstm_diffattn_s5'


def rmsnorm(x, gain):
    xf = x.astype(jnp.float32)
    var = jnp.mean(xf * xf, axis=-1, keepdims=True)
    return (xf * lax.rsqrt(var + EPS) * gain.astype(jnp.float32)).astype(x.dtype)


def causal_dwconv(x, w):
    K = w.shape[0]
    L = x.shape[1]
    xp = jnp.pad(x, ((0, 0), (K - 1, 0), (0, 0)))
    return sum(w[j] * xp[:, j:j + L] for j in range(K))


def t5_causal_bucket(dist):
    n = jnp.maximum(dist, 0)
    max_exact = REL_BUCKETS // 2
    nf = jnp.maximum(n, 1).astype(jnp.float32)
    large = max_exact + (jnp.log(nf / max_exact) / math.log(REL_MAX_DIST / max_exact)
                         * (REL_BUCKETS - max_exact)).astype(jnp.int32)
    large = jnp.minimum(large, REL_BUCKETS - 1)
    return jnp.where(n < max_exact, n, large)


def mlstm_branch(q, k, v, o, i_pre, f_pre, conv_w, norm_g):
    dtype = v.dtype
    B, L, _ = q.shape
    nc = L // M_CHUNK
    qk = jax.nn.silu(causal_dwconv(jnp.concatenate([q, k], axis=-1), conv_w))
    q, k = jnp.split(qk, 2, axis=-1)

    def to_chunks(t):
        return t.astype(jnp.float32).reshape(B, nc, M_CHUNK, M_HEADS, -1).transpose(1, 0, 3, 2, 4)

    def gate_chunks(t):
        return t.astype(jnp.float32).reshape(B, nc, M_CHUNK, M_HEADS).transpose(1, 0, 3, 2)

    qc = to_chunks(q)
    kc = to_chunks(k) * (M_HEAD_DIM ** -0.5)
    vc = to_chunks(v)
    log_i = gate_chunks(i_pre)
    log_f = jax.nn.log_sigmoid(gate_chunks(f_pre))
    causal = jnp.tril(jnp.ones((M_CHUNK, M_CHUNK), dtype=bool))

    def step(carry, inp):
        C, n, m = carry
        qt, kt, vt, li, lf = inp
        b = jnp.cumsum(lf, axis=-1)
        g = b[..., -1]
        dmat = b[..., :, None] - b[..., None, :] + li[..., None, :]
        dmat = jnp.where(causal, dmat, -jnp.inf)
        inter = b + m[..., None]
        m_t = jnp.maximum(inter, jnp.max(dmat, axis=-1))
        s = jnp.einsum('bhtd,bhsd->bhts', qt, kt) * jnp.exp(dmat - m_t[..., None])
        w_inter = jnp.exp(inter - m_t)
        num = (jnp.einsum('bhts,bhsv->bhtv', s, vt)
               + w_inter[..., None] * jnp.einsum('bhtd,bhdv->bhtv', qt, C))
        den = jnp.sum(s, axis=-1) + w_inter * jnp.einsum('bhtd,bhd->bht', qt, n)
        h = num / jnp.maximum(jnp.abs(den), jnp.exp(-m_t))[..., None]
        a_s = g[..., None] - b + li
        m_new = jnp.maximum(g + m, jnp.max(a_s, axis=-1))
        ws = jnp.exp(a_s - m_new[..., None])
        decay = jnp.exp(g + m - m_new)
        C_new = decay[..., None, None] * C + jnp.einsum('bhs,bhsd,bhsv->bhdv', ws, kt, vt)
        n_new = decay[..., None] * n + jnp.einsum('bhs,bhsd->bhd', ws, kt)
        return (C_new, n_new, m_new), h

    init = (jnp.zeros((B, M_HEADS, M_HEAD_DIM, M_HEAD_DIM), jnp.float32),
            jnp.zeros((B, M_HEADS, M_HEAD_DIM), jnp.float32),
            jnp.zeros((B, M_HEADS), jnp.float32))
    _, h = lax.scan(step, init, (qc, kc, vc, log_i, log_f))
    h = h.transpose(1, 0, 3, 2, 4).reshape(B, L, M_HEADS, M_HEAD_DIM)
    h = rmsnorm(h, norm_g.reshape(M_HEADS, M_HEAD_DIM)).reshape(B, L, M_WIDTH)
    return (jax.nn.sigmoid(o.astype(jnp.float32)) * h).astype(dtype)


def diff_attention_branch(q, k, v, lam, norm_g, rel_bias, lambda_init):
    dtype = v.dtype
    B, L, _ = q.shape
    nb = L // Q_BLOCK
    q = q.astype(jnp.float32).reshape(B, L, DA_HEADS, 2, DA_HEAD_DIM) * (DA_HEAD_DIM ** -0.5)
    k = k.astype(jnp.float32).reshape(B, L, DA_HEADS, 2, DA_HEAD_DIM)
    v = v.astype(jnp.float32).reshape(B, L, DA_HEADS, DA_V_DIM)
    lam = lam.astype(jnp.float32)
    lam_full = jnp.exp(jnp.sum(lam[0] * lam[1])) - jnp.exp(jnp.sum(lam[2] * lam[3])) + lambda_init
    bias_table = rel_bias.astype(jnp.float32)
    qb = q.reshape(B, nb, Q_BLOCK, DA_HEADS, 2, DA_HEAD_DIM).transpose(1, 0, 2, 3, 4, 5)
    k_pos = jnp.arange(L)

    def block(args):
        qblk, start = args
        q_pos = start + jnp.arange(Q_BLOCK)
        dist = q_pos[:, None] - k_pos[None, :]
        bias = jnp.transpose(bias_table[t5_causal_bucket(dist)], (2, 0, 1))
        logits = jnp.einsum('bqhmd,bkhmd->bhmqk', qblk, k) + bias[None, :, None]
        logits = jnp.where(dist >= 0, logits, -jnp.inf)
        p = jax.nn.softmax(logits, axis=-1)
        attn = p[:, :, 0] - lam_full * p[:, :, 1]
        return jnp.einsum('bhqk,bkhv->bqhv', attn, v)

    starts = jnp.arange(nb, dtype=jnp.int32) * Q_BLOCK
    out = lax.map(block, (qb, starts))
    out = out.transpose(1, 0, 2, 3, 4).reshape(B, L, DA_HEADS, DA_V_DIM)
    out = rmsnorm(out, norm_g.reshape(DA_HEADS, DA_V_DIM)) * (1.0 - lambda_init)
    return out.reshape(B, L, DA_WIDTH).astype(dtype)


def s5_branch(u, lam_re, lam_im, log_dt, b_re, b_im, c_re, c_im, d_skip, w_glu):
    dtype = u.dtype
    B, L, _ = u.shape
    uf = u.astype(jnp.float32)
    ug = uf.reshape(B, L, S5_GROUPS, S5_GROUP)
    lam_re = lam_re.astype(jnp.float32)
    lam_im = lam_im.astype(jnp.float32)
    dt = jnp.exp(log_dt.astype(jnp.float32))[:, None]
    mag = jnp.exp(lam_re * dt)
    a_re = mag * jnp.cos(lam_im * dt)
    a_im = mag * jnp.sin(lam_im * dt)
    den = lam_re * lam_re + lam_im * lam_im
    z_re = ((a_re - 1.0) * lam_re + a_im * lam_im) / den
    z_im = (a_im * lam_re - (a_re - 1.0) * lam_im) / den
    b_re = b_re.astype(jnp.float32)
    b_im = b_im.astype(jnp.float32)
    bb_re = z_re[..., None] * b_re - z_im[..., None] * b_im
    bb_im = z_re[..., None] * b_im + z_im[..., None] * b_re
    bu_re = jnp.einsum('gpc,blgc->blgp', bb_re, ug)
    bu_im = jnp.einsum('gpc,blgc->blgp', bb_im, ug)
    a_re_t = jnp.broadcast_to(a_re, (1, L, S5_GROUPS, S5_STATE))
    a_im_t = jnp.broadcast_to(a_im, (1, L, S5_GROUPS, S5_STATE))

    def combine(e1, e2):
        a1r, a1i, b1r, b1i = e1
        a2r, a2i, b2r, b2i = e2
        return (a2r * a1r - a2i * a1i,
                a2r * a1i + a2i * a1r,
                a2r * b1r - a2i * b1i + b2r,
                a2r * b1i + a2i * b1r + b2i)

    _, _, x_re, x_im = lax.associative_scan(combine, (a_re_t, a_im_t, bu_re, bu_im), axis=1)
    y = (jnp.einsum('gcp,blgp->blgc', c_re.astype(jnp.float32), x_re)
         - jnp.einsum('gcp,blgp->blgc', c_im.astype(jnp.float32), x_im))
    y = y.reshape(B, L, S5_WIDTH) + d_skip.astype(jnp.float32) * uf
    y = jax.nn.gelu(y)
    ab = jnp.einsum('blw,wv->blv', y, w_glu.astype(jnp.float32))
    a, g = jnp.split(ab, 2, axis=-1)
    return (a * jax.nn.sigmoid(g)).astype(dtype)


def setup_inputs(seed: int = 0) -> dict:
    key = jax.random.key(seed)
    ks = jax.random.split(key, 32)
    f32 = jnp.float32

    def nrm(k, shape, scale):
        return jax.random.normal(k, shape, f32) * scale

    def gain(k, shape):
        return 1.0 + 0.05 * jax.random.normal(k, shape, f32)

    x = nrm(ks[0], (BATCH, SEQ, D_MODEL), 1.0)
    norm_mix_pre = gain(ks[1], (DEPTH, D_MODEL))
    norm_mix_post = gain(ks[2], (DEPTH, D_MODEL))
    norm_ffn_pre = gain(ks[3], (DEPTH, D_MODEL))
    norm_ffn_post = gain(ks[4], (DEPTH, D_MODEL))
    w_in = nrm(ks[5], (DEPTH, D_MODEL, N_IN), D_MODEL ** -0.5)
    b_i = nrm(ks[6], (DEPTH, M_HEADS), 0.1)
    b_f = jnp.linspace(3.0, 6.0, M_HEADS, dtype=f32) + nrm(ks[7], (DEPTH, M_HEADS), 0.1)
    mlstm_b_if = jnp.stack([b_i, b_f], axis=1)
    mlstm_conv = nrm(ks[8], (DEPTH, M_CONV, 2 * M_WIDTH), M_CONV ** -0.5)
    mlstm_norm = gain(ks[9], (DEPTH, M_WIDTH))
    diff_lambda = nrm(ks[10], (DEPTH, 4, DA_HEAD_DIM), 0.1)
    diff_norm = gain(ks[11], (DEPTH, DA_WIDTH))
    rel_bias = nrm(ks[12], (REL_BUCKETS, DA_HEADS), 0.5)
    n_idx = jnp.arange(S5_STATE, dtype=f32)
    s5_lambda_re = -0.5 + nrm(ks[13], (DEPTH, S5_GROUPS, S5_STATE), 1e-3)
    s5_lambda_im = math.pi * n_idx + nrm(ks[14], (DEPTH, S5_GROUPS, S5_STATE), 1e-3)
    s5_log_dt = jax.random.uniform(ks[15], (DEPTH, S5_GROUPS), f32, math.log(1e-3), math.log(1e-1))
    s5_b_re = nrm(ks[16], (DEPTH, S5_GROUPS, S5_STATE, S5_GROUP), (2 * S5_GROUP) ** -0.5)
    s5_b_im = nrm(ks[17], (DEPTH, S5_GROUPS, S5_STATE, S5_GROUP), (2 * S5_GROUP) ** -0.5)
    s5_c_re = nrm(ks[18], (DEPTH, S5_GROUPS, S5_GROUP, S5_STATE), (2 * S5_STATE) ** -0.5)
    s5_c_im = nrm(ks[19], (DEPTH, S5_GROUPS, S5_GROUP, S5_STATE), (2 * S5_STATE) ** -0.5)
    s5_d = nrm(ks[20], (DEPTH, S5_WIDTH), 0.5)
    s5_w_glu = nrm(ks[21], (DEPTH, S5_WIDTH, 2 * S5_WIDTH), S5_WIDTH ** -0.5)
    w_branch = nrm(ks[22], (DEPTH, N_BRANCH, BRANCH_WIDTH, D_MODEL), BRANCH_WIDTH ** -0.5)
    w_out = nrm(ks[23], (DEPTH, D_MODEL, D_MODEL), D_MODEL ** -0.5)
    w_up = nrm(ks[24], (DEPTH, D_MODEL, 2 * D_FF), D_MODEL ** -0.5)
    ffn_conv = nrm(ks[25], (DEPTH, FFN_CONV, 2 * D_FF), FFN_CONV ** -0.5)
    ffn_conv_b = nrm(ks[26], (DEPTH, 2 * D_FF), 0.02)
    w_down = nrm(ks[27], (DEPTH, D_FF, D_MODEL), D_FF ** -0.5)
    return {'x': x, 'norm_mix_pre': norm_mix_pre, 'norm_mix_post': norm_mix_post,
            'norm_ffn_pre': norm_ffn_pre, 'norm_ffn_post': norm_ffn_post, 'w_in': w_in,
            'mlstm_b_if': mlstm_b_if, 'mlstm_conv': mlstm_conv, 'mlstm_norm': mlstm_norm,
            'diff_lambda': diff_lambda, 'diff_norm': diff_norm, 'rel_bias': rel_bias,
            's5_lambda_re': s5_lambda_re, 's5_lambda_im': s5_lambda_im, 's5_log_dt': s5_log_dt,
            's5_b_re': s5_b_re, 's5_b_im': s5_b_im, 's5_c_re': s5_c_re, 's5_c_im': s5_c_im,
            's5_d': s5_d, 's5_w_glu': s5_w_glu, 'w_branch': w_branch, 'w_out': w_out,
            'w_up': w_up, 'ffn_conv': ffn_conv, 'ffn_conv_b': ffn_conv_b, 'w_down': w_down}


def reference(x, norm_mix_pre, norm_mix_post, norm_ffn_pre, norm_ffn_post, w_in,
              mlstm_b_if, mlstm_conv, mlstm_norm, diff_lambda, diff_norm, rel_bias,
              s5_lambda_re, s5_lambda_im, s5_log_dt, s5_b_re, s5_b_im, s5_c_re, s5_c_im,
              s5_d, s5_w_glu, w_branch, w_out, w_up, ffn_conv, ffn_conv_b, w_down):
    B, L, _ = x.shape
    split_points = [int(s) for s in np.cumsum(IN_SIZES)[:-1]]
    for layer in range(DEPTH):
        h = rmsnorm(x, norm_mix_pre[layer])
        proj = jnp.einsum('bld,dn->bln', h, w_in[layer])
        (q_m, k_m, v_m, o_m, i_m, f_m, q_d, k_d, v_d, u_s,
         gate_pre) = jnp.split(proj, split_points, axis=-1)
        y_a = mlstm_branch(q_m, k_m, v_m, o_m, i_m + mlstm_b_if[layer, 0],
                           f_m + mlstm_b_if[layer, 1], mlstm_conv[layer], mlstm_norm[layer])
        lambda_init = 0.8 - 0.6 * math.exp(-0.3 * layer)
        y_b = diff_attention_branch(q_d, k_d, v_d, diff_lambda[layer], diff_norm[layer],
                                    rel_bias, lambda_init)
        y_c = s5_branch(u_s, s5_lambda_re[layer], s5_lambda_im[layer], s5_log_dt[layer],
                        s5_b_re[layer], s5_b_im[layer], s5_c_re[layer], s5_c_im[layer],
                        s5_d[layer], s5_w_glu[layer])
        ys = jnp.stack([y_a, y_b, y_c], axis=2)
        z = jnp.einsum('blnw,nwd->blnd', ys, w_branch[layer])
        gates = jax.nn.sigmoid(gate_pre.reshape(B, L, N_BRANCH, D_MODEL))
        merged = jnp.sum(gates * z, axis=2)
        mix = jnp.einsum('bld,de->ble', merged, w_out[layer])
        x = x + rmsnorm(mix, norm_mix_post[layer])
        h = rmsnorm(x, norm_ffn_pre[layer])
        up = causal_dwconv(jnp.einsum('bld,df->blf', h, w_up[layer]), ffn_conv[layer]) + ffn_conv_b[layer]
        a, v = jnp.split(up, 2, axis=-1)
        ffn = jnp.einsum('blf,fd->bld', jax.nn.gelu(a) * v, w_down[layer])
        x = x + rmsnorm(ffn, norm_ffn_post[layer])
    return x
```

```python
import math
from contextlib import ExitStack
import numpy as np
import concourse.bass as bass
import concourse.mybir as mybir
from concourse.bass_utils import run_bass_kernel_spmd

F32 = mybir.dt.float32
BF16 = mybir.dt.bfloat16
AF = mybir.ActivationFunctionType
ALU = mybir.AluOpType

NCORE = 8
DBG_X1 = False
L = 2048
NB = 2
T = NB * L
D = 2048
NIN = 14344
DFF = 5632
EPS = 1e-6
O_QM, O_KM, O_VM, O_OM, O_I, O_F, O_QD, O_KD, O_VD, O_US, O_G = (
    0, 1024, 2048, 3072, 4096, 4100, 4104, 5128, 6152, 7176, 8200)


class Sync:
    def __init__(self, nc):
        self.nc = nc
        self.eng = dict(pe=nc.tensor, act=nc.scalar, dve=nc.vector, pool=nc.gpsimd, sp=nc.sync)
        self.sems, self.tot, self.isdma = {}, {}, {}
        for e in self.eng:
            self._mk('e_' + e, False)
        self.seen = {e: {} for e in self.eng}
        self.st = {}
        self.pe_pending = []
        self.n_ins = 0

    def _mk(self, sid, isdma):
        if sid not in self.sems:
            self.sems[sid] = self.nc.semaphore(sid).__enter__()
            self.tot[sid] = 0
            self.isdma[sid] = isdma
        return self.sems[sid]

    def _deps(self, reads, writes):
        need = {}
        for k in reads:
            s = self.st.get(k)
            if s:
                for sid, v in s[0].items():
                    if need.get(sid, 0) < v:
                        need[sid] = v
        for k in writes:
            s = self.st.get(k)
            if s:
                for d in s:
                    for sid, v in d.items():
                        if need.get(sid, 0) < v:
                            need[sid] = v
        return need

    def _wait(self, e, need):
        for sid, v in need.items():
            if e == 'pe' and sid == 'e_pe':
                continue
            if self.isdma[sid]:
                v = self.tot[sid]
            if self.seen[e].get(sid, 0) >= v:
                continue
            self.eng[e].wait_ge(self.sems[sid], v)
            self.seen[e][sid] = v
            self.n_ins += 1

    def _record(self, reads, writes, sid, v):
        for k in writes:
            self.st[k] = [{sid: v}, {}]
        for k in reads:
            s = self.st.setdefault(k, [{}, {}])
            if s[1].get(sid, 0) < v:
                s[1][sid] = v

    def op(self, e, fn, reads=(), writes=(), last=True):
        self._wait(e, self._deps(reads, writes))
        ins = fn(self.eng[e])
        self.n_ins += 1
        sid = 'e_' + e
        if e == 'pe' and not last:
            self.pe_pending.append((tuple(reads), tuple(writes)))
            return ins
        self.tot[sid] += 1
        ins.then_inc(self.sems[sid], 1)
        v = self.tot[sid]
        if e == 'pe':
            for r, w in self.pe_pending:
                self._record(r, w, sid, v)
            self.pe_pending = []
        self._record(reads, writes, sid, v)
        return ins

    def dma(self, e, out, in_, reads=(), writes=(), sem='d0', **kw):
        self._wait(e, self._deps(reads, writes))
        sid = 'd_' + sem
        S = self._mk(sid, True)
        self.tot[sid] += 16
        self.eng[e].dma_start(out=out, in_=in_, **kw).then_inc(S, 16)
        self.n_ins += 1
        self._record(reads, writes, sid, self.tot[sid])

    def barrier(self):
        assert not self.pe_pending
        for e in self.eng:
            for sid in self.sems:
                v = self.tot[sid]
                if v == 0 or self.seen[e].get(sid, 0) >= v:
                    continue
                self.eng[e].wait_ge(self.sems[sid], v)
                self.seen[e][sid] = v
                self.n_ins += 1
        self.st = {}


class Ring:
    def __init__(self, name, n):
        self.name, self.n, self.i = name, n, -1

    def next(self):
        self.i += 1
        return self.i % self.n


class L1:
    def __init__(self, ap):
        self.ap = ap

    def __getitem__(self, k):
        if isinstance(k, tuple):
            return self.ap[k[1]] if len(k) == 2 else self.ap[k[1:]]
        return self.ap


class Prog:
    def __init__(self, kind):
        self.kind = kind
        nc = self.nc = bass.Bass("TRN2", target_bir_lowering=False)
        self.S = Sync(nc)
        self.inp = {}
        self.outs = []
        self.build()

    def din(self, name, shape, dt=F32):
        self.inp[name] = (shape, dt)
        return self.nc.dram_tensor(name, list(shape), dt, kind="ExternalInput").ap()

    def dscr(self, name, shape, dt, io=None):
        if io == 'in':
            return self.din(name, shape, dt)
        if io == 'out':
            self.outs.append(name)
        kind = "ExternalOutput" if io == 'out' else "Internal"
        return self.nc.dram_tensor(name, list(shape), dt, kind=kind).ap()

    def sb(self, es, name, shape, dt):
        self._uid = getattr(self, '_uid', 0) + 1
        return es.enter_context(self.nc.sbuf_tensor('%s_%d' % (name, self._uid), list(shape), dt))

    def act(self, out, in_, func, reads, writes, **kw):
        self.S.op('act', lambda e: e.activation(out=out, in_=in_, func=func, **kw), reads, writes)

    def mm(self, out, lhsT, rhs, start, stop, reads, writes, last=None):
        if last is None:
            last = stop
        self.S.op('pe', lambda e: e.matmul(out, lhsT, rhs, start=start, stop=stop), reads, writes, last=last)

    def rstd(self, ss, n, key):
        S = self.S
        S.op('dve', lambda e: e.tensor_scalar(ss, ss, 1.0 / n, EPS, ALU.mult, ALU.add), [key], [key])
        self.act(ss, ss, AF.Ln, [key], [key])
        self.act(ss, ss, AF.Exp, [key], [key], scale=-0.5)

    def build(self):
        nc, S = self.nc, self.S
        kd = self.kind
        fused = kd == 'F'
        din = (lambda n, sh, dt=F32: self.din(n, [2] + list(sh), dt)) if fused else (lambda n, sh, dt=F32: L1(self.din(n, sh, dt)))
        scr = self.dscr
        self.cst = self.din('cst', [128, 512])
        if kd in 'AF':
            self.x_in = self.din('x', [T, D])
            self.w_in = din('w_in', [D, NIN])
            self.gpre = din('gpre', [128, 16])
            self.mconv = din('mconv', [128, 16, 4])
            self.bif = din('bif', [1, 8])
            self.WF_in = scr('WF_in', [88, 128, 16 * 128], BF16)
            self.WT_in = scr('WT_in', [6, 128, 16 * 512], BF16)
        if kd in 'BF':
            self.w_glu = din('s5_w_glu', [1024, 2048])
            self.mnorm = din('mnorm', [1, 1024])
            self.dnorm = din('dnorm', [1, 1024])
            self.lamT = din('lamT', [128, 4])
            self.lami = din('lami', [128, 2])
            self.wide = self.din('wide', [128, 4, 1024])
            self.cfar = self.din('cfar', [128, 4])
            self.s5d = din('s5d', [128, 8])
            self.s5p = din('s5p', [3, 128, 32])
            self.s5r = din('s5r', [3, 128, 32 * 128])
            self.s5bT = din('s5bT', [2, 128, 32 * 128])
            self.s5cP = din('s5cP', [2, 128, 32 * 128])
            self.WF_glu = scr('WF_glu', [16, 128, 8 * 128], BF16)
            self.ycs = scr('ycs', [8, 128, T], BF16)
            self.s5tab = scr('s5tab', [4, 128, 32 * 128], F32)
        if kd in 'CF':
            if not fused:
                self.x_in = self.din('x', [T, D])
            self.y_out = scr('y', [T, D], F32, 'out')
            self.xmid = scr('xmid', [T, D], F32)
            self.w_branch = din('w_branch', [3, 1024, D])
            self.w_out = din('w_out', [D, D])
            self.w_up = din('w_up', [D, 2 * DFF])
            self.w_down = din('w_down', [DFF, D])
            self.gffn = din('gffn', [128, 16])
            self.gpost = din('gpost', [1, D])
            self.gfpost = din('gfpost', [1, D])
            self.fconv = din('fconv', [128, 88, 3])
            self.fbias = din('fbias', [128, 88])
            self.WF_br = scr('WF_br', [48, 128, 8 * 128], BF16)
            self.WT_out = scr('WT_out', [4, 128, 16 * 512], BF16)
            self.WF_up = scr('WF_up', [88, 128, 16 * 128], BF16)
            self.WT_dn = scr('WT_dn', [16, 128, 11 * 512], BF16)
            self.x1 = scr('x1', [T, D], F32, 'out' if DBG_X1 else None)
        ab = {'A': 'out', 'B': 'in'}.get(kd)
        if kd in 'ABF':
            self.qkm = scr('qkm', [16, 128, T], BF16, ab)
            self.qkd = scr('qkd', [16, 128, T], BF16, ab)
            self.uT = scr('uT', [8, 128, T], F32, ab)
            self.vm = scr('vm', [T, 1024], BF16, ab)
            self.om = scr('om', [T, 1024], BF16, ab)
            self.vd = scr('vd', [T, 1024], BF16, ab)
            self.lif = scr('lif', [T, 8], F32, ab)
        if kd in 'ACF':
            self.gT = scr('gT', [48, 128, T], BF16, {'A': 'out', 'C': 'in'}.get(kd))
        if kd in 'BCF':
            self.yT = scr('yT', [24, 128, T], BF16, {'B': 'out', 'C': 'in'}.get(kd))

        self.C = nc.alloc_sbuf_tensor('cst_sb', [128, 512], F32)
        self.IDB = nc.alloc_sbuf_tensor('idb', [128, 128], BF16)
        S.dma('sp', self.C[:], self.cst[:, :], writes=['cst'], sem='c')
        S.op('dve', lambda e: e.tensor_copy(self.IDB[:], self.C[:, 0:128]), ['cst'], ['idb'])
        self.ID32 = self.C[:, 0:128]
        self.U = self.C[:, 128:256]
        self.NEGM = self.C[:, 256:384]
        self.ONES = self.C[:, 384:512]
        self.PS = [nc.alloc_psum_tensor('ps%d' % i, [128, 512], F32) for i in range(8)]
        S.barrier()
        for l in range(2 if fused else 1):
            xin = getattr(self, 'x_in', None) if l == 0 else self.xmid
            xout = getattr(self, 'y_out', None) if (l == 1 or not fused) else self.xmid
            self.phase_prep(l)
            if kd in 'AF':
                self.phase_inproj(l, xin)
            if kd in 'BF':
                self.phase_mix(l)
            if kd in 'CF':
                self.phase_merge_ffn(l, xin, xout)
        S.barrier()

    def phase_prep(self, l):
        nc, S = self.nc, self.S
        with ExitStack() as es:
            s32 = [self.sb(es, 'p32_%d' % i, [128, 16, 512], F32) for i in range(2)]
            s16 = [self.sb(es, 'p16_%d' % i, [128, 16 * 512], BF16) for i in range(2)]
            gp = self.sb(es, 'pgp', [128, 16], F32)
            gf = self.sb(es, 'pgf', [128, 16], F32)
            if self.kind in 'AF':
                S.dma('sp', gp[:], self.gpre[l], writes=['pgp'], sem='c')
            if self.kind in 'CF':
                S.dma('sp', gf[:], self.gffn[l], writes=['pgf'], sem='c')
            ring = Ring('p', 2)
            cnt = [0]

            def block(W2d, c0, kt0, KT, gain, gkey, form, dst):
                i = ring.next()
                a, b = s32[i], s16[i]
                for q in range(0, KT, 4):
                    n = min(4, KT - q)
                    src = W2d[(kt0 + q) * 128:(kt0 + q + n) * 128, c0:c0 + 512].rearrange(
                        "(kt p) c -> p kt c", p=128)
                    S.dma('sp', a[:, q:q + n, :], src, writes=[('p32', i, q // 4)], sem='p32_%d' % i)
                r32 = [('p32', i, q) for q in range((KT + 3) // 4)]
                if form == 'F':
                    ov = b[:, 0:4 * KT * 128].rearrange("p (nt kt c) -> p kt nt c", nt=4, kt=KT)
                    iv = a[:, 0:KT, :].rearrange("p kt (nt c) -> p kt nt c", c=128)
                else:
                    ov = b[:, 0:KT * 512].rearrange("p (kt c) -> p kt c", kt=KT)
                    iv = a[:, 0:KT, :]
                if gain is not None:
                    for kt in range(KT):
                        cnt[0] += 1
                        if cnt[0] % 2:
                            self.act(ov[:, kt], iv[:, kt], AF.Copy, r32 + [gkey], [('p16', i)],
                                     scale=gain[:, kt0 + kt:kt0 + kt + 1])
                        else:
                            S.op('dve', lambda e, kt=kt: e.tensor_scalar(
                                ov[:, kt], iv[:, kt], gain[:, kt0 + kt:kt0 + kt + 1], None, ALU.mult),
                                r32 + [gkey], [('p16', i)])
                else:
                    h = KT // 2
                    self.act(ov[:, 0:h], iv[:, 0:h], AF.Copy, r32, [('p16', i)])
                    S.op('dve', lambda e: e.tensor_copy(ov[:, h:KT], iv[:, h:KT]), r32, [('p16', i)])
                if form == 'F':
                    n = dst.shape[0]
                    S.dma('act', dst.rearrange("nt p f -> p nt f"),
                          b[:, 0:n * KT * 128].rearrange("p (nt f) -> p nt f", nt=n),
                          reads=[('p16', i)], writes=['wscr'], sem='p16_%d' % i)
                else:
                    S.dma('act', dst, b[:, 0:KT * 512], reads=[('p16', i)], writes=['wscr'], sem='p16_%d' % i)

            if self.kind in 'AF':
                Win = self.w_in[l]
                fcols = ([O_QM + 512 * j for j in range(4)] + [O_QD + 512 * j for j in range(4)] +
                         [O_US + 512 * j for j in range(2)] + [O_G + 512 * j for j in range(12)])
                for j, c0 in enumerate(fcols):
                    block(Win, c0, 0, 16, gp, 'pgp', 'F', self.WF_in[4 * j:4 * j + 4])
                tcols = [O_VM, O_VM + 512, O_OM, O_OM + 512, O_VD, O_VD + 512]
                for j, c0 in enumerate(tcols):
                    block(Win, c0, 0, 16, gp, 'pgp', 'T', self.WT_in[j])
            if self.kind in 'BF':
                for j in range(4):
                    block(self.w_glu[l], 512 * j, 0, 8, None, None, 'F', self.WF_glu[4 * j:4 * j + 4])
            if self.kind in 'CF':
                for n in range(3):
                    for j in range(4):
                        block(self.w_branch[l, n], 512 * j, 0, 8, None, None, 'F',
                              self.WF_br[n * 16 + 4 * j:n * 16 + 4 * j + 4])
                for j in range(4):
                    block(self.w_out[l], 512 * j, 0, 16, None, None, 'T', self.WT_out[j])
                for j in range(22):
                    block(self.w_up[l], 512 * j, 0, 16, gf, 'pgf', 'F', self.WF_up[4 * j:4 * j + 4])
                for j in range(4):
                    for kp in range(4):
                        block(self.w_down[l], 512 * j, kp * 11, 11, None, None, 'T', self.WT_dn[j * 4 + kp])
            S.barrier()

    def norm_T(self, xt, xkey, xn, xnkey, ss, sskey, hT, hkey, tt, psA, psB):
        S = self.S
        S.op('pool', lambda e: e.memset(ss, 0.0), [], [sskey])
        self.act(xn, xt, AF.Square, [xkey], [xnkey, sskey], accum_out=ss)
        self.rstd(ss, D, sskey)
        S.op('dve', lambda e: e.tensor_scalar(xn, xt, ss, None, ALU.mult), [xkey, sskey], [xnkey])
        for g in range(4):
            ps = psA if g % 2 == 0 else psB
            pk = ('ps', ps)
            for j in range(4):
                kt = g * 4 + j
                self.mm(self.PS[ps][:, j * 128:(j + 1) * 128], xn[:, kt * 128:(kt + 1) * 128], self.IDB[:],
                        True, True, [xnkey, 'idb'], [pk], last=(j == 3))
            dst = hT[:, g * 4:g * 4 + 4, tt * 128:(tt + 1) * 128]
            src = self.PS[ps][:, :].rearrange("p (a b) -> p a b", a=4)
            if g % 2 == 0:
                self.act(dst, src, AF.Copy, [pk], [hkey])
            else:
                S.op('dve', lambda e, dst=dst, src=src: e.tensor_copy(dst, src), [pk], [hkey])

    def phase_inproj(self, l, xin):
        nc, S = self.nc, self.S
        with ExitStack() as es:
            sb = lambda n, s, d: self.sb(es, n, s, d)
            hT = [sb('hT%d' % i, [128, 16, 512], BF16) for i in range(2)]
            xt = [sb('xt%d' % i, [128, D], F32) for i in range(2)]
            xn = [sb('xn%d' % i, [128, D], BF16) for i in range(2)]
            ss = [sb('ss%d' % i, [128, 1], F32) for i in range(2)]
            wf = [sb('wf%d' % i, [128, 16, 128], BF16) for i in range(3)]
            wt = [sb('wt%d' % i, [128, 16, 512], BF16) for i in range(2)]
            stg = [sb('stg%d' % i, [128, 512], BF16) for i in range(4)]
            stf = [sb('stf%d' % i, [128, 512], F32) for i in range(2)]
            Ub = [sb('U%d' % i, [128, 515], F32) for i in range(2)]
            acc = [sb('acc%d' % i, [128, 512], F32) for i in range(2)]
            H = sb('halo', [128, 16, 3], F32)
            wc = sb('wc', [128, 16, 4], F32)
            wif32 = sb('wif32', [128, 16, 8], F32)
            wif = sb('wif', [128, 16, 8], BF16)
            gp = sb('gp', [128, 16], F32)
            bifb = sb('bifb', [128, 8], F32)
            lifs = [sb('lifs%d' % i, [128, 8], F32) for i in range(2)]
            S.dma('sp', wc[:], self.mconv[l], writes=['wc'], sem='c')
            S.dma('sp', gp[:], self.gpre[l], writes=['gp'], sem='c')
            S.dma('sp', bifb[:], self.bif[l].partition_broadcast(128), writes=['bifb'], sem='c')
            S.dma('sp', wif32[:], self.w_in[l][:, O_I:O_I + 8].rearrange("(kt p) c -> p kt c", p=128),
                  writes=['wif32'], sem='c', allow_slow_non_contiguous=True)
            for kt in range(16):
                S.op('dve', lambda e, kt=kt: e.tensor_scalar(wif[:, kt], wif32[:, kt], gp[:, kt:kt + 1], None, ALU.mult),
                     ['wif32', 'gp'], ['wif'])
            r_x, r_wf, r_wt, r_stg, r_stf, r_U, r_ps = (Ring('x', 2), Ring('wf', 3), Ring('wt', 2), Ring('stg', 4),
                                                        Ring('stf', 2), Ring('U', 2), Ring('ps', 4))
            cnt = 0
            for tb in range(8):
                hi = tb % 2
                hk = ('hT', hi)
                t0 = tb * 512
                for tt in range(4):
                    i = r_x.next()
                    S.dma('sp', xt[i][:], xin[t0 + tt * 128:t0 + (tt + 1) * 128, :], writes=[('xt', i)], sem='xt%d' % i)
                    self.norm_T(xt[i][:], ('xt', i), xn[i][:], ('xn', i), ss[i][:], ('ss', i), hT[hi], hk, tt, 4, 5)
                if tb % 4 == 0:
                    S.op('pool', lambda e: e.memset(H[:], 0.0), [], ['halo'])
                for nt in range(88):
                    iw = r_wf.next()
                    S.dma('sp', wf[iw][:], self.WF_in[nt].rearrange("p (kt c) -> p kt c", kt=16),
                          reads=['wscr'], writes=[('wf', iw)], sem='wf%d' % iw)
                    ps = r_ps.next()
                    pk = ('ps', ps)
                    for kt in range(16):
                        self.mm(self.PS[ps][:, :], wf[iw][:, kt, :], hT[hi][:, kt, :], kt == 0, kt == 15,
                                [('wf', iw), hk], [pk])
                    P = self.PS[ps][:, :]
                    cnt += 1
                    if nt < 16:
                        iu = r_U.next()
                        uk = ('U', iu)
                        Ut = Ub[iu]
                        self.act(Ut[:, 3:515], P, AF.Copy, [pk], [uk])
                        S.op('pool', lambda e, Ut=Ut, nt=nt: e.tensor_copy(Ut[:, 0:3], H[:, nt, :]), ['halo'], [uk])
                        S.op('pool', lambda e, Ut=Ut, nt=nt: e.tensor_copy(H[:, nt, :], Ut[:, 512:515]), [uk], ['halo'])
                        ak = ('acc', iu)
                        A = acc[iu]
                        S.op('dve', lambda e, Ut=Ut, A=A, nt=nt: e.tensor_scalar(A[:], Ut[:, 3:515], wc[:, nt, 3:4], None, ALU.mult),
                             [uk, 'wc'], [ak])
                        for j in (2, 1, 0):
                            S.op('dve', lambda e, Ut=Ut, A=A, nt=nt, j=j: e.scalar_tensor_tensor(
                                out=A[:], in0=Ut[:, j:j + 512], scalar=wc[:, nt, j:j + 1], in1=A[:],
                                op0=ALU.mult, op1=ALU.add), [uk, 'wc', ak], [ak])
                        ig = r_stg.next()
                        if nt < 8:
                            self.act(stg[ig][:], A[:], AF.Silu, [ak], [('stg', ig)])
                        else:
                            self.act(A[:], A[:], AF.Silu, [ak], [ak])
                            S.op('pool', lambda e, A=A, ig=ig: e.tensor_scalar(stg[ig][:], A[:], 1.0 / 16, None, ALU.mult),
                                 [ak], [('stg', ig)])
                        S.dma('act', self.qkm[nt][:, t0:t0 + 512], stg[ig][:], reads=[('stg', ig)], writes=['qkm'],
                              sem='stg%d' % ig)
                    elif nt < 32:
                        ig = r_stg.next()
                        if cnt % 2:
                            self.act(stg[ig][:], P, AF.Copy, [pk], [('stg', ig)])
                        else:
                            S.op('dve', lambda e, ig=ig, P=P: e.tensor_copy(stg[ig][:], P), [pk], [('stg', ig)])
                        S.dma('act', self.qkd[nt - 16][:, t0:t0 + 512], stg[ig][:], reads=[('stg', ig)], writes=['qkd'],
                              sem='stg%d' % ig)
                    elif nt < 40:
                        ig = r_stf.next()
                        S.op('dve', lambda e, ig=ig, P=P: e.tensor_copy(stf[ig][:], P), [pk], [('stf', ig)])
                        S.dma('act', self.uT[nt - 32][:, t0:t0 + 512], stf[ig][:], reads=[('stf', ig)], writes=['uT'],
                              sem='stf%d' % ig)
                    else:
                        ig = r_stg.next()
                        self.act(stg[ig][:], P, AF.Sigmoid, [pk], [('stg', ig)])
                        S.dma('act', self.gT[nt - 40][:, t0:t0 + 512], stg[ig][:], reads=[('stg', ig)], writes=['gT'],
                              sem='stg%d' % ig)
                for j in range(6):
                    iw = r_wt.next()
                    for q in range(4):
                        S.dma('sp', wt[iw][:, 4 * q:4 * q + 4, :],
                              self.WT_in[j][:, 4 * q * 512:(4 * q + 4) * 512].rearrange("p (kt c) -> p kt c", kt=4),
                              reads=['wscr'], writes=[('wt', iw, q)], sem='wt%d' % iw)
                    wk = [('wt', iw, q) for q in range(4)]
                    dst, c0 = [(self.vm, 0), (self.vm, 512), (self.om, 0), (self.om, 512), (self.vd, 0), (self.vd, 512)][j]
                    for tt in range(4):
                        ps = r_ps.next()
                        pk = ('ps', ps)
                        for kt in range(16):
                            self.mm(self.PS[ps][:, :], hT[hi][:, kt, tt * 128:(tt + 1) * 128], wt[iw][:, kt, :],
                                    kt == 0, kt == 15, wk + [hk], [pk])
                        ig = r_stg.next()
                        P = self.PS[ps][:, :]
                        if j in (2, 3):
                            self.act(stg[ig][:], P, AF.Sigmoid, [pk], [('stg', ig)])
                        else:
                            S.op('dve', lambda e, ig=ig, P=P: e.tensor_copy(stg[ig][:], P), [pk], [('stg', ig)])
                        S.dma('act', dst[t0 + tt * 128:t0 + (tt + 1) * 128, c0:c0 + 512], stg[ig][:],
                              reads=[('stg', ig)], writes=['tok'], sem='stg%d' % ig)
                for tt in range(4):
                    ps = r_ps.next()
                    pk = ('ps', ps)
                    for kt in range(16):
                        self.mm(self.PS[ps][:, 0:8], hT[hi][:, kt, tt * 128:(tt + 1) * 128], wif[:, kt, :],
                                kt == 0, kt == 15, ['wif', hk], [pk])
                    il = tt % 2
                    lk = ('lifs', il)
                    Lt = lifs[il]
                    S.op('dve', lambda e, Lt=Lt, ps=ps: e.tensor_tensor(Lt[:], self.PS[ps][:, 0:8], bifb[:], ALU.add),
                         [pk, 'bifb'], [lk])
                    self.act(Lt[:, 4:8], Lt[:, 4:8], AF.Exp, [lk], [lk], scale=-1.0)
                    self.act(Lt[:, 4:8], Lt[:, 4:8], AF.Ln, [lk], [lk], bias=1.0)
                    S.op('dve', lambda e, Lt=Lt: e.tensor_scalar(Lt[:, 4:8], Lt[:, 4:8], -1.0, None, ALU.mult), [lk], [lk])
                    S.dma('act', self.lif[t0 + tt * 128:t0 + (tt + 1) * 128, :], Lt[:], reads=[lk], writes=['tok'],
                          sem='lifs%d' % il)
            S.barrier()

    def mlstm_gen(self, l, es):
        nc, S = self.nc, self.S
        PS = self.PS
        if True:
            sb = lambda n, s, d: self.sb(es, n, s, d)
            qk = [sb('mqk%d' % i, [128, 16, 512], BF16) for i in range(2)]
            va = [sb('mva%d' % i, [128, 4, 257], BF16) for i in range(2)]
            os_ = [sb('mo%d' % i, [128, 1024], BF16) for i in range(2)]
            lf = [sb('mlf%d' % i, [128, 8], F32) for i in range(2)]
            sm = [sb('msm%d' % i, [128, 40], F32) for i in range(2)]
            LFb = sb('mLFb', [128, 4, 128], F32)
            At = sb('mAt', [128, 128], F32)
            Pt = sb('mPt', [128, 128], BF16)
            isb = sb('misb', [128, 257], F32)
            tot = sb('mtot', [128, 257], F32)
            hh = sb('mhh', [128, 256], F32)
            junk = sb('mjunk', [128, 256], BF16)
            s1 = sb('ms1', [128, 4], F32)
            kw = sb('mkw', [128, 256], BF16)
            Cst = sb('mC', [128, 4, 2, 257], F32)
            Cb = sb('mCb', [128, 4, 2, 257], BF16)
            gno = [sb('mgno%d' % i, [128, 1024], F32) for i in range(2)]
            ya = [sb('mya%d' % i, [128, 1024], BF16) for i in range(2)]
            yst = [sb('myst%d' % i, [128, 8, 128], BF16) for i in range(2)]
            mnb = sb('mnb', [128, 1024], F32)
            S.dma('sp', mnb[:], self.mnorm[l].partition_broadcast(128), writes=['mnb'], sem='c')
            for i in range(2):
                S.op('pool', lambda e, i=i: e.memset(va[i][:, :, 256:257], 1.0), [], [('mva', i)])
            ci = -1
            for b in range(NB):
                S.op('pool', lambda e: e.memset(Cst[:], 0.0), [], ['mC'])
                S.op('pool', lambda e: e.memset(Cb[:], 0.0), [], ['mCb'])
                for c in range(16):
                    ci += 1
                    i2 = ci % 2
                    tok0 = b * L + c * 128
                    if c % 4 == 0:
                        iq = (ci // 4) % 2
                        for q in range(4):
                            S.dma('sp', qk[iq][:, 4 * q:4 * q + 4, :],
                                  self.qkm[4 * q:4 * q + 4, :, tok0:tok0 + 512].rearrange("n p t -> p n t"),
                                  reads=['qkm'], writes=[('mqk', iq, q)], sem='mqk%d' % iq)
                    qkk = [('mqk', iq, q) for q in range(4)]
                    co = (c % 4) * 128
                    S.dma('sp', va[i2][:, :, 0:256], self.vm[tok0:tok0 + 128, :].rearrange("t (h d) -> t h d", h=4),
                          reads=['tok'], writes=[('mva', i2)], sem='mva%d' % i2)
                    S.dma('sp', os_[i2][:], self.om[tok0:tok0 + 128, :], reads=['tok'], writes=[('mo', i2)], sem='mo%d' % i2)
                    S.dma('sp', lf[i2][:], self.lif[tok0:tok0 + 128, :], reads=['tok'], writes=[('mlf', i2)], sem='mlf%d' % i2)
                    lfk, smk, vak = ('mlf', i2), ('msm', i2), ('mva', i2)
                    LF, SM = lf[i2], sm[i2]
                    self.mm(PS[4][:, 0:4], self.U, LF[:, 4:8], True, True, ['cst', lfk], [('ps', 4)], last=False)
                    self.mm(PS[4][:, 4:8], self.ONES, LF[:, 4:8], True, True, ['cst', lfk], [('ps', 4)])
                    self.act(SM[:, 0:8], PS[4][:, 0:8], AF.Copy, [('ps', 4)], [smk])
                    self.act(SM[:, 8:16], SM[:, 0:8], AF.Exp, [smk], [smk])
                    S.op('dve', lambda e, SM=SM: e.tensor_tensor(SM[:, 16:20], SM[:, 4:8], SM[:, 0:4], ALU.subtract), [smk], [smk])
                    S.op('dve', lambda e, SM=SM, LF=LF: e.tensor_tensor(SM[:, 16:20], SM[:, 16:20], LF[:, 0:4], ALU.add), [smk, lfk], [smk])
                    self.act(SM[:, 16:20], SM[:, 16:20], AF.Exp, [smk], [smk])
                    S.op('dve', lambda e, SM=SM, LF=LF: e.tensor_tensor(SM[:, 20:24], LF[:, 0:4], SM[:, 0:4], ALU.subtract), [smk, lfk], [smk])
                    for h in range(4):
                        S.op('pool', lambda e, h=h, LF=LF: e.tensor_scalar(LFb[:, h, :], self.ONES, LF[:, 4 + h:5 + h], None, ALU.mult),
                             ['cst', lfk], ['mLFb'])
                    S.op('pool', lambda e, i2=i2: e.tensor_tensor(gno[i2][:], os_[i2][:], mnb[:], ALU.mult),
                         [('mo', i2), 'mnb'], [('mgno', i2)])
                    for h in range(4):
                        qT = [qk[iq][:, 2 * h + j, co:co + 128] for j in range(2)]
                        kT = [qk[iq][:, 8 + 2 * h + j, co:co + 128] for j in range(2)]
                        self.mm(PS[0][:, 0:128], LFb[:, h, :], self.U, True, False, ['mLFb', 'cst'], [('ps', 0)], last=False)
                        self.mm(PS[0][:, 0:128], self.ID32, self.NEGM, False, True, ['cst'], [('ps', 0)])
                        self.act(At[:], PS[0][:, 0:128], AF.Exp, [('ps', 0), smk], ['mAt'], bias=SM[:, 20 + h:21 + h])
                        for j in range(2):
                            self.mm(PS[1][:, 0:128], kT[j], qT[j], j == 0, j == 1, qkk, [('ps', 1)])
                        S.op('dve', lambda e: e.tensor_tensor(Pt[:], At[:], PS[1][:, 0:128], ALU.mult), ['mAt', ('ps', 1)], ['mPt'])
                        self.mm(PS[2][:, 0:257], Pt[:], va[i2][:, h, :], True, True, ['mPt', vak], [('ps', 2)])
                        for j in range(2):
                            self.mm(PS[3][:, 0:257], qT[j], Cb[:, h, j, :], j == 0, j == 1, qkk + ['mCb'], [('ps', 3)])
                        self.act(isb[:], PS[3][:, 0:257], AF.Copy, [('ps', 3), smk], ['misb'], scale=SM[:, 8 + h:9 + h])
                        S.op('dve', lambda e: e.tensor_tensor(tot[:], isb[:], PS[2][:, 0:257], ALU.add), ['misb', ('ps', 2)], ['mtot'])
                        self.act(s1[:, 0:1], tot[:, 256:257], AF.Abs, ['mtot'], ['ms1'])
                        S.op('dve', lambda e: e.tensor_scalar_max(s1[:, 0:1], s1[:, 0:1], 1.0), ['ms1'], ['ms1'])
                        S.op('dve', lambda e: e.reciprocal(s1[:, 0:1], s1[:, 0:1]), ['ms1'], ['ms1'])
                        S.op('dve', lambda e: e.tensor_scalar(hh[:], tot[:, 0:256], s1[:, 0:1], None, ALU.mult), ['mtot', 'ms1'], ['mhh'])
                        S.op('pool', lambda e: e.memset(s1[:, 1:2], 0.0), [], ['ms1b'])
                        self.act(junk[:], hh[:], AF.Square, ['mhh'], ['mjunk', 'ms1b'], accum_out=s1[:, 1:2])
                        self.rstd(s1[:, 1:2], 256, 'ms1b')
                        S.op('dve', lambda e, h=h, i2=i2: e.scalar_tensor_tensor(
                            out=ya[i2][:, h * 256:(h + 1) * 256], in0=hh[:], scalar=s1[:, 1:2],
                            in1=gno[i2][:, h * 256:(h + 1) * 256], op0=ALU.mult, op1=ALU.mult),
                            ['mhh', 'ms1b', ('mgno', i2)], [('mya', i2)])
                        for j in range(2):
                            self.mm(PS[4][:, j * 128:(j + 1) * 128], kT[j], self.IDB[:], True, True, qkk + ['idb'], [('ps', 4)],
                                    last=(j == 1))
                        self.act(kw[:], PS[4][:, 0:256], AF.Copy, [('ps', 4), smk], ['mkw'], scale=SM[:, 16 + h:17 + h])
                        for j in range(2):
                            self.mm(PS[(5, 7)[j]][:, 0:257], kw[:, j * 128:(j + 1) * 128], va[i2][:, h, :], True, True,
                                    ['mkw', vak], [('ps', (5, 7)[j])])
                            S.op('dve', lambda e, h=h, j=j, SM=SM: e.scalar_tensor_tensor(
                                out=Cst[:, h, j, :], in0=Cst[:, h, j, :], scalar=SM[:, 12 + h:13 + h], in1=PS[(5, 7)[j]][:, 0:257],
                                op0=ALU.mult, op1=ALU.add), ['mC', smk, ('ps', (5, 7)[j])], ['mC'])
                            S.op('pool', lambda e, h=h, j=j: e.tensor_copy(Cb[:, h, j, :], Cst[:, h, j, :]), ['mC'], ['mCb'])
                    for g in range(2):
                        for j in range(4):
                            self.mm(PS[4][:, j * 128:(j + 1) * 128], ya[i2][:, (g * 4 + j) * 128:(g * 4 + j + 1) * 128], self.IDB[:],
                                    True, True, [('mya', i2), 'idb'], [('ps', 4)], last=(j == 3))
                        self.act(yst[i2][:, g * 4:g * 4 + 4, :], PS[4][:, :].rearrange("p (a b) -> p a b", a=4), AF.Copy,
                                 [('ps', 4)], [('myst', i2)])
                    S.dma('act', self.yT[0:8, :, tok0:tok0 + 128].rearrange("n p t -> p n t"), yst[i2][:],
                          reads=[('myst', i2)], writes=['yT'], sem='myst%d' % i2)
                    yield

    def attn_gen(self, l, es):
        nc, S = self.nc, self.S
        PS = self.PS
        scale = 128 ** -0.5
        if True:
            sb = lambda n, s, d: self.sb(es, n, s, d)
            wide = sb('awide', [128, 4, 1024], F32)
            cfar = sb('acfar', [128, 4], F32)
            lamT = sb('alamT', [128, 4], F32)
            lm = sb('alm', [128, 4], F32)
            dnb = sb('adnb', [128, 1024], F32)
            kseq = [sb('akseq%d' % i, [128, L], BF16) for i in range(2)]
            qblk = [sb('aqblk%d' % i, [128, 512], BF16) for i in range(2)]
            vaug = sb('avaug', [128, 16, 257], BF16)
            tmp = [sb('atmp%d' % i, [128, 512], F32) for i in range(2)]
            pT = [sb('apT%d' % i, [128, 512], BF16) for i in range(3)]
            s4 = sb('as4', [128, 4], F32)
            o0 = sb('ao0', [128, 4, 256], F32)
            oo = sb('aoo', [128, 256], F32)
            junk = sb('ajunk', [128, 256], BF16)
            yb = sb('ayb', [128, 256], BF16)
            yst = [sb('ayst%d' % i, [128, 2, 128], BF16) for i in range(2)]
            S.dma('sp', wide[:], self.wide[:, :, :], writes=['awide'], sem='c')
            S.dma('sp', cfar[:], self.cfar[:, :], writes=['acfar'], sem='c')
            S.dma('sp', lamT[:], self.lamT[l], writes=['alamT'], sem='c')
            S.dma('sp', dnb[:], self.dnorm[l].partition_broadcast(128), writes=['adnb'], sem='c')
            lami = sb('alami', [128, 2], F32)
            S.dma('sp', lami[:], self.lami[l], writes=['alami'], sem='c')
            S.op('pool', lambda e: e.tensor_scalar(dnb[:], dnb[:], lami[:, 0:1], None, ALU.mult), ['adnb', 'alami'], ['adnb'])
            S.op('dve', lambda e: e.tensor_tensor(lm[:, 0:1], lamT[:, 0:1], lamT[:, 1:2], ALU.mult), ['alamT'], ['alm'])
            S.op('dve', lambda e: e.tensor_tensor(lm[:, 1:2], lamT[:, 2:3], lamT[:, 3:4], ALU.mult), ['alamT', 'alm'], ['alm'])
            self.mm(PS[7][:, 0:2], self.ONES, lm[:, 0:2], True, True, ['cst', 'alm'], [('ps', 7)])
            self.act(lm[:, 2:4], PS[7][:, 0:2], AF.Exp, [('ps', 7)], ['alm'])
            S.op('dve', lambda e: e.tensor_tensor(lm[:, 0:1], lm[:, 3:4], lm[:, 2:3], ALU.subtract), ['alm'], ['alm'])
            S.op('dve', lambda e: e.tensor_scalar(lm[:, 0:1], lm[:, 0:1], lami[:, 1:2], None, ALU.add), ['alm', 'alami'], ['alm'])
            S.op('pool', lambda e: e.memset(vaug[:, :, 256:257], 1.0), [], ['avaug'])
            r_q, r_tmp, r_pT, r_st, r_y = Ring('q', 2), Ring('tmp', 2), Ring('pT', 3), Ring('st', 2), Ring('y', 2)
            for b in range(NB):
                for h in range(4):
                    S.dma('sp', vaug[:, :, 0:256],
                          self.vd[b * L:(b + 1) * L, h * 256:(h + 1) * 256].rearrange("(kt p) d -> p kt d", p=128),
                          reads=['tok'], writes=['avaug'], sem='avaug')
                    for m in range(2):
                        S.dma('sp', kseq[m][:], self.qkd[8 + 2 * h + m][:, b * L:(b + 1) * L], reads=['qkd'],
                              writes=[('akseq', m)], sem='akseq%d' % m)
                    for I in range(4):
                        for m in range(2):
                            iq = r_q.next()
                            S.dma('sp', qblk[iq][:], self.qkd[2 * h + m][:, b * L + I * 512:b * L + (I + 1) * 512],
                                  reads=['qkd'], writes=[('aqblk', iq)], sem='aqblk%d' % iq)
                            for j in range(4 * I + 4):
                                ist = r_st.next()
                                pk = ('ps', 4 + ist)
                                self.mm(PS[4 + ist][:, :], kseq[m][:, j * 128:(j + 1) * 128], qblk[iq][:], True, True,
                                        [('akseq', m), ('aqblk', iq)], [pk])
                                ip = r_pT.next()
                                ppk = ('apT', ip)
                                if j <= 4 * I - 2:
                                    self.act(pT[ip][:], PS[4 + ist][:, :], AF.Exp, [pk, 'acfar'], [ppk], scale=scale,
                                             bias=cfar[:, h:h + 1])
                                else:
                                    x0 = (4 * I - j) * 128 + 384
                                    it = r_tmp.next()
                                    S.op('dve', lambda e, it=it, ist=ist, x0=x0, h=h: e.scalar_tensor_tensor(
                                        out=tmp[it][:], in0=PS[4 + ist][:, :], scalar=scale, in1=wide[:, h, x0:x0 + 512],
                                        op0=ALU.mult, op1=ALU.add), [pk, 'awide'], [('atmp', it)])
                                    self.act(pT[ip][:], tmp[it][:], AF.Exp, [('atmp', it)], [ppk])
                                for qi in range(4):
                                    if 4 * I + qi < j:
                                        continue
                                    self.mm(PS[qi][:, 0:257], pT[ip][:, qi * 128:(qi + 1) * 128], vaug[:, j, :],
                                            j == 0, j == 4 * I + qi, [ppk, 'avaug'], [('ps', qi)])
                            for qi in range(4):
                                acc = PS[qi][:, 0:257]
                                ak_ = ('ps', qi)
                                S.op('dve', lambda e, acc=acc: e.reciprocal(s4[:, 0:1], acc[:, 256:257]), [ak_], ['as4'])
                                if m == 0:
                                    self.act(o0[:, qi, :], acc[:, 0:256], AF.Copy, [ak_, 'as4'], [('ao0', qi)], scale=s4[:, 0:1])
                                    continue
                                S.op('dve', lambda e: e.tensor_tensor(s4[:, 1:2], s4[:, 0:1], lm[:, 0:1], ALU.mult), ['as4', 'alm'], ['as4'])
                                S.op('dve', lambda e, acc=acc, qi=qi: e.scalar_tensor_tensor(
                                    out=oo[:], in0=acc[:, 0:256], scalar=s4[:, 1:2], in1=o0[:, qi, :], op0=ALU.mult, op1=ALU.add),
                                    [ak_, 'as4', ('ao0', qi)], ['aoo'])
                                S.op('pool', lambda e: e.memset(s4[:, 2:3], 0.0), [], ['as4b'])
                                self.act(junk[:], oo[:], AF.Square, ['aoo'], ['ajunk', 'as4b'], accum_out=s4[:, 2:3])
                                self.rstd(s4[:, 2:3], 256, 'as4b')
                                S.op('dve', lambda e, h=h: e.scalar_tensor_tensor(out=yb[:], in0=oo[:], scalar=s4[:, 2:3],
                                                                                  in1=dnb[:, h * 256:(h + 1) * 256],
                                                                                  op0=ALU.mult, op1=ALU.mult),
                                     ['aoo', 'as4b', 'adnb'], ['ayb'])
                                iy = r_y.next()
                                for j in range(2):
                                    self.mm(PS[7][:, j * 128:(j + 1) * 128], yb[:, j * 128:(j + 1) * 128], self.IDB[:], True, True,
                                            ['ayb', 'idb'], [('ps', 7)], last=(j == 1))
                                self.act(yst[iy][:], PS[7][:, 0:256].rearrange("p (a b) -> p a b", a=2), AF.Copy, [('ps', 7)],
                                         [('ayst', iy)])
                                tok0 = b * L + I * 512 + qi * 128
                                S.dma('act', self.yT[8 + 2 * h:8 + 2 * h + 2, :, tok0:tok0 + 128].rearrange("n p t -> p n t"),
                                      yst[iy][:], reads=[('ayst', iy)], writes=['yT'], sem='ayst%d' % iy)
                            yield

    def s5_setup(self, l, es):
        nc, S = self.nc, self.S
        PS = self.PS
        if True:
            sb = lambda n, s, d: self.sb(es, n, s, d)
            pw = sb('spw', [128, 12, 2, 32], F32)
            d8 = sb('sd8', [128, 8], F32)
            pj = sb('spj', [128, 16, 3, 32], F32)
            ER = [sb('sER%d' % i, [128, 128], F32) for i in range(2)]
            EI = [sb('sEI%d' % i, [128, 128], F32) for i in range(2)]
            npw = sb('snpw', [128, 11, 32], F32)
            es_tab = ExitStack()
            sbt = lambda n, s, d: self.sb(es_tab, n, s, d)
            bbr = sbt('sbbr', [128, 32, 128], F32)
            bbi = sbt('sbbi', [128, 32, 128], F32)
            cpr = sbt('scpr', [128, 32, 128], F32)
            cpi = sbt('scpi', [128, 32, 128], F32)
            S.dma('sp', d8[:], self.s5d[l], writes=['sd8'], sem='c')
            def zcalc(es2, tag, n, srcs, want_z):
                sb2 = lambda nm, s_, d_: self.sb(es2, nm, s_, d_)
                lr, li_, ld = (sb2('sz%s%d' % (tag, i), [128, n], F32) for i in range(3))
                t = [sb2('szt%s%d' % (tag, i), [128, n], F32) for i in range(6)]
                k = 'sz' + tag
                for i, dst in enumerate((lr, li_, ld)):
                    S.dma('sp', dst[:], srcs[i], writes=[k], sem='c')
                V = lambda fn: S.op('dve', fn, [k], [k])
                A = lambda out, in_, f, **kw: self.act(out, in_, f, [k], [k], **kw)
                A(ld[:], ld[:], AF.Exp)
                V(lambda e: e.tensor_tensor(t[0][:], lr[:], ld[:], ALU.mult))
                A(t[0][:], t[0][:], AF.Exp, scale=1.0 / 32)
                V(lambda e: e.tensor_tensor(t[1][:], li_[:], ld[:], ALU.mult))
                A(t[2][:], t[1][:], AF.Sin, scale=1.0 / 32)
                V(lambda e: e.tensor_scalar(t[1][:], t[1][:], 1.0 / 32, math.pi / 2, ALU.mult, ALU.add))
                A(t[1][:], t[1][:], AF.Sin)
                V(lambda e: e.tensor_tensor(t[1][:], t[1][:], t[0][:], ALU.mult))
                V(lambda e: e.tensor_tensor(t[2][:], t[2][:], t[0][:], ALU.mult))
                for _ in range(5):
                    V(lambda e: e.tensor_tensor(t[3][:], t[1][:], t[1][:], ALU.mult))
                    V(lambda e: e.tensor_tensor(t[4][:], t[2][:], t[2][:], ALU.mult))
                    V(lambda e: e.tensor_tensor(t[2][:], t[1][:], t[2][:], ALU.mult))
                    V(lambda e: e.tensor_scalar(t[2][:], t[2][:], 2.0, None, ALU.mult))
                    V(lambda e: e.tensor_tensor(t[1][:], t[3][:], t[4][:], ALU.subtract))
                are, aim = t[1], t[2]
                if not want_z:
                    return are, aim, k
                V(lambda e: e.tensor_tensor(t[3][:], lr[:], lr[:], ALU.mult))
                V(lambda e: e.tensor_tensor(t[4][:], li_[:], li_[:], ALU.mult))
                V(lambda e: e.tensor_tensor(t[3][:], t[3][:], t[4][:], ALU.add))
                V(lambda e: e.reciprocal(t[3][:], t[3][:]))
                V(lambda e: e.tensor_scalar(t[0][:], are[:], -1.0, None, ALU.add))
                V(lambda e: e.tensor_tensor(t[4][:], t[0][:], lr[:], ALU.mult))
                V(lambda e: e.tensor_tensor(t[5][:], aim[:], li_[:], ALU.mult))
                V(lambda e: e.tensor_tensor(t[4][:], t[4][:], t[5][:], ALU.add))
                V(lambda e: e.tensor_tensor(t[4][:], t[4][:], t[3][:], ALU.mult))
                V(lambda e: e.tensor_tensor(t[5][:], aim[:], lr[:], ALU.mult))
                V(lambda e: e.tensor_tensor(t[0][:], t[0][:], li_[:], ALU.mult))
                V(lambda e: e.tensor_tensor(t[5][:], t[5][:], t[0][:], ALU.subtract))
                V(lambda e: e.tensor_tensor(t[5][:], t[5][:], t[3][:], ALU.mult))
                return t[4], t[5], k

            bbrf = bbr[:].rearrange("p a b -> p (a b)")
            bbif = bbi[:].rearrange("p a b -> p (a b)")
            for q in range(4):
                cs = slice(q * 1024, (q + 1) * 1024)
                with ExitStack() as es2:
                    zre, zim, zk = zcalc(es2, 'r', 1024, [self.s5r[l, i][:, cs] for i in range(3)], True)
                    bT = [self.sb(es2, 'sbT%d' % i, [128, 1024], F32) for i in range(2)]
                    t6 = self.sb(es2, 'st6', [128, 1024], F32)
                    for i in range(2):
                        S.dma('sp', bT[i][:], self.s5bT[l, i][:, cs], writes=['sbT'], sem='c')
                    V = lambda fn, w: S.op('dve', fn, [zk, 'sbT', 'st6', 'sbb'], w)
                    V(lambda e: e.tensor_tensor(bbrf[:, cs], zre[:], bT[0][:], ALU.mult), ['sbb'])
                    V(lambda e: e.tensor_tensor(t6[:], zim[:], bT[1][:], ALU.mult), ['st6'])
                    V(lambda e: e.tensor_tensor(bbrf[:, cs], bbrf[:, cs], t6[:], ALU.subtract), ['sbb'])
                    V(lambda e: e.tensor_tensor(bbif[:, cs], zre[:], bT[1][:], ALU.mult), ['sbb'])
                    V(lambda e: e.tensor_tensor(t6[:], zim[:], bT[0][:], ALU.mult), ['st6'])
                    V(lambda e: e.tensor_tensor(bbif[:, cs], bbif[:, cs], t6[:], ALU.add), ['sbb'])
                    S.barrier()
            with ExitStack() as es2:
                are, aim, ak = zcalc(es2, 'p', 32, [self.s5p[l, i] for i in range(3)], False)
                S.op('dve', lambda e: e.tensor_copy(pw[:, 0, 0, :], are[:]), [ak], ['spw'])
                S.op('dve', lambda e: e.tensor_copy(pw[:, 0, 1, :], aim[:]), [ak], ['spw'])
                tq = self.sb(es2, 'stq', [128, 2, 32], F32)
                for k in range(1, 11):
                    V = lambda fn, w: S.op('dve', fn, ['spw', 'stq'], w)
                    V(lambda e, k=k: e.tensor_tensor(tq[:, 0, :], pw[:, k - 1, 0, :], pw[:, k - 1, 0, :], ALU.mult), ['stq'])
                    V(lambda e, k=k: e.tensor_tensor(tq[:, 1, :], pw[:, k - 1, 1, :], pw[:, k - 1, 1, :], ALU.mult), ['stq'])
                    V(lambda e, k=k: e.tensor_tensor(pw[:, k, 0, :], tq[:, 0, :], tq[:, 1, :], ALU.subtract), ['spw'])
                    V(lambda e, k=k: e.tensor_tensor(tq[:, 0, :], pw[:, k - 1, 0, :], pw[:, k - 1, 1, :], ALU.mult), ['stq'])
                    V(lambda e, k=k: e.tensor_scalar(pw[:, k, 1, :], tq[:, 0, :], 2.0, None, ALU.mult), ['spw'])
                S.dma('sp', cpr[:].rearrange("p a b -> p (a b)"), self.s5cP[l, 0], writes=['scp'], sem='c')
                S.dma('sp', cpi[:].rearrange("p a b -> p (a b)"), self.s5cP[l, 1], writes=['scp'], sem='c')
                S.op('pool', lambda e: e.tensor_scalar(cpi[:], cpi[:], -1.0, None, ALU.mult), ['scp'], ['scp'])
                S.barrier()
            S.op('dve', lambda e: e.tensor_copy(pj[:, 0, 0:2, :], pw[:, 0, :, :]), ['spw'], ['spj'])
            with ExitStack() as es2:
                tj = self.sb(es2, 'stj', [128, 2, 32], F32)
                for j in range(1, 16):
                    V = lambda fn, w: S.op('dve', fn, ['spw', 'spj', 'stj'], w)
                    V(lambda e, j=j: e.tensor_tensor(tj[:, 0, :], pj[:, j - 1, 0, :], pw[:, 0, 0, :], ALU.mult), ['stj'])
                    V(lambda e, j=j: e.tensor_tensor(tj[:, 1, :], pj[:, j - 1, 1, :], pw[:, 0, 1, :], ALU.mult), ['stj'])
                    V(lambda e, j=j: e.tensor_tensor(pj[:, j, 0, :], tj[:, 0, :], tj[:, 1, :], ALU.subtract), ['spj'])
                    V(lambda e, j=j: e.tensor_tensor(tj[:, 0, :], pj[:, j - 1, 0, :], pw[:, 0, 1, :], ALU.mult), ['stj'])
                    V(lambda e, j=j: e.tensor_tensor(tj[:, 1, :], pj[:, j - 1, 1, :], pw[:, 0, 0, :], ALU.mult), ['stj'])
                    V(lambda e, j=j: e.tensor_tensor(pj[:, j, 1, :], tj[:, 0, :], tj[:, 1, :], ALU.add), ['spj'])
                S.op('dve', lambda e: e.tensor_scalar(pj[:, :, 2, :], pj[:, :, 1, :], -1.0, None, ALU.mult), ['spj'], ['spj'])
                S.barrier()
            S.op('dve', lambda e: e.tensor_scalar(npw[:], pw[:, 0:11, 1, :], -1.0, None, ALU.mult), ['spw'], ['snpw'])
            for i, tab in enumerate((bbr, bbi, cpr, cpi)):
                S.dma('act', self.s5tab[i], tab[:].rearrange("p a b -> p (a b)"), reads=['sbb', 'scp'], writes=['s5tab'], sem='c')
            S.barrier()
            es_tab.close()
            st = dict(pw=pw, npw=npw, pj=pj, d8=d8, ER=ER, EI=EI)
            st['tabs'] = [sb('stab%d' % i, [128, 4, 4, 128], F32) for i in range(2)]
            st['ut'] = sb('sut', [128, L], F32)
            st['XR'] = [sb('sXR%d' % i, [128, L], F32) for i in range(2)]
            st['XI'] = [sb('sXI%d' % i, [128, L], F32) for i in range(2)]
            st['Yacc'] = sb('sYacc', [128, L], F32)
            st['yg'] = sb('syg', [128, L], BF16)
            return st

    def s5_main(self, l, st):
        S, PS = self.S, self.PS
        pw, npw, pj, d8, ER, EI, tabs, ut, XR, XI, Yacc, yg = (st[k] for k in (
            'pw', 'npw', 'pj', 'd8', 'ER', 'EI', 'tabs', 'ut', 'XR', 'XI', 'Yacc', 'yg'))
        pk = ('ps', 6)
        bv = lambda X: X[:].rearrange("p (n j) -> p n j", j=16)

        def hs(R0, I0, R1, I1, kr0, ki0, kr1, ki1, k, d, W, gp_):
            pr, pi_, npi = pw[:, k, 0, gp_:gp_ + 1], pw[:, k, 1, gp_:gp_ + 1], npw[:, k, gp_:gp_ + 1]
            lo, hi = (lambda X: X[..., 0:W - d]), (lambda X: X[..., d:])
            hd = lambda X: X[..., 0:d]
            S.op('dve', lambda e: e.scalar_tensor_tensor(out=hi(R1), in0=lo(R0), scalar=pr, in1=hi(R0), op0=ALU.mult, op1=ALU.add),
                 [kr0, 'spw'], [kr1])
            S.op('dve', lambda e: e.scalar_tensor_tensor(out=hi(R1), in0=lo(I0), scalar=npi, in1=hi(R1), op0=ALU.mult, op1=ALU.add),
                 [ki0, 'snpw', kr1], [kr1])
            S.op('pool', lambda e: e.tensor_copy(hd(R1), hd(R0)), [kr0], [kr1])
            S.op('dve', lambda e: e.scalar_tensor_tensor(out=hi(I1), in0=lo(I0), scalar=pr, in1=hi(I0), op0=ALU.mult, op1=ALU.add),
                 [ki0, 'spw'], [ki1])
            S.op('dve', lambda e: e.scalar_tensor_tensor(out=hi(I1), in0=lo(R0), scalar=pi_, in1=hi(I1), op0=ALU.mult, op1=ALU.add),
                 [kr0, 'spw', ki1], [ki1])
            S.op('pool', lambda e: e.tensor_copy(hd(I1), hd(I0)), [ki0], [ki1])

        for ct in range(8):
            it = ct % 2
            TB, tk = tabs[it], ('stab', it)
            for i in range(4):
                S.dma('sp', TB[:, i], self.s5tab[i][:, ct * 512:(ct + 1) * 512].rearrange("p (a b) -> p a b", a=4),
                      reads=['s5tab'], writes=[tk], sem='stab%d' % it)
            for b in range(NB):
                S.dma('sp', ut[:], self.uT[ct][:, b * L:(b + 1) * L], reads=['uT'], writes=['sut'], sem='sut')
                for g4 in range(4):
                    gp_ = ct * 4 + g4
                    n = 0
                    for tb in range(4):
                        sl = slice(tb * 512, (tb + 1) * 512)
                        for ri, X in enumerate((XR[0], XI[0])):
                            n += 1
                            self.mm(PS[6][:, :], TB[:, ri, g4, :], ut[:, sl], True, True, [tk, 'sut'], [pk])
                            key = ('sX', ri, 0)
                            if n % 2:
                                self.act(X[:, sl], PS[6][:, :], AF.Copy, [pk], [key])
                            else:
                                S.op('dve', lambda e, X=X, sl=sl: e.tensor_copy(X[:, sl], PS[6][:, :]), [pk], [key])
                    cur = 0
                    for k in range(4):
                        nx = 1 - cur
                        hs(bv(XR[cur]), bv(XI[cur]), bv(XR[nx]), bv(XI[nx]), ('sX', 0, cur), ('sX', 1, cur), ('sX', 0, nx), ('sX', 1, nx),
                           k, 1 << k, 16, gp_)
                        cur = nx
                    XRv, XIv = bv(XR[cur]), bv(XI[cur])
                    kxr, kxi = ('sX', 0, cur), ('sX', 1, cur)
                    S.op('pool', lambda e, XRv=XRv: e.tensor_copy(ER[0][:], XRv[:, :, 15]), [kxr], [('sE', 0, 0)])
                    S.op('pool', lambda e, XIv=XIv: e.tensor_copy(EI[0][:], XIv[:, :, 15]), [kxi], [('sE', 1, 0)])
                    ec = 0
                    for k2 in range(7):
                        nx = 1 - ec
                        hs(ER[ec][:], EI[ec][:], ER[nx][:], EI[nx][:], ('sE', 0, ec), ('sE', 1, ec), ('sE', 0, nx), ('sE', 1, nx),
                           4 + k2, 1 << k2, 128, gp_)
                        ec = nx
                    FR, FI = ER[ec][:, 0:127], EI[ec][:, 0:127]
                    kfr, kfi = ('sE', 0, ec), ('sE', 1, ec)
                    for j in range(16):
                        ar, ai, nai = pj[:, j, 0, gp_:gp_ + 1], pj[:, j, 1, gp_:gp_ + 1], pj[:, j, 2, gp_:gp_ + 1]
                        xr, xi = XRv[:, 1:, j], XIv[:, 1:, j]
                        S.op('dve', lambda e, xr=xr, ar=ar: e.scalar_tensor_tensor(out=xr, in0=FR, scalar=ar, in1=xr, op0=ALU.mult, op1=ALU.add),
                             [kfr, 'spj', kxr], [kxr])
                        S.op('dve', lambda e, xr=xr, nai=nai: e.scalar_tensor_tensor(out=xr, in0=FI, scalar=nai, in1=xr, op0=ALU.mult, op1=ALU.add),
                             [kfi, 'spj', kxr], [kxr])
                        S.op('dve', lambda e, xi=xi, ar=ar: e.scalar_tensor_tensor(out=xi, in0=FI, scalar=ar, in1=xi, op0=ALU.mult, op1=ALU.add),
                             [kfi, 'spj', kxi], [kxi])
                        S.op('dve', lambda e, xi=xi, ai=ai: e.scalar_tensor_tensor(out=xi, in0=FR, scalar=ai, in1=xi, op0=ALU.mult, op1=ALU.add),
                             [kfr, 'spj', kxi], [kxi])
                    for tb in range(4):
                        sl = slice(tb * 512, (tb + 1) * 512)
                        self.mm(PS[6][:, :], TB[:, 2, g4, :], XR[cur][:, sl], True, False, [tk, kxr], [pk], last=False)
                        self.mm(PS[6][:, :], TB[:, 3, g4, :], XI[cur][:, sl], False, True, [tk, kxi], [pk])
                        if g4 == 0:
                            self.act(Yacc[:, sl], PS[6][:, :], AF.Copy, [pk], ['sYacc'])
                        else:
                            S.op('dve', lambda e, sl=sl: e.tensor_tensor(Yacc[:, sl], Yacc[:, sl], PS[6][:, :], ALU.add),
                                 [pk, 'sYacc'], ['sYacc'])
                    yield
                S.op('dve', lambda e, ct=ct: e.scalar_tensor_tensor(out=Yacc[:], in0=ut[:], scalar=d8[:, ct:ct + 1], in1=Yacc[:],
                                                                    op0=ALU.mult, op1=ALU.add), ['sut', 'sd8', 'sYacc'], ['sYacc'])
                self.act(yg[:], Yacc[:], AF.Gelu_apprx_tanh, ['sYacc'], ['syg'])
                S.dma('act', self.ycs[ct][:, b * L:(b + 1) * L], yg[:], reads=['syg'], writes=['ycs'], sem='syg')

    def phase_mix(self, l):
        S = self.S
        with ExitStack() as es_s5:
            st = self.s5_setup(l, es_s5)
            g_s5 = self.s5_main(l, st)
            done, total = 0, 96
            for fn in (self.mlstm_gen, self.attn_gen):
                with ExitStack() as es2:
                    for _ in fn(l, es2):
                        done += 1
                        while st.setdefault('steps', 0) < (done * 64) // total:
                            st['steps'] += 1
                            next(g_s5, None)
                    S.barrier()
            for _ in g_s5:
                pass
            S.barrier()
        self.s5_glu(l)

    def s5_glu(self, l):
        nc, S = self.nc, self.S
        PS = self.PS
        with ExitStack() as es:
            sb = lambda n, s, d: self.sb(es, n, s, d)
            wg = sb('gwg', [128, 16, 8, 128], BF16)
            yc = [sb('gyc%d' % i, [128, 8, 512], BF16) for i in range(2)]
            sg = [sb('gsg%d' % i, [128, 512], F32) for i in range(2)]
            st = [sb('gst%d' % i, [128, 512], BF16) for i in range(2)]
            for q in range(4):
                S.dma('sp', wg[:, 4 * q:4 * q + 4], self.WF_glu[4 * q:4 * q + 4].rearrange("n p (kt c) -> p n kt c", kt=8),
                      reads=['wscr'], writes=['gwg'], sem='c')
            n = 0
            for tb in range(8):
                i = tb % 2
                S.dma('sp', yc[i][:], self.ycs[0:8, :, tb * 512:(tb + 1) * 512].rearrange("n p t -> p n t"),
                      reads=['ycs'], writes=[('gyc', i)], sem='gyc%d' % i)
                for ft in range(8):
                    for kt in range(8):
                        self.mm(PS[0 + (ft % 2) * 2][:, :], wg[:, ft, kt, :], yc[i][:, kt, :], kt == 0, kt == 7,
                                ['gwg', ('gyc', i)], [('ps', (ft % 2) * 2)])
                    for kt in range(8):
                        self.mm(PS[1 + (ft % 2) * 2][:, :], wg[:, 8 + ft, kt, :], yc[i][:, kt, :], kt == 0, kt == 7,
                                ['gwg', ('gyc', i)], [('ps', 1 + (ft % 2) * 2)])
                    j = n % 2
                    n += 1
                    self.act(sg[j][:], PS[1 + (ft % 2) * 2][:, :], AF.Sigmoid, [('ps', 1 + (ft % 2) * 2)], [('gsg', j)])
                    S.op('dve', lambda e, j=j, ft=ft: e.tensor_tensor(st[j][:], sg[j][:], PS[(ft % 2) * 2][:, :], ALU.mult),
                         [('gsg', j), ('ps', (ft % 2) * 2)], [('gst', j)])
                    S.dma('act', self.yT[16 + ft][:, tb * 512:(tb + 1) * 512], st[j][:], reads=[('gst', j)], writes=['yT'],
                          sem='gst%d' % j)
            S.barrier()

    def phase_merge_ffn(self, l, xin, xout):
        nc, S = self.nc, self.S
        PS = self.PS
        with ExitStack() as es:
            sb = lambda n, s, d: self.sb(es, n, s, d)
            H2 = sb('fH2', [128, 88, 2], F32)
            fc = sb('ffc', [128, 88, 3], F32)
            fb = sb('ffb', [128, 88], F32)
            S.dma('sp', fc[:], self.fconv[l], writes=['ffc'], sem='c')
            S.dma('sp', fb[:], self.fbias[l], writes=['ffb'], sem='c')
            h2T = sb('fh2T', [128, 16, 512], BF16)
            for tb in range(8):
                t0 = tb * 512
                with ExitStack() as e2:
                    sb2 = lambda n, s, d: self.sb(e2, n, s, d)
                    gpo = sb2('fgpo', [128, D], F32)
                    S.dma('sp', gpo[:], self.gpost[l].partition_broadcast(128), writes=['fgpo'], sem='c')
                    yTb = sb2('fyT', [128, 24, 512], BF16)
                    mT = sb2('fmT', [128, 16, 512], BF16)
                    gt = [sb2('fgt%d' % i, [128, 3, 512], BF16) for i in range(2)]
                    wb = [sb2('fwb%d' % i, [128, 3, 8, 128], BF16) for i in range(2)]
                    wo = [sb2('fwo%d' % i, [128, 16, 512], BF16) for i in range(2)]
                    m1 = [sb2('fm1%d' % i, [128, 512], F32) for i in range(2)]
                    m2 = [sb2('fm2%d' % i, [128, 512], F32) for i in range(2)]
                    xt = [sb2('fxt%d' % i, [128, D], F32) for i in range(2)]
                    xo = [sb2('fxo%d' % i, [128, D], F32) for i in range(2)]
                    xn = [sb2('fxn%d' % i, [128, D], BF16) for i in range(2)]
                    ss = [sb2('fss%d' % i, [128, 8], F32) for i in range(2)]
                    for q in range(6):
                        S.dma('sp', yTb[:, 4 * q:4 * q + 4, :], self.yT[4 * q:4 * q + 4, :, t0:t0 + 512].rearrange("n p t -> p n t"),
                              reads=['yT'], writes=[('fyT', q)], sem='fyT')
                    yk = [('fyT', q) for q in range(6)]
                    for ft in range(16):
                        i = ft % 2
                        S.dma('sp', wb[i][:], self.WF_br[ft:48:16].rearrange("n p (kt c) -> p n kt c", kt=8),
                              reads=['wscr'], writes=[('fwb', i)], sem='fwb%d' % i)
                        S.dma('sp', gt[i][:], self.gT[ft:48:16, :, t0:t0 + 512].rearrange("n p t -> p n t"),
                              reads=['gT'], writes=[('fgt', i)], sem='fgt%d' % i)
                        for n in range(3):
                            for kt in range(8):
                                self.mm(PS[n + 3 * i][:, :], wb[i][:, n, kt, :], yTb[:, n * 8 + kt, :], kt == 0, kt == 7,
                                        [('fwb', i)] + yk, [('ps', n + 3 * i)])
                        S.op('dve', lambda e, i=i: e.tensor_tensor(m1[i][:], gt[i][:, 0, :], PS[0 + 3 * i][:, :], ALU.mult),
                             [('fgt', i), ('ps', 3 * i)], [('fm1', i)])
                        S.op('dve', lambda e, i=i: e.tensor_tensor(m2[i][:], gt[i][:, 1, :], PS[1 + 3 * i][:, :], ALU.mult),
                             [('fgt', i), ('ps', 1 + 3 * i)], [('fm2', i)])
                        S.op('pool', lambda e, i=i: e.tensor_tensor(m1[i][:], m1[i][:], m2[i][:], ALU.add),
                             [('fm1', i), ('fm2', i)], [('fm1', i)])
                        S.op('dve', lambda e, i=i: e.tensor_tensor(m2[i][:], gt[i][:, 2, :], PS[2 + 3 * i][:, :], ALU.mult),
                             [('fgt', i), ('ps', 2 + 3 * i)], [('fm2', i)])
                        S.op('pool', lambda e, i=i, ft=ft: e.tensor_tensor(mT[:, ft, :], m1[i][:], m2[i][:], ALU.add),
                             [('fm1', i), ('fm2', i)], ['fmT'])
                    for cb in range(4):
                        S.dma('sp', wo[cb % 2][:, 0:8, :], self.WT_out[cb][:, 0:8 * 512].rearrange("p (kt c) -> p kt c", kt=8),
                              reads=['wscr'], writes=[('fwo', cb % 2, 0)], sem='fwo%d' % (cb % 2))
                        S.dma('sp', wo[cb % 2][:, 8:16, :], self.WT_out[cb][:, 8 * 512:16 * 512].rearrange("p (kt c) -> p kt c", kt=8),
                              reads=['wscr'], writes=[('fwo', cb % 2, 1)], sem='fwo%d' % (cb % 2))
                        wk = [('fwo', cb % 2, 0), ('fwo', cb % 2, 1)]
                        for tt in range(4):
                            bank = (cb * 4 + tt) % 2 + 6
                            for kt in range(16):
                                self.mm(PS[bank][:, :], mT[:, kt, tt * 128:(tt + 1) * 128], wo[cb % 2][:, kt, :], kt == 0, kt == 15,
                                        ['fmT'] + wk, [('ps', bank)])
                            i = (cb * 4 + tt) % 2
                            self.act(m1[i][:], PS[bank][:, :], AF.Copy, [('ps', bank)], [('fm1', i)])
                            S.dma('act', self.x1[t0 + tt * 128:t0 + (tt + 1) * 128, cb * 512:(cb + 1) * 512], m1[i][:],
                                  reads=[('fm1', i)], writes=[('x1', tt)], sem='fm1%d' % i)
                    for tt in range(4):
                        i = tt % 2
                        rows = slice(t0 + tt * 128, t0 + (tt + 1) * 128)
                        S.dma('sp', xo[i][:], self.x1[rows, :], reads=[('x1', tt)], writes=[('fxo', i)], sem='fxo%d' % i)
                        S.dma('sp', xt[i][:], xin[rows, :], reads=['xin'], writes=[('fxt', i)], sem='fxt%d' % i)
                        S.op('pool', lambda e, i=i: e.memset(ss[i][:, 0:1], 0.0), [], [('fss', i)])
                        self.act(xn[i][:], xo[i][:], AF.Square, [('fxo', i)], [('fxn', i), ('fss', i)], accum_out=ss[i][:, 0:1])
                        self.rstd(ss[i][:, 0:1], D, ('fss', i))
                        S.op('dve', lambda e, i=i: e.scalar_tensor_tensor(out=xo[i][:], in0=xo[i][:], scalar=ss[i][:, 0:1], in1=gpo[:],
                                                                          op0=ALU.mult, op1=ALU.mult),
                             [('fxo', i), ('fss', i), 'fgpo'], [('fxo', i)])
                        S.op('pool', lambda e, i=i: e.tensor_tensor(xt[i][:], xt[i][:], xo[i][:], ALU.add),
                             [('fxt', i), ('fxo', i)], [('fxt', i)])
                        S.dma('act', self.x1[rows, :], xt[i][:], reads=[('fxt', i)], writes=[('x1', tt)], sem='fxt%d' % i)
                        self.norm_T(xt[i][:], ('fxt', i), xn[i][:], ('fxn', i), ss[i][:, 1:2], ('fss2', i), h2T, 'fh2T', tt, 0, 1)
                    S.barrier()
                with ExitStack() as e2:
                    sb2 = lambda n, s, d: self.sb(e2, n, s, d)
                    gfo = sb2('fgfo', [128, D], F32)
                    S.dma('sp', gfo[:], self.gfpost[l].partition_broadcast(128), writes=['fgfo'], sem='c')
                    aT = sb2('faT', [128, 44, 512], BF16)
                    wf = [sb2('fwf%d' % i, [128, 16, 128], BF16) for i in range(4)]
                    wd = [sb2('fwd%d' % i, [128, 11, 512], BF16) for i in range(3)]
                    Ub = [sb2('fU%d' % i, [128, 514], F32) for i in range(2)]
                    ca = [sb2('fca%d' % i, [128, 512], F32) for i in range(2)]
                    cv = [sb2('fcv%d' % i, [128, 512], F32) for i in range(2)]
                    xo = [sb2('gxo%d' % i, [128, D], F32) for i in range(4)]
                    xt = [sb2('gxt%d' % i, [128, D], F32) for i in range(1)]
                    jk = sb2('gjk', [128, D], BF16)
                    ss = [sb2('gss%d' % i, [128, 4], F32) for i in range(2)]
                    if tb % 4 == 0:
                        S.op('pool', lambda e: e.memset(H2[:], 0.0), [], ['fH2'])
                    r_wf, r_U = Ring('wf', 4), Ring('U', 2)
                    for jp in range(44):
                        res = []
                        for half in range(2):
                            nt = jp + 44 * half
                            iw = r_wf.next()
                            S.dma('sp', wf[iw][:], self.WF_up[nt].rearrange("p (kt c) -> p kt c", kt=16),
                                  reads=['wscr'], writes=[('fwf', iw)], sem='fwf%d' % iw)
                            bank = (jp % 2) * 2 + half
                            pk = ('ps', bank)
                            for kt in range(16):
                                self.mm(PS[bank][:, :], wf[iw][:, kt, :], h2T[:, kt, :], kt == 0, kt == 15, [('fwf', iw), 'fh2T'], [pk])
                            iu = r_U.next()
                            Ut, uk = Ub[iu], ('fU', iu)
                            self.act(Ut[:, 2:514], PS[bank][:, :], AF.Copy, [pk], [uk])
                            S.op('pool', lambda e, Ut=Ut, nt=nt: e.tensor_copy(Ut[:, 0:2], H2[:, nt, :]), ['fH2'], [uk])
                            S.op('pool', lambda e, Ut=Ut, nt=nt: e.tensor_copy(H2[:, nt, :], Ut[:, 512:514]), [uk], ['fH2'])
                            o = (ca if half == 0 else cv)[jp % 2]
                            ok = ('fc', half, jp % 2)
                            S.op('dve', lambda e, Ut=Ut, o=o, nt=nt: e.tensor_scalar(o[:], Ut[:, 2:514], fc[:, nt, 2:3], fb[:, nt:nt + 1],
                                                                                     ALU.mult, ALU.add), [uk, 'ffc', 'ffb'], [ok])
                            for j in (1, 0):
                                S.op('dve', lambda e, Ut=Ut, o=o, nt=nt, j=j: e.scalar_tensor_tensor(
                                    out=o[:], in0=Ut[:, j:j + 512], scalar=fc[:, nt, j:j + 1], in1=o[:], op0=ALU.mult, op1=ALU.add),
                                    [uk, 'ffc', ok], [ok])
                            res.append((o, ok))
                        (oa, ka), (ov, kv) = res
                        self.act(oa[:], oa[:], AF.Gelu_apprx_tanh, [ka], [ka])
                        S.op('pool', lambda e, oa=oa, ov=ov, jp=jp: e.tensor_tensor(aT[:, jp, :], oa[:], ov[:], ALU.mult), [ka, kv], ['faT'])
                    r_wd = Ring('wd', 3)
                    for cb in range(4):
                        for kp in range(4):
                            iw = r_wd.next()
                            S.dma('sp', wd[iw][:], self.WT_dn[cb * 4 + kp].rearrange("p (kt c) -> p kt c", kt=11),
                                  reads=['wscr'], writes=[('fwd', iw)], sem='fwd%d' % iw)
                            for tt in range(4):
                                for kt in range(11):
                                    self.mm(PS[4 + tt][:, :], aT[:, kp * 11 + kt, tt * 128:(tt + 1) * 128], wd[iw][:, kt, :],
                                            kp == 0 and kt == 0, kp == 3 and kt == 10, ['faT', ('fwd', iw)], [('ps', 4 + tt)],
                                            last=(kt == 10))
                        for tt in range(4):
                            if (cb + tt) % 2:
                                self.act(xo[tt][:, cb * 512:(cb + 1) * 512], PS[4 + tt][:, :], AF.Copy, [('ps', 4 + tt)], [('gxo', tt)])
                            else:
                                S.op('dve', lambda e, tt=tt, cb=cb: e.tensor_copy(xo[tt][:, cb * 512:(cb + 1) * 512], PS[4 + tt][:, :]),
                                     [('ps', 4 + tt)], [('gxo', tt)])
                    for tt in range(4):
                        i = tt
                        rows = slice(t0 + tt * 128, t0 + (tt + 1) * 128)
                        S.dma('sp', xt[0][:], self.x1[rows, :], reads=[('x1', tt)], writes=[('gxt', 0)], sem='gxt0')
                        S.op('pool', lambda e, i=i: e.memset(ss[0][:, 0:1], 0.0), [], [('gss', 0)])
                        self.act(jk[:], xo[i][:], AF.Square, [('gxo', i)], ['gjk', ('gss', 0)], accum_out=ss[0][:, 0:1])
                        self.rstd(ss[0][:, 0:1], D, ('gss', 0))
                        S.op('dve', lambda e, i=i: e.scalar_tensor_tensor(out=xo[i][:], in0=xo[i][:], scalar=ss[0][:, 0:1], in1=gfo[:],
                                                                          op0=ALU.mult, op1=ALU.mult),
                             [('gxo', i), ('gss', 0), 'fgfo'], [('gxo', i)])
                        S.op('pool', lambda e, i=i: e.tensor_tensor(xo[i][:], xo[i][:], xt[0][:], ALU.add),
                             [('gxo', i), ('gxt', 0)], [('gxo', i)])
                        S.dma('act', xout[rows, :], xo[i][:], reads=[('gxo', i)], writes=['xout'], sem='gxo%d' % i)
                    S.barrier()
            S.barrier()


def _bucket_table():
    dist = np.arange(0, 4096, dtype=np.int64)
    n = np.maximum(dist, 0)
    nf = np.maximum(n, 1).astype(np.float32)
    large = 16 + (np.log(nf / np.float32(16)) / np.float32(math.log(8.0)) * np.float32(16)).astype(np.int32)
    large = np.minimum(large, 31)
    return np.where(n < 16, n, large).astype(np.int64)


def host_layout(inp):
    f32 = np.float32
    out = {}
    cst = np.zeros((128, 512), f32)
    cst[:, 0:128] = np.eye(128, dtype=f32)
    cst[:, 128:256] = np.triu(np.ones((128, 128), f32))
    cst[:, 256:384] = np.where(np.arange(128)[:, None] > np.arange(128)[None, :], f32(-30000.0), f32(0.0))
    cst[:, 384:512] = 1.0
    out['cst'] = cst
    g = lambda k: np.asarray(inp[k], f32)
    out['gpre'] = np.ascontiguousarray(g('norm_mix_pre').reshape(2, 16, 128).transpose(0, 2, 1))
    out['gffn'] = np.ascontiguousarray(g('norm_ffn_pre').reshape(2, 16, 128).transpose(0, 2, 1))
    out['gpost'] = g('norm_mix_post').reshape(2, 1, D)
    out['gfpost'] = g('norm_ffn_post').reshape(2, 1, D)
    out['mconv'] = np.ascontiguousarray(g('mlstm_conv').reshape(2, 4, 16, 128).transpose(0, 3, 2, 1))
    out['mnorm'] = g('mlstm_norm').reshape(2, 1, 1024)
    out['dnorm'] = g('diff_norm').reshape(2, 1, 1024)
    out['bif'] = g('mlstm_b_if').reshape(2, 1, 8)
    out['lamT'] = np.ascontiguousarray(g('diff_lambda').transpose(0, 2, 1))
    rb = g('rel_bias')
    bt = _bucket_table()
    kl = np.arange(128)[:, None]
    xx = np.arange(1024)[None, :] - 384
    dist = xx - kl
    wide = np.empty((128, 4, 1024), f32)
    for h in range(4):
        vals = rb[bt[np.clip(dist, 0, 4095)], h]
        wide[:, h, :] = np.where(dist >= 0, vals, f32(-30000.0))
    out['wide'] = wide
    out['cfar'] = np.ascontiguousarray(np.broadcast_to(rb[31][None, :], (128, 4)))
    out['fconv'] = np.ascontiguousarray(g('ffn_conv').reshape(2, 3, 88, 128).transpose(0, 3, 2, 1))
    out['fbias'] = np.ascontiguousarray(g('ffn_conv_b').reshape(2, 88, 128).transpose(0, 2, 1))
    out['s5d'] = np.ascontiguousarray(g('s5_d').reshape(2, 8, 128).transpose(0, 2, 1))
    lre, lim, ldt = g('s5_lambda_re'), g('s5_lambda_im'), g('s5_log_dt')
    ldt_full = np.broadcast_to(ldt[:, :, None], (2, 64, 64))
    s5p = np.empty((2, 3, 128, 32), f32)
    s5r = np.empty((2, 3, 128, 32 * 128), f32)
    for i, a in enumerate((lre, lim, ldt_full)):
        a4 = a.reshape(2, 32, 2, 64)
        s5p[:, i] = a4.transpose(0, 2, 3, 1).reshape(2, 128, 32)
        s5r[:, i] = np.broadcast_to(a4.reshape(2, 1, 32 * 128), (2, 128, 32 * 128))
    out['s5p'], out['s5r'] = s5p, s5r
    bT = np.zeros((2, 2, 128, 32, 128), f32)
    cP = np.zeros((2, 2, 128, 32, 128), f32)
    for i, (bk, ck) in enumerate((('s5_b_re', 's5_c_re'), ('s5_b_im', 's5_c_im'))):
        bb = g(bk)
        cc = g(ck)
        for gi in range(64):
            gp_, gg = gi // 2, gi % 2
            r0 = (gi % 8) * 16
            bT[:, i, r0:r0 + 16, gp_, gg * 64:(gg + 1) * 64] = bb[:, gi].transpose(0, 2, 1)
            cP[:, i, gg * 64:(gg + 1) * 64, gp_, r0:r0 + 16] = cc[:, gi].transpose(0, 2, 1)
    out['s5bT'] = bT.reshape(2, 2, 128, 32 * 128)
    out['s5cP'] = cP.reshape(2, 2, 128, 32 * 128)
    for k in ('w_in', 'w_branch', 's5_w_glu', 'w_out', 'w_up', 'w_down'):
        out[k] = np.ascontiguousarray(g(k))
    return out


_PROG = {}
_PER_LAYER = ('gpre', 'gffn', 'gpost', 'gfpost', 'mconv', 'mnorm', 'dnorm', 'bif', 'lamT', 'fconv', 'fbias', 's5d',
              's5p', 's5r', 's5bT', 's5cP', 'w_in', 'w_branch', 's5_w_glu', 'w_out', 'w_up', 'w_down')


def _prog(kind):
    if kind not in _PROG:
        _PROG[kind] = Prog(kind)
    return _PROG[kind]


def _launch(kind, per_core, shared):
    prog = _prog(kind)
    in_maps = []
    for c in range(NCORE):
        m = {}
        for k in prog.inp:
            m[k] = per_core[c][k] if k in per_core[c] else shared[k]
        in_maps.append(m)
    res = run_bass_kernel_spmd(prog.nc, in_maps, core_ids=list(range(NCORE)))
    return [{k: np.asarray(res.results[c][k]) for k in prog.outs} for c in range(NCORE)]


def kernel(**inputs):
    lay = host_layout(inputs)
    lami = np.empty((2, 128, 2), np.float32)
    for l in range(2):
        li = 0.8 - 0.6 * math.exp(-0.3 * l)
        lami[l, :, 0] = 1.0 - li
        lami[l, :, 1] = -li
    lay['lami'] = lami
    x = np.asarray(inputs['x'], np.float32)
    prog = _prog('F')
    in_maps = []
    for c in range(NCORE):
        m = {k: lay[k] for k in prog.inp if k != 'x'}
        m['x'] = np.ascontiguousarray(x[NB * c:NB * (c + 1)].reshape(T, D))
        in_maps.append(m)
    res = run_bass_kernel_spmd(prog.nc, in_maps, core_ids=list(range(NCORE)))
    out = np.empty((16, L, D), np.float32)
    for c in range(NCORE):
        out[NB * c:NB * (c + 1)] = np.asarray(res.results[c]['y'], np.float32).reshape(NB, L, D)
    return out
```

```python
import math
from contextlib import ExitStack
import numpy as np
import concourse.bass as bass
import concourse.mybir as mybir
from concourse.bass_utils import run_bass_kernel_spmd

F32 = mybir.dt.float32
BF16 = mybir.dt.bfloat16
AF = mybir.ActivationFunctionType
ALU = mybir.AluOpType

NCORE = 8
DBG_X1 = False
L = 2048
NB = 2
T = NB * L
D = 2048
NIN = 14344
DFF = 5632
EPS = 1e-6
O_QM, O_KM, O_VM, O_OM, O_I, O_F, O_QD, O_KD, O_VD, O_US, O_G = (
    0, 1024, 2048, 3072, 4096, 4100, 4104, 5128, 6152, 7176, 8200)


class Sync:
    def __init__(self, nc):
        self.nc = nc
        self.eng = dict(pe=nc.tensor, act=nc.scalar, dve=nc.vector, pool=nc.gpsimd, sp=nc.sync)
        self.sems, self.tot, self.isdma = {}, {}, {}
        for e in self.eng:
            self._mk('e_' + e, False)
        self.seen = {e: {} for e in self.eng}
        self.st = {}
        self.pe_pending = []
        self.n_ins = 0

    def _mk(self, sid, isdma):
        if sid not in self.sems:
            self.sems[sid] = self.nc.semaphore(sid).__enter__()
            self.tot[sid] = 0
            self.isdma[sid] = isdma
        return self.sems[sid]

    def _deps(self, reads, writes):
        need = {}
        for k in reads:
            s = self.st.get(k)
            if s:
                for sid, v in s[0].items():
                    if need.get(sid, 0) < v:
                        need[sid] = v
        for k in writes:
            s = self.st.get(k)
            if s:
                for d in s:
                    for sid, v in d.items():
                        if need.get(sid, 0) < v:
                            need[sid] = v
        return need

    def _wait(self, e, need):
        for sid, v in need.items():
            if e == 'pe' and sid == 'e_pe':
                continue
            if self.isdma[sid]:
                v = self.tot[sid]
            if self.seen[e].get(sid, 0) >= v:
                continue
            self.eng[e].wait_ge(self.sems[sid], v)
            self.seen[e][sid] = v
            self.n_ins += 1

    def _record(self, reads, writes, sid, v):
        for k in writes:
            self.st[k] = [{sid: v}, {}]
        for k in reads:
            s = self.st.setdefault(k, [{}, {}])
            if s[1].get(sid, 0) < v:
                s[1][sid] = v

    def op(self, e, fn, reads=(), writes=(), last=True):
        self._wait(e, self._deps(reads, writes))
        ins = fn(self.eng[e])
        self.n_ins += 1
        sid = 'e_' + e
        if e == 'pe' and not last:
            self.pe_pending.append((tuple(reads), tuple(writes)))
            return ins
        self.tot[sid] += 1
        ins.then_inc(self.sems[sid], 1)
        v = self.tot[sid]
        if e == 'pe':
            for r, w in self.pe_pending:
                self._record(r, w, sid, v)
            self.pe_pending = []
        self._record(reads, writes, sid, v)
        return ins

    def dma(self, e, out, in_, reads=(), writes=(), sem='d0', **kw):
        self._wait(e, self._deps(reads, writes))
        sid = 'd_' + sem
        S = self._mk(sid, True)
        self.tot[sid] += 16
        self.eng[e].dma_start(out=out, in_=in_, **kw).then_inc(S, 16)
        self.n_ins += 1
        self._record(reads, writes, sid, self.tot[sid])

    def barrier(self):
        assert not self.pe_pending
        for e in self.eng:
            for sid in self.sems:
                v = self.tot[sid]
                if v == 0 or self.seen[e].get(sid, 0) >= v:
                    continue
                self.eng[e].wait_ge(self.sems[sid], v)
                self.seen[e][sid] = v
                self.n_ins += 1
        self.st = {}


class Ring:
    def __init__(self, name, n):
        self.name, self.n, self.i = name, n, -1

    def next(self):
        self.i += 1
        return self.i % self.n


class L1:
    def __init__(self, ap):
        self.ap = ap

    def __getitem__(self, k):
        if isinstance(k, tuple):
            return self.ap[k[1]] if len(k) == 2 else self.ap[k[1:]]
        return self.ap


class Prog:
    def __init__(self, kind):
        self.kind = kind
        nc = self.nc = bass.Bass("TRN2", target_bir_lowering=False)
        self.S = Sync(nc)
        self.inp = {}
        self.outs = []
        self.build()

    def din(self, name, shape, dt=F32):
        self.inp[name] = (shape, dt)
        return self.nc.dram_tensor(name, list(shape), dt, kind="ExternalInput").ap()

    def dscr(self, name, shape, dt, io=None):
        if io == 'in':
            return self.din(name, shape, dt)
        if io == 'out':
            self.outs.append(name)
        kind = "ExternalOutput" if io == 'out' else "Internal"
        return self.nc.dram_tensor(name, list(shape), dt, kind=kind).ap()

    def sb(self, es, name, shape, dt):
        self._uid = getattr(self, '_uid', 0) + 1
        return es.enter_context(self.nc.sbuf_tensor('%s_%d' % (name, self._uid), list(shape), dt))

    def act(self, out, in_, func, reads, writes, **kw):
        self.S.op('act', lambda e: e.activation(out=out, in_=in_, func=func, **kw), reads, writes)

    def mm(self, out, lhsT, rhs, start, stop, reads, writes, last=None):
        if last is None:
            last = stop
        self.S.op('pe', lambda e: e.matmul(out, lhsT, rhs, start=start, stop=stop), reads, writes, last=last)

    def rstd(self, ss, n, key):
        S = self.S
        S.op('dve', lambda e: e.tensor_scalar(ss, ss, 1.0 / n, EPS, ALU.mult, ALU.add), [key], [key])
        self.act(ss, ss, AF.Ln, [key], [key])
        self.act(ss, ss, AF.Exp, [key], [key], scale=-0.5)

    def build(self):
        nc, S = self.nc, self.S
        kd = self.kind
        fused = kd == 'F'
        din = (lambda n, sh, dt=F32: self.din(n, [2] + list(sh), dt)) if fused else (lambda n, sh, dt=F32: L1(self.din(n, sh, dt)))
        scr = self.dscr
        self.cst = self.din('cst', [128, 512])
        if kd in 'AF':
            self.x_in = self.din('x', [T, D])
            self.w_in = din('w_in', [D, NIN])
            self.gpre = din('gpre', [128, 16])
            self.mconv = din('mconv', [128, 16, 4])
            self.bif = din('bif', [1, 8])
            self.WF_in = scr('WF_in', [88, 128, 16 * 128], BF16)
            self.WT_in = scr('WT_in', [6, 128, 16 * 512], BF16)
        if kd in 'BF':
            self.w_glu = din('s5_w_glu', [1024, 2048])
            self.mnorm = din('mnorm', [1, 1024])
            self.dnorm = din('dnorm', [1, 1024])
            self.lamT = din('lamT', [128, 4])
            self.lami = din('lami', [128, 2])
            self.wide = self.din('wide', [128, 4, 1024])
            self.cfar = self.din('cfar', [128, 4])
            self.s5d = din('s5d', [128, 8])
            self.s5p = din('s5p', [3, 128, 32])
            self.s5r = din('s5r', [3, 128, 32 * 128])
            self.s5bT = din('s5bT', [2, 128, 32 * 128])
            self.s5cP = din('s5cP', [2, 128, 32 * 128])
            self.WF_glu = scr('WF_glu', [16, 128, 8 * 128], BF16)
            self.ycs = scr('ycs', [8, 128, T], BF16)
            self.s5tab = scr('s5tab', [4, 128, 32 * 128], F32)
        if kd in 'CF':
            if not fused:
                self.x_in = self.din('x', [T, D])
            self.y_out = scr('y', [T, D], F32, 'out')
            self.xmid = scr('xmid', [T, D], F32)
            self.w_branch = din('w_branch', [3, 1024, D])
            self.w_out = din('w_out', [D, D])
            self.w_up = din('w_up', [D, 2 * DFF])
            self.w_down = din('w_down', [DFF, D])
            self.gffn = din('gffn', [128, 16])
            self.gpost = din('gpost', [1, D])
            self.gfpost = din('gfpost', [1, D])
            self.fconv = din('fconv', [128, 88, 3])
            self.fbias = din('fbias', [128, 88])
            self.WF_br = scr('WF_br', [48, 128, 8 * 128], BF16)
            self.WT_out = scr('WT_out', [4, 128, 16 * 512], BF16)
            self.WF_up = scr('WF_up', [88, 128, 16 * 128], BF16)
            self.WT_dn = scr('WT_dn', [16, 128, 11 * 512], BF16)
            self.x1 = scr('x1', [T, D], F32, 'out' if DBG_X1 else None)
        ab = {'A': 'out', 'B': 'in'}.get(kd)
        if kd in 'ABF':
            self.qkm = scr('qkm', [16, 128, T], BF16, ab)
            self.qkd = scr('qkd', [16, 128, T], BF16, ab)
            self.uT = scr('uT', [8, 128, T], F32, ab)
            self.vm = scr('vm', [T, 1024], BF16, ab)
            self.om = scr('om', [T, 1024], BF16, ab)
            self.vd = scr('vd', [T, 1024], BF16, ab)
            self.lif = scr('lif', [T, 8], F32, ab)
        if kd in 'ACF':
            self.gT = scr('gT', [48, 128, T], BF16, {'A': 'out', 'C': 'in'}.get(kd))
        if kd in 'BCF':
            self.yT = scr('yT', [24, 128, T], BF16, {'B': 'out', 'C': 'in'}.get(kd))

        self.C = nc.alloc_sbuf_tensor('cst_sb', [128, 512], F32)
        self.IDB = nc.alloc_sbuf_tensor('idb', [128, 128], BF16)
        S.dma('sp', self.C[:], self.cst[:, :], writes=['cst'], sem='c')
        S.op('dve', lambda e: e.tensor_copy(self.IDB[:], self.C[:, 0:128]), ['cst'], ['idb'])
        self.ID32 = self.C[:, 0:128]
        self.U = self.C[:, 128:256]
        self.NEGM = self.C[:, 256:384]
        self.ONES = self.C[:, 384:512]
        self.PS = [nc.alloc_psum_tensor('ps%d' % i, [128, 512], F32) for i in range(8)]
        S.barrier()
        for l in range(2 if fused else 1):
            xin = getattr(self, 'x_in', None) if l == 0 else self.xmid
            xout = getattr(self, 'y_out', None) if (l == 1 or not fused) else self.xmid
            self.phase_prep(l)
            if kd in 'AF':
                self.phase_inproj(l, xin)
            if kd in 'BF':
                self.phase_mix(l)
            if kd in 'CF':
                self.phase_merge_ffn(l, xin, xout)
        S.barrier()

    def phase_prep(self, l):
        nc, S = self.nc, self.S
        with ExitStack() as es:
            s32 = [self.sb(es, 'p32_%d' % i, [128, 16, 512], F32) for i in range(2)]
            s16 = [self.sb(es, 'p16_%d' % i, [128, 16 * 512], BF16) for i in range(2)]
            gp = self.sb(es, 'pgp', [128, 16], F32)
            gf = self.sb(es, 'pgf', [128, 16], F32)
            if self.kind in 'AF':
                S.dma('sp', gp[:], self.gpre[l], writes=['pgp'], sem='c')
            if self.kind in 'CF':
                S.dma('sp', gf[:], self.gffn[l], writes=['pgf'], sem='c')
            ring = Ring('p', 2)
            cnt = [0]

            def block(W2d, c0, kt0, KT, gain, gkey, form, dst):
                i = ring.next()
                a, b = s32[i], s16[i]
                for q in range(0, KT, 4):
                    n = min(4, KT - q)
                    src = W2d[(kt0 + q) * 128:(kt0 + q + n) * 128, c0:c0 + 512].rearrange(
                        "(kt p) c -> p kt c", p=128)
                    S.dma('sp', a[:, q:q + n, :], src, writes=[('p32', i, q // 4)], sem='p32_%d' % i)
                r32 = [('p32', i, q) for q in range((KT + 3) // 4)]
                if form == 'F':
                    ov = b[:, 0:4 * KT * 128].rearrange("p (nt kt c) -> p kt nt c", nt=4, kt=KT)
                    iv = a[:, 0:KT, :].rearrange("p kt (nt c) -> p kt nt c", c=128)
                else:
                    ov = b[:, 0:KT * 512].rearrange("p (kt c) -> p kt c", kt=KT)
                    iv = a[:, 0:KT, :]
                if gain is not None:
                    for kt in range(KT):
                        cnt[0] += 1
                        if cnt[0] % 2:
                            self.act(ov[:, kt], iv[:, kt], AF.Copy, r32 + [gkey], [('p16', i)],
                                     scale=gain[:, kt0 + kt:kt0 + kt + 1])
                        else:
                            S.op('dve', lambda e, kt=kt: e.tensor_scalar(
                                ov[:, kt], iv[:, kt], gain[:, kt0 + kt:kt0 + kt + 1], None, ALU.mult),
                                r32 + [gkey], [('p16', i)])
                else:
                    h = KT // 2
                    self.act(ov[:, 0:h], iv[:, 0:h], AF.Copy, r32, [('p16', i)])
                    S.op('dve', lambda e: e.tensor_copy(ov[:, h:KT], iv[:, h:KT]), r32, [('p16', i)])
                if form == 'F':
                    n = dst.shape[0]
                    S.dma('act', dst.rearrange("nt p f -> p nt f"),
                          b[:, 0:n * KT * 128].rearrange("p (nt f) -> p nt f", nt=n),
                          reads=[('p16', i)], writes=['wscr'], sem='p16_%d' % i)
                else:
                    S.dma('act', dst, b[:, 0:KT * 512], reads=[('p16', i)], writes=['wscr'], sem='p16_%d' % i)

            if self.kind in 'AF':
                Win = self.w_in[l]
                fcols = ([O_QM + 512 * j for j in range(4)] + [O_QD + 512 * j for j in range(4)] +
                         [O_US + 512 * j for j in range(2)] + [O_G + 512 * j for j in range(12)])
                for j, c0 in enumerate(fcols):
                    block(Win, c0, 0, 16, gp, 'pgp', 'F', self.WF_in[4 * j:4 * j + 4])
                tcols = [O_VM, O_VM + 512, O_OM, O_OM + 512, O_VD, O_VD + 512]
                for j, c0 in enumerate(tcols):
                    block(Win, c0, 0, 16, gp, 'pgp', 'T', self.WT_in[j])
            if self.kind in 'BF':
                for j in range(4):
                    block(self.w_glu[l], 512 * j, 0, 8, None, None, 'F', self.WF_glu[4 * j:4 * j + 4])
            if self.kind in 'CF':
                for n in range(3):
                    for j in range(4):
                        block(self.w_branch[l, n], 512 * j, 0, 8, None, None, 'F',
                              self.WF_br[n * 16 + 4 * j:n * 16 + 4 * j + 4])
                for j in range(4):
                    block(self.w_out[l], 512 * j, 0, 16, None, None, 'T', self.WT_out[j])
                for j in range(22):
                    block(self.w_up[l], 512 * j, 0, 16, gf, 'pgf', 'F', self.WF_up[4 * j:4 * j + 4])
                for j in range(4):
                    for kp in range(4):
                        block(self.w_down[l], 512 * j, kp * 11, 11, None, None, 'T', self.WT_dn[j * 4 + kp])
            S.barrier()

    def norm_T(self, xt, xkey, xn, xnkey, ss, sskey, hT, hkey, tt, psA, psB):
        S = self.S
        S.op('pool', lambda e: e.memset(ss, 0.0), [], [sskey])
        self.act(xn, xt, AF.Square, [xkey], [xnkey, sskey], accum_out=ss)
        self.rstd(ss, D, sskey)
        S.op('dve', lambda e: e.tensor_scalar(xn, xt, ss, None, ALU.mult), [xkey, sskey], [xnkey])
        for g in range(4):
            ps = psA if g % 2 == 0 else psB
            pk = ('ps', ps)
            for j in range(4):
                kt = g * 4 + j
                self.mm(self.PS[ps][:, j * 128:(j + 1) * 128], xn[:, kt * 128:(kt + 1) * 128], self.IDB[:],
                        True, True, [xnkey, 'idb'], [pk], last=(j == 3))
            dst = hT[:, g * 4:g * 4 + 4, tt * 128:(tt + 1) * 128]
            src = self.PS[ps][:, :].rearrange("p (a b) -> p a b", a=4)
            if g % 2 == 0:
                self.act(dst, src, AF.Copy, [pk], [hkey])
            else:
                S.op('dve', lambda e, dst=dst, src=src: e.tensor_copy(dst, src), [pk], [hkey])

    def phase_inproj(self, l, xin):
        nc, S = self.nc, self.S
        with ExitStack() as es:
            sb = lambda n, s, d: self.sb(es, n, s, d)
            hT = [sb('hT%d' % i, [128, 16, 512], BF16) for i in range(2)]
            xt = [sb('xt%d' % i, [128, D], F32) for i in range(2)]
            xn = [sb('xn%d' % i, [128, D], BF16) for i in range(2)]
            ss = [sb('ss%d' % i, [128, 1], F32) for i in range(2)]
            wf = [sb('wf%d' % i, [128, 16, 128], BF16) for i in range(3)]
            wt = [sb('wt%d' % i, [128, 16, 512], BF16) for i in range(2)]
            stg = [sb('stg%d' % i, [128, 512], BF16) for i in range(4)]
            stf = [sb('stf%d' % i, [128, 512], F32) for i in range(2)]
            Ub = [sb('U%d' % i, [128, 515], F32) for i in range(2)]
            acc = [sb('acc%d' % i, [128, 512], F32) for i in range(2)]
            H = sb('halo', [128, 16, 3], F32)
            wc = sb('wc', [128, 16, 4], F32)
            wif32 = sb('wif32', [128, 16, 8], F32)
            wif = sb('wif', [128, 16, 8], BF16)
            gp = sb('gp', [128, 16], F32)
            bifb = sb('bifb', [128, 8], F32)
            lifs = [sb('lifs%d' % i, [128, 8], F32) for i in range(2)]
            S.dma('sp', wc[:], self.mconv[l], writes=['wc'], sem='c')
            S.dma('sp', gp[:], self.gpre[l], writes=['gp'], sem='c')
            S.dma('sp', bifb[:], self.bif[l].partition_broadcast(128), writes=['bifb'], sem='c')
            S.dma('sp', wif32[:], self.w_in[l][:, O_I:O_I + 8].rearrange("(kt p) c -> p kt c", p=128),
                  writes=['wif32'], sem='c', allow_slow_non_contiguous=True)
            for kt in range(16):
                S.op('dve', lambda e, kt=kt: e.tensor_scalar(wif[:, kt], wif32[:, kt], gp[:, kt:kt + 1], None, ALU.mult),
                     ['wif32', 'gp'], ['wif'])
            r_x, r_wf, r_wt, r_stg, r_stf, r_U, r_ps = (Ring('x', 2), Ring('wf', 3), Ring('wt', 2), Ring('stg', 4),
                                                        Ring('stf', 2), Ring('U', 2), Ring('ps', 4))
            cnt = 0
            for tb in range(8):
                hi = tb % 2
                hk = ('hT', hi)
                t0 = tb * 512
                for tt in range(4):
                    i = r_x.next()
                    S.dma('sp', xt[i][:], xin[t0 + tt * 128:t0 + (tt + 1) * 128, :], writes=[('xt', i)], sem='xt%d' % i)
                    self.norm_T(xt[i][:], ('xt', i), xn[i][:], ('xn', i), ss[i][:], ('ss', i), hT[hi], hk, tt, 4, 5)
                if tb % 4 == 0:
                    S.op('pool', lambda e: e.memset(H[:], 0.0), [], ['halo'])
                for nt in range(88):
                    iw = r_wf.next()
                    S.dma('sp', wf[iw][:], self.WF_in[nt].rearrange("p (kt c) -> p kt c", kt=16),
                          reads=['wscr'], writes=[('wf', iw)], sem='wf%d' % iw)
                    ps = r_ps.next()
                    pk = ('ps', ps)
                    for kt in range(16):
                        self.mm(self.PS[ps][:, :], wf[iw][:, kt, :], hT[hi][:, kt, :], kt == 0, kt == 15,
                                [('wf', iw), hk], [pk])
                    P = self.PS[ps][:, :]
                    cnt += 1
                    if nt < 16:
                        iu = r_U.next()
                        uk = ('U', iu)
                        Ut = Ub[iu]
                        self.act(Ut[:, 3:515], P, AF.Copy, [pk], [uk])
                        S.op('pool', lambda e, Ut=Ut, nt=nt: e.tensor_copy(Ut[:, 0:3], H[:, nt, :]), ['halo'], [uk])
                        S.op('pool', lambda e, Ut=Ut, nt=nt: e.tensor_copy(H[:, nt, :], Ut[:, 512:515]), [uk], ['halo'])
                        ak = ('acc', iu)
                        A = acc[iu]
                        S.op('dve', lambda e, Ut=Ut, A=A, nt=nt: e.tensor_scalar(A[:], Ut[:, 3:515], wc[:, nt, 3:4], None, ALU.mult),
                             [uk, 'wc'], [ak])
                        for j in (2, 1, 0):
                            S.op('dve', lambda e, Ut=Ut, A=A, nt=nt, j=j: e.scalar_tensor_tensor(
                                out=A[:], in0=Ut[:, j:j + 512], scalar=wc[:, nt, j:j + 1], in1=A[:],
                                op0=ALU.mult, op1=ALU.add), [uk, 'wc', ak], [ak])
                        ig = r_stg.next()
                        if nt < 8:
                            self.act(stg[ig][:], A[:], AF.Silu, [ak], [('stg', ig)])
                        else:
                            self.act(A[:], A[:], AF.Silu, [ak], [ak])
                            S.op('pool', lambda e, A=A, ig=ig: e.tensor_scalar(stg[ig][:], A[:], 1.0 / 16, None, ALU.mult),
                                 [ak], [('stg', ig)])
                        S.dma('act', self.qkm[nt][:, t0:t0 + 512], stg[ig][:], reads=[('stg', ig)], writes=['qkm'],
                              sem='stg%d' % ig)
                    elif nt < 32:
                        ig = r_stg.next()
                        if cnt % 2:
                            self.act(stg[ig][:], P, AF.Copy, [pk], [('stg', ig)])
                        else:
                            S.op('dve', lambda e, ig=ig, P=P: e.tensor_copy(stg[ig][:], P), [pk], [('stg', ig)])
                        S.dma('act', self.qkd[nt - 16][:, t0:t0 + 512], stg[ig][:], reads=[('stg', ig)], writes=['qkd'],
                              sem='stg%d' % ig)
                    elif nt < 40:
                        ig = r_stf.next()
                        S.op('dve', lambda e, ig=ig, P=P: e.tensor_copy(stf[ig][:], P), [pk], [('stf', ig)])
                        S.dma('act', self.uT[nt - 32][:, t0:t0 + 512], stf[ig][:], reads=[('stf', ig)], writes=['uT'],
                              sem='stf%d' % ig)
                    else:
                        ig = r_stg.next()
                        self.act(stg[ig][:], P, AF.Sigmoid, [pk], [('stg', ig)])
                        S.dma('act', self.gT[nt - 40][:, t0:t0 + 512], stg[ig][:], reads=[('stg', ig)], writes=['gT'],
                              sem='stg%d' % ig)
                for j in range(6):
                    iw = r_wt.next()
                    for q in range(4):
                        S.dma('sp', wt[iw][:, 4 * q:4 * q + 4, :],
                              self.WT_in[j][:, 4 * q * 512:(4 * q + 4) * 512].rearrange("p (kt c) -> p kt c", kt=4),
                              reads=['wscr'], writes=[('wt', iw, q)], sem='wt%d' % iw)
                    wk = [('wt', iw, q) for q in range(4)]
                    dst, c0 = [(self.vm, 0), (self.vm, 512), (self.om, 0), (self.om, 512), (self.vd, 0), (self.vd, 512)][j]
                    for tt in range(4):
                        ps = r_ps.next()
                        pk = ('ps', ps)
                        for kt in range(16):
                            self.mm(self.PS[ps][:, :], hT[hi][:, kt, tt * 128:(tt + 1) * 128], wt[iw][:, kt, :],
                                    kt == 0, kt == 15, wk + [hk], [pk])
                        ig = r_stg.next()
                        P = self.PS[ps][:, :]
                        if j in (2, 3):
                            self.act(stg[ig][:], P, AF.Sigmoid, [pk], [('stg', ig)])
                        else:
                            S.op('dve', lambda e, ig=ig, P=P: e.tensor_copy(stg[ig][:], P), [pk], [('stg', ig)])
                        S.dma('act', dst[t0 + tt * 128:t0 + (tt + 1) * 128, c0:c0 + 512], stg[ig][:],
                              reads=[('stg', ig)], writes=['tok'], sem='stg%d' % ig)
                for tt in range(4):
                    ps = r_ps.next()
                    pk = ('ps', ps)
                    for kt in range(16):
                        self.mm(self.PS[ps][:, 0:8], hT[hi][:, kt, tt * 128:(tt + 1) * 128], wif[:, kt, :],
                                kt == 0, kt == 15, ['wif', hk], [pk])
                    il = tt % 2
                    lk = ('lifs', il)
                    Lt = lifs[il]
                    S.op('dve', lambda e, Lt=Lt, ps=ps: e.tensor_tensor(Lt[:], self.PS[ps][:, 0:8], bifb[:], ALU.add),
                         [pk, 'bifb'], [lk])
                    self.act(Lt[:, 4:8], Lt[:, 4:8], AF.Exp, [lk], [lk], scale=-1.0)
                    self.act(Lt[:, 4:8], Lt[:, 4:8], AF.Ln, [lk], [lk], bias=1.0)
                    S.op('dve', lambda e, Lt=Lt: e.tensor_scalar(Lt[:, 4:8], Lt[:, 4:8], -1.0, None, ALU.mult), [lk], [lk])
                    S.dma('act', self.lif[t0 + tt * 128:t0 + (tt + 1) * 128, :], Lt[:], reads=[lk], writes=['tok'],
                          sem='lifs%d' % il)
            S.barrier()

    def mlstm_gen(self, l, es):
        nc, S = self.nc, self.S
        PS = self.PS
        if True:
            sb = lambda n, s, d: self.sb(es, n, s, d)
            qk = [sb('mqk%d' % i, [128, 16, 512], BF16) for i in range(2)]
            va = [sb('mva%d' % i, [128, 4, 257], BF16) for i in range(2)]
            os_ = [sb('mo%d' % i, [128, 1024], BF16) for i in range(2)]
            lf = [sb('mlf%d' % i, [128, 8], F32) for i in range(2)]
            sm = [sb('msm%d' % i, [128, 40], F32) for i in range(2)]
            LFb = sb('mLFb', [128, 4, 128], F32)
            At = sb('mAt', [128, 128], F32)
            Pt = sb('mPt', [128, 128], BF16)
            isb = sb('misb', [128, 257], F32)
            tot = sb('mtot', [128, 257], F32)
            hh = sb('mhh', [128, 256], F32)
            junk = sb('mjunk', [128, 256], BF16)
            s1 = sb('ms1', [128, 4], F32)
            kw = sb('mkw', [128, 256], BF16)
            Cst = sb('mC', [128, 4, 2, 257], F32)
            Cb = sb('mCb', [128, 4, 2, 257], BF16)
            gno = [sb('mgno%d' % i, [128, 1024], F32) for i in range(2)]
            ya = [sb('mya%d' % i, [128, 1024], BF16) for i in range(2)]
            yst = [sb('myst%d' % i, [128, 8, 128], BF16) for i in range(2)]
            mnb = sb('mnb', [128, 1024], F32)
            S.dma('sp', mnb[:], self.mnorm[l].partition_broadcast(128), writes=['mnb'], sem='c')
            for i in range(2):
                S.op('pool', lambda e, i=i: e.memset(va[i][:, :, 256:257], 1.0), [], [('mva', i)])
            ci = -1
            for b in range(NB):
                S.op('pool', lambda e: e.memset(Cst[:], 0.0), [], ['mC'])
                S.op('pool', lambda e: e.memset(Cb[:], 0.0), [], ['mCb'])
                for c in range(16):
                    ci += 1
                    i2 = ci % 2
                    tok0 = b * L + c * 128
                    if c % 4 == 0:
                        iq = (ci // 4) % 2
                        for q in range(4):
                            S.dma('sp', qk[iq][:, 4 * q:4 * q + 4, :],
                                  self.qkm[4 * q:4 * q + 4, :, tok0:tok0 + 512].rearrange("n p t -> p n t"),
                                  reads=['qkm'], writes=[('mqk', iq, q)], sem='mqk%d' % iq)
                    qkk = [('mqk', iq, q) for q in range(4)]
                    co = (c % 4) * 128
                    S.dma('sp', va[i2][:, :, 0:256], self.vm[tok0:tok0 + 128, :].rearrange("t (h d) -> t h d", h=4),
                          reads=['tok'], writes=[('mva', i2)], sem='mva%d' % i2)
                    S.dma('sp', os_[i2][:], self.om[tok0:tok0 + 128, :], reads=['tok'], writes=[('mo', i2)], sem='mo%d' % i2)
                    S.dma('sp', lf[i2][:], self.lif[tok0:tok0 + 128, :], reads=['tok'], writes=[('mlf', i2)], sem='mlf%d' % i2)
                    lfk, smk, vak = ('mlf', i2), ('msm', i2), ('mva', i2)
                    LF, SM = lf[i2], sm[i2]
                    self.mm(PS[4][:, 0:4], self.U, LF[:, 4:8], True, True, ['cst', lfk], [('ps', 4)], last=False)
                    self.mm(PS[4][:, 4:8], self.ONES, LF[:, 4:8], True, True, ['cst', lfk], [('ps', 4)])
                    self.act(SM[:, 0:8], PS[4][:, 0:8], AF.Copy, [('ps', 4)], [smk])
                    self.act(SM[:, 8:16], SM[:, 0:8], AF.Exp, [smk], [smk])
                    S.op('dve', lambda e, SM=SM: e.tensor_tensor(SM[:, 16:20], SM[:, 4:8], SM[:, 0:4], ALU.subtract), [smk], [smk])
                    S.op('dve', lambda e, SM=SM, LF=LF: e.tensor_tensor(SM[:, 16:20], SM[:, 16:20], LF[:, 0:4], ALU.add), [smk, lfk], [smk])
                    self.act(SM[:, 16:20], SM[:, 16:20], AF.Exp, [smk], [smk])
                    S.op('dve', lambda e, SM=SM, LF=LF: e.tensor_tensor(SM[:, 20:24], LF[:, 0:4], SM[:, 0:4], ALU.subtract), [smk, lfk], [smk])
                    for h in range(4):
                        S.op('pool', lambda e, h=h, LF=LF: e.tensor_scalar(LFb[:, h, :], self.ONES, LF[:, 4 + h:5 + h], None, ALU.mult),
                             ['cst', lfk], ['mLFb'])
                    S.op('pool', lambda e, i2=i2: e.tensor_tensor(gno[i2][:], os_[i2][:], mnb[:], ALU.mult),
                         [('mo', i2), 'mnb'], [('mgno', i2)])
                    for h in range(4):
                        qT = [qk[iq][:, 2 * h + j, co:co + 128] for j in range(2)]
                        kT = [qk[iq][:, 8 + 2 * h + j, co:co + 128] for j in range(2)]
                        self.mm(PS[0][:, 0:128], LFb[:, h, :], self.U, True, False, ['mLFb', 'cst'], [('ps', 0)], last=False)
                        self.mm(PS[0][:, 0:128], self.ID32, self.NEGM, False, True, ['cst'], [('ps', 0)])
                        self.act(At[:], PS[0][:, 0:128], AF.Exp, [('ps', 0), smk], ['mAt'], bias=SM[:, 20 + h:21 + h])
                        for j in range(2):
                            self.mm(PS[1][:, 0:128], kT[j], qT[j], j == 0, j == 1, qkk, [('ps', 1)])
                        S.op('dve', lambda e: e.tensor_tensor(Pt[:], At[:], PS[1][:, 0:128], ALU.mult), ['mAt', ('ps', 1)], ['mPt'])
                        self.mm(PS[2][:, 0:257], Pt[:], va[i2][:, h, :], True, True, ['mPt', vak], [('ps', 2)])
                        for j in range(2):
                            self.mm(PS[3][:, 0:257], qT[j], Cb[:, h, j, :], j == 0, j == 1, qkk + ['mCb'], [('ps', 3)])
                        self.act(isb[:], PS[3][:, 0:257], AF.Copy, [('ps', 3), smk], ['misb'], scale=SM[:, 8 + h:9 + h])
                        S.op('dve', lambda e: e.tensor_tensor(tot[:], isb[:], PS[2][:, 0:257], ALU.add), ['misb', ('ps', 2)], ['mtot'])
                        self.act(s1[:, 0:1], tot[:, 256:257], AF.Abs, ['mtot'], ['ms1'])
                        S.op('dve', lambda e: e.tensor_scalar_max(s1[:, 0:1], s1[:, 0:1], 1.0), ['ms1'], ['ms1'])
                        S.op('dve', lambda e: e.reciprocal(s1[:, 0:1], s1[:, 0:1]), ['ms1'], ['ms1'])
                        S.op('dve', lambda e: e.tensor_scalar(hh[:], tot[:, 0:256], s1[:, 0:1], None, ALU.mult), ['mtot', 'ms1'], ['mhh'])
                        S.op('pool', lambda e: e.memset(s1[:, 1:2], 0.0), [], ['ms1b'])
                        self.act(junk[:], hh[:], AF.Square, ['mhh'], ['mjunk', 'ms1b'], accum_out=s1[:, 1:2])
                        self.rstd(s1[:, 1:2], 256, 'ms1b')
                        S.op('dve', lambda e, h=h, i2=i2: e.scalar_tensor_tensor(
                            out=ya[i2][:, h * 256:(h + 1) * 256], in0=hh[:], scalar=s1[:, 1:2],
                            in1=gno[i2][:, h * 256:(h + 1) * 256], op0=ALU.mult, op1=ALU.mult),
                            ['mhh', 'ms1b', ('mgno', i2)], [('mya', i2)])
                        for j in range(2):
                            self.mm(PS[4][:, j * 128:(j + 1) * 128], kT[j], self.IDB[:], True, True, qkk + ['idb'], [('ps', 4)],
                                    last=(j == 1))
                        self.act(kw[:], PS[4][:, 0:256], AF.Copy, [('ps', 4), smk], ['mkw'], scale=SM[:, 16 + h:17 + h])
                        for j in range(2):
                            self.mm(PS[(5, 7)[j]][:, 0:257], kw[:, j * 128:(j + 1) * 128], va[i2][:, h, :], True, True,
                                    ['mkw', vak], [('ps', (5, 7)[j])])
                            S.op('dve', lambda e, h=h, j=j, SM=SM: e.scalar_tensor_tensor(
                                out=Cst[:, h, j, :], in0=Cst[:, h, j, :], scalar=SM[:, 12 + h:13 + h], in1=PS[(5, 7)[j]][:, 0:257],
                                op0=ALU.mult, op1=ALU.add), ['mC', smk, ('ps', (5, 7)[j])], ['mC'])
                            S.op('pool', lambda e, h=h, j=j: e.tensor_copy(Cb[:, h, j, :], Cst[:, h, j, :]), ['mC'], ['mCb'])
                        yield
                    for g in range(2):
                        for j in range(4):
                            self.mm(PS[4][:, j * 128:(j + 1) * 128], ya[i2][:, (g * 4 + j) * 128:(g * 4 + j + 1) * 128], self.IDB[:],
                                    True, True, [('mya', i2), 'idb'], [('ps', 4)], last=(j == 3))
                        self.act(yst[i2][:, g * 4:g * 4 + 4, :], PS[4][:, :].rearrange("p (a b) -> p a b", a=4), AF.Copy,
                                 [('ps', 4)], [('myst', i2)])
                    S.dma('act', self.yT[0:8, :, tok0:tok0 + 128].rearrange("n p t -> p n t"), yst[i2][:],
                          reads=[('myst', i2)], writes=['yT'], sem='myst%d' % i2)
                    yield

    def attn_gen(self, l, es):
        nc, S = self.nc, self.S
        PS = self.PS
        scale = 128 ** -0.5
        if True:
            sb = lambda n, s, d: self.sb(es, n, s, d)
            wide = sb('awide', [128, 4, 1024], F32)
            cfar = sb('acfar', [128, 4], F32)
            lamT = sb('alamT', [128, 4], F32)
            lm = sb('alm', [128, 4], F32)
            dnb = sb('adnb', [128, 1024], F32)
            kseq = [sb('akseq%d' % i, [128, L], BF16) for i in range(2)]
            qblk = [sb('aqblk%d' % i, [128, 512], BF16) for i in range(2)]
            vaug = sb('avaug', [128, 16, 257], BF16)
            tmp = [sb('atmp%d' % i, [128, 512], F32) for i in range(2)]
            pT = [sb('apT%d' % i, [128, 512], BF16) for i in range(3)]
            s4 = sb('as4', [128, 4], F32)
            o0 = sb('ao0', [128, 4, 256], F32)
            oo = sb('aoo', [128, 256], F32)
            junk = sb('ajunk', [128, 256], BF16)
            yb = sb('ayb', [128, 256], BF16)
            yst = [sb('ayst%d' % i, [128, 2, 128], BF16) for i in range(2)]
            S.dma('sp', wide[:], self.wide[:, :, :], writes=['awide'], sem='c')
            S.dma('sp', cfar[:], self.cfar[:, :], writes=['acfar'], sem='c')
            S.dma('sp', lamT[:], self.lamT[l], writes=['alamT'], sem='c')
            S.dma('sp', dnb[:], self.dnorm[l].partition_broadcast(128), writes=['adnb'], sem='c')
            lami = sb('alami', [128, 2], F32)
            S.dma('sp', lami[:], self.lami[l], writes=['alami'], sem='c')
            S.op('pool', lambda e: e.tensor_scalar(dnb[:], dnb[:], lami[:, 0:1], None, ALU.mult), ['adnb', 'alami'], ['adnb'])
            S.op('dve', lambda e: e.tensor_tensor(lm[:, 0:1], lamT[:, 0:1], lamT[:, 1:2], ALU.mult), ['alamT'], ['alm'])
            S.op('dve', lambda e: e.tensor_tensor(lm[:, 1:2], lamT[:, 2:3], lamT[:, 3:4], ALU.mult), ['alamT', 'alm'], ['alm'])
            self.mm(PS[7][:, 0:2], self.ONES, lm[:, 0:2], True, True, ['cst', 'alm'], [('ps', 7)])
            self.act(lm[:, 2:4], PS[7][:, 0:2], AF.Exp, [('ps', 7)], ['alm'])
            S.op('dve', lambda e: e.tensor_tensor(lm[:, 0:1], lm[:, 3:4], lm[:, 2:3], ALU.subtract), ['alm'], ['alm'])
            S.op('dve', lambda e: e.tensor_scalar(lm[:, 0:1], lm[:, 0:1], lami[:, 1:2], None, ALU.add), ['alm', 'alami'], ['alm'])
            S.op('pool', lambda e: e.memset(vaug[:, :, 256:257], 1.0), [], ['avaug'])
            r_q, r_tmp, r_pT, r_st, r_y = Ring('q', 2), Ring('tmp', 2), Ring('pT', 3), Ring('st', 2), Ring('y', 2)
            for b in range(NB):
                for h in range(4):
                    S.dma('sp', vaug[:, :, 0:256],
                          self.vd[b * L:(b + 1) * L, h * 256:(h + 1) * 256].rearrange("(kt p) d -> p kt d", p=128),
                          reads=['tok'], writes=['avaug'], sem='avaug')
                    for m in range(2):
                        S.dma('sp', kseq[m][:], self.qkd[8 + 2 * h + m][:, b * L:(b + 1) * L], reads=['qkd'],
                              writes=[('akseq', m)], sem='akseq%d' % m)
                    for I in range(4):
                        for m in range(2):
                            iq = r_q.next()
                            S.dma('sp', qblk[iq][:], self.qkd[2 * h + m][:, b * L + I * 512:b * L + (I + 1) * 512],
                                  reads=['qkd'], writes=[('aqblk', iq)], sem='aqblk%d' % iq)
                            for j in range(4 * I + 4):
                                ist = r_st.next()
                                pk = ('ps', 4 + ist)
                                self.mm(PS[4 + ist][:, :], kseq[m][:, j * 128:(j + 1) * 128], qblk[iq][:], True, True,
                                        [('akseq', m), ('aqblk', iq)], [pk])
                                ip = r_pT.next()
                                ppk = ('apT', ip)
                                if j <= 4 * I - 2:
                                    self.act(pT[ip][:], PS[4 + ist][:, :], AF.Exp, [pk, 'acfar'], [ppk], scale=scale,
                                             bias=cfar[:, h:h + 1])
                                else:
                                    x0 = (4 * I - j) * 128 + 384
                                    it = r_tmp.next()
                                    S.op('dve', lambda e, it=it, ist=ist, x0=x0, h=h: e.scalar_tensor_tensor(
                                        out=tmp[it][:], in0=PS[4 + ist][:, :], scalar=scale, in1=wide[:, h, x0:x0 + 512],
                                        op0=ALU.mult, op1=ALU.add), [pk, 'awide'], [('atmp', it)])
                                    self.act(pT[ip][:], tmp[it][:], AF.Exp, [('atmp', it)], [ppk])
                                for qi in range(4):
                                    if 4 * I + qi < j:
                                        continue
                                    self.mm(PS[qi][:, 0:257], pT[ip][:, qi * 128:(qi + 1) * 128], vaug[:, j, :],
                                            j == 0, j == 4 * I + qi, [ppk, 'avaug'], [('ps', qi)])
                                yield
                            for qi in range(4):
                                acc = PS[qi][:, 0:257]
                                ak_ = ('ps', qi)
                                S.op('dve', lambda e, acc=acc: e.reciprocal(s4[:, 0:1], acc[:, 256:257]), [ak_], ['as4'])
                                if m == 0:
                                    self.act(o0[:, qi, :], acc[:, 0:256], AF.Copy, [ak_, 'as4'], [('ao0', qi)], scale=s4[:, 0:1])
                                    continue
                                S.op('dve', lambda e: e.tensor_tensor(s4[:, 1:2], s4[:, 0:1], lm[:, 0:1], ALU.mult), ['as4', 'alm'], ['as4'])
                                S.op('dve', lambda e, acc=acc, qi=qi: e.scalar_tensor_tensor(
                                    out=oo[:], in0=acc[:, 0:256], scalar=s4[:, 1:2], in1=o0[:, qi, :], op0=ALU.mult, op1=ALU.add),
                                    [ak_, 'as4', ('ao0', qi)], ['aoo'])
                                S.op('pool', lambda e: e.memset(s4[:, 2:3], 0.0), [], ['as4b'])
                                self.act(junk[:], oo[:], AF.Square, ['aoo'], ['ajunk', 'as4b'], accum_out=s4[:, 2:3])
                                self.rstd(s4[:, 2:3], 256, 'as4b')
                                S.op('dve', lambda e, h=h: e.scalar_tensor_tensor(out=yb[:], in0=oo[:], scalar=s4[:, 2:3],
                                                                                  in1=dnb[:, h * 256:(h + 1) * 256],
                                                                                  op0=ALU.mult, op1=ALU.mult),
                                     ['aoo', 'as4b', 'adnb'], ['ayb'])
                                iy = r_y.next()
                                for j in range(2):
                                    self.mm(PS[7][:, j * 128:(j + 1) * 128], yb[:, j * 128:(j + 1) * 128], self.IDB[:], True, True,
                                            ['ayb', 'idb'], [('ps', 7)], last=(j == 1))
                                self.act(yst[iy][:], PS[7][:, 0:256].rearrange("p (a b) -> p a b", a=2), AF.Copy, [('ps', 7)],
                                         [('ayst', iy)])
                                tok0 = b * L + I * 512 + qi * 128
                                S.dma('act', self.yT[8 + 2 * h:8 + 2 * h + 2, :, tok0:tok0 + 128].rearrange("n p t -> p n t"),
                                      yst[iy][:], reads=[('ayst', iy)], writes=['yT'], sem='ayst%d' % iy)
                            yield

    def s5_setup(self, l, es):
        nc, S = self.nc, self.S
        PS = self.PS
        if True:
            sb = lambda n, s, d: self.sb(es, n, s, d)
            pw = sb('spw', [128, 12, 2, 32], F32)
            d8 = sb('sd8', [128, 8], F32)
            pj = sb('spj', [128, 16, 3, 32], F32)
            ER = [sb('sER%d' % i, [128, 128], F32) for i in range(2)]
            EI = [sb('sEI%d' % i, [128, 128], F32) for i in range(2)]
            npw = sb('snpw', [128, 11, 32], F32)
            es_tab = ExitStack()
            sbt = lambda n, s, d: self.sb(es_tab, n, s, d)
            bbr = sbt('sbbr', [128, 32, 128], F32)
            bbi = sbt('sbbi', [128, 32, 128], F32)
            cpr = sbt('scpr', [128, 32, 128], F32)
            cpi = sbt('scpi', [128, 32, 128], F32)
            S.dma('sp', d8[:], self.s5d[l], writes=['sd8'], sem='c')
            def zcalc(es2, tag, n, srcs, want_z):
                sb2 = lambda nm, s_, d_: self.sb(es2, nm, s_, d_)
                lr, li_, ld = (sb2('sz%s%d' % (tag, i), [128, n], F32) for i in range(3))
                t = [sb2('szt%s%d' % (tag, i), [128, n], F32) for i in range(6)]
                k = 'sz' + tag
                for i, dst in enumerate((lr, li_, ld)):
                    S.dma('sp', dst[:], srcs[i], writes=[k], sem='c')
                V = lambda fn: S.op('dve', fn, [k], [k])
                A = lambda out, in_, f, **kw: self.act(out, in_, f, [k], [k], **kw)
                A(ld[:], ld[:], AF.Exp)
                V(lambda e: e.tensor_tensor(t[0][:], lr[:], ld[:], ALU.mult))
                A(t[0][:], t[0][:], AF.Exp, scale=1.0 / 32)
                V(lambda e: e.tensor_tensor(t[1][:], li_[:], ld[:], ALU.mult))
                A(t[2][:], t[1][:], AF.Sin, scale=1.0 / 32)
                V(lambda e: e.tensor_scalar(t[1][:], t[1][:], 1.0 / 32, math.pi / 2, ALU.mult, ALU.add))
                A(t[1][:], t[1][:], AF.Sin)
                V(lambda e: e.tensor_tensor(t[1][:], t[1][:], t[0][:], ALU.mult))
                V(lambda e: e.tensor_tensor(t[2][:], t[2][:], t[0][:], ALU.mult))
                for _ in range(5):
                    V(lambda e: e.tensor_tensor(t[3][:], t[1][:], t[1][:], ALU.mult))
                    V(lambda e: e.tensor_tensor(t[4][:], t[2][:], t[2][:], ALU.mult))
                    V(lambda e: e.tensor_tensor(t[2][:], t[1][:], t[2][:], ALU.mult))
                    V(lambda e: e.tensor_scalar(t[2][:], t[2][:], 2.0, None, ALU.mult))
                    V(lambda e: e.tensor_tensor(t[1][:], t[3][:], t[4][:], ALU.subtract))
                are, aim = t[1], t[2]
                if not want_z:
                    return are, aim, k
                V(lambda e: e.tensor_tensor(t[3][:], lr[:], lr[:], ALU.mult))
                V(lambda e: e.tensor_tensor(t[4][:], li_[:], li_[:], ALU.mult))
                V(lambda e: e.tensor_tensor(t[3][:], t[3][:], t[4][:], ALU.add))
                V(lambda e: e.reciprocal(t[3][:], t[3][:]))
                V(lambda e: e.tensor_scalar(t[0][:], are[:], -1.0, None, ALU.add))
                V(lambda e: e.tensor_tensor(t[4][:], t[0][:], lr[:], ALU.mult))
                V(lambda e: e.tensor_tensor(t[5][:], aim[:], li_[:], ALU.mult))
                V(lambda e: e.tensor_tensor(t[4][:], t[4][:], t[5][:], ALU.add))
                V(lambda e: e.tensor_tensor(t[4][:], t[4][:], t[3][:], ALU.mult))
                V(lambda e: e.tensor_tensor(t[5][:], aim[:], lr[:], ALU.mult))
                V(lambda e: e.tensor_tensor(t[0][:], t[0][:], li_[:], ALU.mult))
                V(lambda e: e.tensor_tensor(t[5][:], t[5][:], t[0][:], ALU.subtract))
                V(lambda e: e.tensor_tensor(t[5][:], t[5][:], t[3][:], ALU.mult))
                return t[4], t[5], k

            bbrf = bbr[:].rearrange("p a b -> p (a b)")
            bbif = bbi[:].rearrange("p a b -> p (a b)")
            for q in range(4):
                cs = slice(q * 1024, (q + 1) * 1024)
                with ExitStack() as es2:
                    zre, zim, zk = zcalc(es2, 'r', 1024, [self.s5r[l, i][:, cs] for i in range(3)], True)
                    bT = [self.sb(es2, 'sbT%d' % i, [128, 1024], F32) for i in range(2)]
                    t6 = self.sb(es2, 'st6', [128, 1024], F32)
                    for i in range(2):
                        S.dma('sp', bT[i][:], self.s5bT[l, i][:, cs], writes=['sbT'], sem='c')
                    V = lambda fn, w: S.op('dve', fn, [zk, 'sbT', 'st6', 'sbb'], w)
                    V(lambda e: e.tensor_tensor(bbrf[:, cs], zre[:], bT[0][:], ALU.mult), ['sbb'])
                    V(lambda e: e.tensor_tensor(t6[:], zim[:], bT[1][:], ALU.mult), ['st6'])
                    V(lambda e: e.tensor_tensor(bbrf[:, cs], bbrf[:, cs], t6[:], ALU.subtract), ['sbb'])
                    V(lambda e: e.tensor_tensor(bbif[:, cs], zre[:], bT[1][:], ALU.mult), ['sbb'])
                    V(lambda e: e.tensor_tensor(t6[:], zim[:], bT[0][:], ALU.mult), ['st6'])
                    V(lambda e: e.tensor_tensor(bbif[:, cs], bbif[:, cs], t6[:], ALU.add), ['sbb'])
                    S.barrier()
            with ExitStack() as es2:
                are, aim, ak = zcalc(es2, 'p', 32, [self.s5p[l, i] for i in range(3)], False)
                S.op('dve', lambda e: e.tensor_copy(pw[:, 0, 0, :], are[:]), [ak], ['spw'])
                S.op('dve', lambda e: e.tensor_copy(pw[:, 0, 1, :], aim[:]), [ak], ['spw'])
                tq = self.sb(es2, 'stq', [128, 2, 32], F32)
                for k in range(1, 11):
                    V = lambda fn, w: S.op('dve', fn, ['spw', 'stq'], w)
                    V(lambda e, k=k: e.tensor_tensor(tq[:, 0, :], pw[:, k - 1, 0, :], pw[:, k - 1, 0, :], ALU.mult), ['stq'])
                    V(lambda e, k=k: e.tensor_tensor(tq[:, 1, :], pw[:, k - 1, 1, :], pw[:, k - 1, 1, :], ALU.mult), ['stq'])
                    V(lambda e, k=k: e.tensor_tensor(pw[:, k, 0, :], tq[:, 0, :], tq[:, 1, :], ALU.subtract), ['spw'])
                    V(lambda e, k=k: e.tensor_tensor(tq[:, 0, :], pw[:, k - 1, 0, :], pw[:, k - 1, 1, :], ALU.mult), ['stq'])
                    V(lambda e, k=k: e.tensor_scalar(pw[:, k, 1, :], tq[:, 0, :], 2.0, None, ALU.mult), ['spw'])
                S.dma('sp', cpr[:].rearrange("p a b -> p (a b)"), self.s5cP[l, 0], writes=['scp'], sem='c')
                S.dma('sp', cpi[:].rearrange("p a b -> p (a b)"), self.s5cP[l, 1], writes=['scp'], sem='c')
                S.op('pool', lambda e: e.tensor_scalar(cpi[:], cpi[:], -1.0, None, ALU.mult), ['scp'], ['scp'])
                S.barrier()
            S.op('dve', lambda e: e.tensor_copy(pj[:, 0, 0:2, :], pw[:, 0, :, :]), ['spw'], ['spj'])
            with ExitStack() as es2:
                tj = self.sb(es2, 'stj', [128, 2, 32], F32)
                for j in range(1, 16):
                    V = lambda fn, w: S.op('dve', fn, ['spw', 'spj', 'stj'], w)
                    V(lambda e, j=j: e.tensor_tensor(tj[:, 0, :], pj[:, j - 1, 0, :], pw[:, 0, 0, :], ALU.mult), ['stj'])
                    V(lambda e, j=j: e.tensor_tensor(tj[:, 1, :], pj[:, j - 1, 1, :], pw[:, 0, 1, :], ALU.mult), ['stj'])
                    V(lambda e, j=j: e.tensor_tensor(pj[:, j, 0, :], tj[:, 0, :], tj[:, 1, :], ALU.subtract), ['spj'])
                    V(lambda e, j=j: e.tensor_tensor(tj[:, 0, :], pj[:, j - 1, 0, :], pw[:, 0, 1, :], ALU.mult), ['stj'])
                    V(lambda e, j=j: e.tensor_tensor(tj[:, 1, :], pj[:, j - 1, 1, :], pw[:, 0, 0, :], ALU.mult), ['stj'])
                    V(lambda e, j=j: e.tensor_tensor(pj[:, j, 1, :], tj[:, 0, :], tj[:, 1, :], ALU.add), ['spj'])
                S.op('dve', lambda e: e.tensor_scalar(pj[:, :, 2, :], pj[:, :, 1, :], -1.0, None, ALU.mult), ['spj'], ['spj'])
                S.barrier()
            S.op('dve', lambda e: e.tensor_scalar(npw[:], pw[:, 0:11, 1, :], -1.0, None, ALU.mult), ['spw'], ['snpw'])
            for i, tab in enumerate((bbr, bbi, cpr, cpi)):
                S.dma('act', self.s5tab[i], tab[:].rearrange("p a b -> p (a b)"), reads=['sbb', 'scp'], writes=['s5tab'], sem='c')
            S.barrier()
            es_tab.close()
            st = dict(pw=pw, npw=npw, pj=pj, d8=d8, ER=ER, EI=EI)
            st['tabs'] = [sb('stab%d' % i, [128, 4, 4, 128], F32) for i in range(2)]
            st['ut'] = sb('sut', [128, L], F32)
            st['XR'] = [sb('sXR%d' % i, [128, L], F32) for i in range(2)]
            st['XI'] = [sb('sXI%d' % i, [128, L], F32) for i in range(2)]
            st['Yacc'] = sb('sYacc', [128, L], F32)
            st['yg'] = sb('syg', [128, L], BF16)
            return st

    def s5_main(self, l, st):
        S, PS = self.S, self.PS
        pw, npw, pj, d8, ER, EI, tabs, ut, XR, XI, Yacc, yg = (st[k] for k in (
            'pw', 'npw', 'pj', 'd8', 'ER', 'EI', 'tabs', 'ut', 'XR', 'XI', 'Yacc', 'yg'))
        pk = ('ps', 6)
        bv = lambda X: X[:].rearrange("p (n j) -> p n j", j=16)

        def hs(R0, I0, R1, I1, kr0, ki0, kr1, ki1, k, d, W, gp_):
            pr, pi_, npi = pw[:, k, 0, gp_:gp_ + 1], pw[:, k, 1, gp_:gp_ + 1], npw[:, k, gp_:gp_ + 1]
            lo, hi = (lambda X: X[..., 0:W - d]), (lambda X: X[..., d:])
            hd = lambda X: X[..., 0:d]
            S.op('dve', lambda e: e.scalar_tensor_tensor(out=hi(R1), in0=lo(R0), scalar=pr, in1=hi(R0), op0=ALU.mult, op1=ALU.add),
                 [kr0, 'spw'], [kr1])
            S.op('dve', lambda e: e.scalar_tensor_tensor(out=hi(R1), in0=lo(I0), scalar=npi, in1=hi(R1), op0=ALU.mult, op1=ALU.add),
                 [ki0, 'snpw', kr1], [kr1])
            S.op('pool', lambda e: e.tensor_copy(hd(R1), hd(R0)), [kr0], [kr1])
            S.op('dve', lambda e: e.scalar_tensor_tensor(out=hi(I1), in0=lo(I0), scalar=pr, in1=hi(I0), op0=ALU.mult, op1=ALU.add),
                 [ki0, 'spw'], [ki1])
            S.op('dve', lambda e: e.scalar_tensor_tensor(out=hi(I1), in0=lo(R0), scalar=pi_, in1=hi(I1), op0=ALU.mult, op1=ALU.add),
                 [kr0, 'spw', ki1], [ki1])
            S.op('pool', lambda e: e.tensor_copy(hd(I1), hd(I0)), [ki0], [ki1])

        for ct in range(8):
            it = ct % 2
            TB, tk = tabs[it], ('stab', it)
            for i in range(4):
                S.dma('sp', TB[:, i], self.s5tab[i][:, ct * 512:(ct + 1) * 512].rearrange("p (a b) -> p a b", a=4),
                      reads=['s5tab'], writes=[tk], sem='stab%d' % it)
            for b in range(NB):
                S.dma('sp', ut[:], self.uT[ct][:, b * L:(b + 1) * L], reads=['uT'], writes=['sut'], sem='sut')
                for g4 in range(4):
                    gp_ = ct * 4 + g4
                    n = 0
                    for tb in range(4):
                        sl = slice(tb * 512, (tb + 1) * 512)
                        for ri, X in enumerate((XR[0], XI[0])):
                            n += 1
                            self.mm(PS[6][:, :], TB[:, ri, g4, :], ut[:, sl], True, True, [tk, 'sut'], [pk])
                            key = ('sX', ri, 0)
                            if n % 2:
                                self.act(X[:, sl], PS[6][:, :], AF.Copy, [pk], [key])
                            else:
                                S.op('dve', lambda e, X=X, sl=sl: e.tensor_copy(X[:, sl], PS[6][:, :]), [pk], [key])
                    yield
                    cur = 0
                    for k in range(4):
                        nx = 1 - cur
                        hs(bv(XR[cur]), bv(XI[cur]), bv(XR[nx]), bv(XI[nx]), ('sX', 0, cur), ('sX', 1, cur), ('sX', 0, nx), ('sX', 1, nx),
                           k, 1 << k, 16, gp_)
                        cur = nx
                        yield
                    XRv, XIv = bv(XR[cur]), bv(XI[cur])
                    kxr, kxi = ('sX', 0, cur), ('sX', 1, cur)
                    S.op('pool', lambda e, XRv=XRv: e.tensor_copy(ER[0][:], XRv[:, :, 15]), [kxr], [('sE', 0, 0)])
                    S.op('pool', lambda e, XIv=XIv: e.tensor_copy(EI[0][:], XIv[:, :, 15]), [kxi], [('sE', 1, 0)])
                    ec = 0
                    for k2 in range(7):
                        nx = 1 - ec
                        hs(ER[ec][:], EI[ec][:], ER[nx][:], EI[nx][:], ('sE', 0, ec), ('sE', 1, ec), ('sE', 0, nx), ('sE', 1, nx),
                           4 + k2, 1 << k2, 128, gp_)
                        ec = nx
                    yield
                    FR, FI = ER[ec][:, 0:127], EI[ec][:, 0:127]
                    kfr, kfi = ('sE', 0, ec), ('sE', 1, ec)
                    for j in range(16):
                        ar, ai, nai = pj[:, j, 0, gp_:gp_ + 1], pj[:, j, 1, gp_:gp_ + 1], pj[:, j, 2, gp_:gp_ + 1]
                        xr, xi = XRv[:, 1:, j], XIv[:, 1:, j]
                        S.op('dve', lambda e, xr=xr, ar=ar: e.scalar_tensor_tensor(out=xr, in0=FR, scalar=ar, in1=xr, op0=ALU.mult, op1=ALU.add),
                             [kfr, 'spj', kxr], [kxr])
                        S.op('dve', lambda e, xr=xr, nai=nai: e.scalar_tensor_tensor(out=xr, in0=FI, scalar=nai, in1=xr, op0=ALU.mult, op1=ALU.add),
                             [kfi, 'spj', kxr], [kxr])
                        S.op('dve', lambda e, xi=xi, ar=ar: e.scalar_tensor_tensor(out=xi, in0=FI, scalar=ar, in1=xi, op0=ALU.mult, op1=ALU.add),
                             [kfi, 'spj', kxi], [kxi])
                        S.op('dve', lambda e, xi=xi, ai=ai: e.scalar_tensor_tensor(out=xi, in0=FR, scalar=ai, in1=xi, op0=ALU.mult, op1=ALU.add),
                             [kfr, 'spj', kxi], [kxi])
                        if j % 4 == 3:
                            yield
                    for tb in range(4):
                        sl = slice(tb * 512, (tb + 1) * 512)
                        self.mm(PS[6][:, :], TB[:, 2, g4, :], XR[cur][:, sl], True, False, [tk, kxr], [pk], last=False)
                        self.mm(PS[6][:, :], TB[:, 3, g4, :], XI[cur][:, sl], False, True, [tk, kxi], [pk])
                        if g4 == 0:
                            self.act(Yacc[:, sl], PS[6][:, :], AF.Copy, [pk], ['sYacc'])
                        else:
                            S.op('dve', lambda e, sl=sl: e.tensor_tensor(Yacc[:, sl], Yacc[:, sl], PS[6][:, :], ALU.add),
                                 [pk, 'sYacc'], ['sYacc'])
                    yield
                S.op('dve', lambda e, ct=ct: e.scalar_tensor_tensor(out=Yacc[:], in0=ut[:], scalar=d8[:, ct:ct + 1], in1=Yacc[:],
                                                                    op0=ALU.mult, op1=ALU.add), ['sut', 'sd8', 'sYacc'], ['sYacc'])
                self.act(yg[:], Yacc[:], AF.Gelu_apprx_tanh, ['sYacc'], ['syg'])
                S.dma('act', self.ycs[ct][:, b * L:(b + 1) * L], yg[:], reads=['syg'], writes=['ycs'], sem='syg')

    def phase_mix(self, l):
        S = self.S
        with ExitStack() as es_s5:
            st = self.s5_setup(l, es_s5)
            g_s5 = self.s5_main(l, st)
            credit = 0.0
            for fn, rate in ((self.mlstm_gen, 0.3 * 704 / 160), (self.attn_gen, 0.7)):
                with ExitStack() as es2:
                    for _ in fn(l, es2):
                        credit += rate
                        while credit >= 1.0:
                            credit -= 1.0
                            next(g_s5, None)
                    S.barrier()
            for _ in g_s5:
                pass
            S.barrier()
        self.s5_glu(l)

    def s5_glu(self, l):
        nc, S = self.nc, self.S
        PS = self.PS
        with ExitStack() as es:
            sb = lambda n, s, d: self.sb(es, n, s, d)
            wg = sb('gwg', [128, 16, 8, 128], BF16)
            yc = [sb('gyc%d' % i, [128, 8, 512], BF16) for i in range(2)]
            sg = [sb('gsg%d' % i, [128, 512], F32) for i in range(2)]
            st = [sb('gst%d' % i, [128, 512], BF16) for i in range(2)]
            for q in range(4):
                S.dma('sp', wg[:, 4 * q:4 * q + 4], self.WF_glu[4 * q:4 * q + 4].rearrange("n p (kt c) -> p n kt c", kt=8),
                      reads=['wscr'], writes=['gwg'], sem='c')
            n = 0
            for tb in range(8):
                i = tb % 2
                S.dma('sp', yc[i][:], self.ycs[0:8, :, tb * 512:(tb + 1) * 512].rearrange("n p t -> p n t"),
                      reads=['ycs'], writes=[('gyc', i)], sem='gyc%d' % i)
                for ft in range(8):
                    for kt in range(8):
                        self.mm(PS[0 + (ft % 2) * 2][:, :], wg[:, ft, kt, :], yc[i][:, kt, :], kt == 0, kt == 7,
                                ['gwg', ('gyc', i)], [('ps', (ft % 2) * 2)])
                    for kt in range(8):
                        self.mm(PS[1 + (ft % 2) * 2][:, :], wg[:, 8 + ft, kt, :], yc[i][:, kt, :], kt == 0, kt == 7,
                                ['gwg', ('gyc', i)], [('ps', 1 + (ft % 2) * 2)])
                    j = n % 2
                    n += 1
                    self.act(sg[j][:], PS[1 + (ft % 2) * 2][:, :], AF.Sigmoid, [('ps', 1 + (ft % 2) * 2)], [('gsg', j)])
                    S.op('dve', lambda e, j=j, ft=ft: e.tensor_tensor(st[j][:], sg[j][:], PS[(ft % 2) * 2][:, :], ALU.mult),
                         [('gsg', j), ('ps', (ft % 2) * 2)], [('gst', j)])
                    S.dma('act', self.yT[16 + ft][:, tb * 512:(tb + 1) * 512], st[j][:], reads=[('gst', j)], writes=['yT'],
                          sem='gst%d' % j)
            S.barrier()

    def phase_merge_ffn(self, l, xin, xout):
        nc, S = self.nc, self.S
        PS = self.PS
        with ExitStack() as es:
            sb = lambda n, s, d: self.sb(es, n, s, d)
            H2 = sb('fH2', [128, 88, 2], F32)
            fc = sb('ffc', [128, 88, 3], F32)
            fb = sb('ffb', [128, 88], F32)
            S.dma('sp', fc[:], self.fconv[l], writes=['ffc'], sem='c')
            S.dma('sp', fb[:], self.fbias[l], writes=['ffb'], sem='c')
            h2T = sb('fh2T', [128, 16, 512], BF16)
            for tb in range(8):
                t0 = tb * 512
                with ExitStack() as e2:
                    sb2 = lambda n, s, d: self.sb(e2, n, s, d)
                    gpo = sb2('fgpo', [128, D], F32)
                    S.dma('sp', gpo[:], self.gpost[l].partition_broadcast(128), writes=['fgpo'], sem='c')
                    yTb = sb2('fyT', [128, 24, 512], BF16)
                    mT = sb2('fmT', [128, 16, 512], BF16)
                    gt = [sb2('fgt%d' % i, [128, 3, 512], BF16) for i in range(2)]
                    wb = [sb2('fwb%d' % i, [128, 3, 8, 128], BF16) for i in range(2)]
                    wo = [sb2('fwo%d' % i, [128, 16, 512], BF16) for i in range(2)]
                    m1 = [sb2('fm1%d' % i, [128, 512], F32) for i in range(2)]
                    m2 = [sb2('fm2%d' % i, [128, 512], F32) for i in range(2)]
                    xt = [sb2('fxt%d' % i, [128, D], F32) for i in range(2)]
                    xo = [sb2('fxo%d' % i, [128, D], F32) for i in range(2)]
                    xn = [sb2('fxn%d' % i, [128, D], BF16) for i in range(2)]
                    ss = [sb2('fss%d' % i, [128, 8], F32) for i in range(2)]
                    for q in range(6):
                        S.dma('sp', yTb[:, 4 * q:4 * q + 4, :], self.yT[4 * q:4 * q + 4, :, t0:t0 + 512].rearrange("n p t -> p n t"),
                              reads=['yT'], writes=[('fyT', q)], sem='fyT')
                    yk = [('fyT', q) for q in range(6)]
                    for ft in range(16):
                        i = ft % 2
                        S.dma('sp', wb[i][:], self.WF_br[ft:48:16].rearrange("n p (kt c) -> p n kt c", kt=8),
                              reads=['wscr'], writes=[('fwb', i)], sem='fwb%d' % i)
                        S.dma('sp', gt[i][:], self.gT[ft:48:16, :, t0:t0 + 512].rearrange("n p t -> p n t"),
                              reads=['gT'], writes=[('fgt', i)], sem='fgt%d' % i)
                        for n in range(3):
                            for kt in range(8):
                                self.mm(PS[n + 3 * i][:, :], wb[i][:, n, kt, :], yTb[:, n * 8 + kt, :], kt == 0, kt == 7,
                                        [('fwb', i)] + yk, [('ps', n + 3 * i)])
                        S.op('dve', lambda e, i=i: e.tensor_tensor(m1[i][:], gt[i][:, 0, :], PS[0 + 3 * i][:, :], ALU.mult),
                             [('fgt', i), ('ps', 3 * i)], [('fm1', i)])
                        S.op('dve', lambda e, i=i: e.tensor_tensor(m2[i][:], gt[i][:, 1, :], PS[1 + 3 * i][:, :], ALU.mult),
                             [('fgt', i), ('ps', 1 + 3 * i)], [('fm2', i)])
                        S.op('pool', lambda e, i=i: e.tensor_tensor(m1[i][:], m1[i][:], m2[i][:], ALU.add),
                             [('fm1', i), ('fm2', i)], [('fm1', i)])
                        S.op('dve', lambda e, i=i: e.tensor_tensor(m2[i][:], gt[i][:, 2, :], PS[2 + 3 * i][:, :], ALU.mult),
                             [('fgt', i), ('ps', 2 + 3 * i)], [('fm2', i)])
                        S.op('pool', lambda e, i=i, ft=ft: e.tensor_tensor(mT[:, ft, :], m1[i][:], m2[i][:], ALU.add),
                             [('fm1', i), ('fm2', i)], ['fmT'])
                    for cb in range(4):
                        S.dma('sp', wo[cb % 2][:, 0:8, :], self.WT_out[cb][:, 0:8 * 512].rearrange("p (kt c) -> p kt c", kt=8),
                              reads=['wscr'], writes=[('fwo', cb % 2, 0)], sem='fwo%d' % (cb % 2))
                        S.dma('sp', wo[cb % 2][:, 8:16, :], self.WT_out[cb][:, 8 * 512:16 * 512].rearrange("p (kt c) -> p kt c", kt=8),
                              reads=['wscr'], writes=[('fwo', cb % 2, 1)], sem='fwo%d' % (cb % 2))
                        wk = [('fwo', cb % 2, 0), ('fwo', cb % 2, 1)]
                        for tt in range(4):
                            bank = (cb * 4 + tt) % 2 + 6
                            for kt in range(16):
                                self.mm(PS[bank][:, :], mT[:, kt, tt * 128:(tt + 1) * 128], wo[cb % 2][:, kt, :], kt == 0, kt == 15,
                                        ['fmT'] + wk, [('ps', bank)])
                            i = (cb * 4 + tt) % 2
                            self.act(m1[i][:], PS[bank][:, :], AF.Copy, [('ps', bank)], [('fm1', i)])
                            S.dma('act', self.x1[t0 + tt * 128:t0 + (tt + 1) * 128, cb * 512:(cb + 1) * 512], m1[i][:],
                                  reads=[('fm1', i)], writes=[('x1', tt)], sem='fm1%d' % i)
                    for tt in range(4):
                        i = tt % 2
                        rows = slice(t0 + tt * 128, t0 + (tt + 1) * 128)
                        S.dma('sp', xo[i][:], self.x1[rows, :], reads=[('x1', tt)], writes=[('fxo', i)], sem='fxo%d' % i)
                        S.dma('sp', xt[i][:], xin[rows, :], reads=['xin'], writes=[('fxt', i)], sem='fxt%d' % i)
                        S.op('pool', lambda e, i=i: e.memset(ss[i][:, 0:1], 0.0), [], [('fss', i)])
                        self.act(xn[i][:], xo[i][:], AF.Square, [('fxo', i)], [('fxn', i), ('fss', i)], accum_out=ss[i][:, 0:1])
                        self.rstd(ss[i][:, 0:1], D, ('fss', i))
                        S.op('dve', lambda e, i=i: e.scalar_tensor_tensor(out=xo[i][:], in0=xo[i][:], scalar=ss[i][:, 0:1], in1=gpo[:],
                                                                          op0=ALU.mult, op1=ALU.mult),
                             [('fxo', i), ('fss', i), 'fgpo'], [('fxo', i)])
                        S.op('pool', lambda e, i=i: e.tensor_tensor(xt[i][:], xt[i][:], xo[i][:], ALU.add),
                             [('fxt', i), ('fxo', i)], [('fxt', i)])
                        S.dma('act', self.x1[rows, :], xt[i][:], reads=[('fxt', i)], writes=[('x1', tt)], sem='fxt%d' % i)
                        self.norm_T(xt[i][:], ('fxt', i), xn[i][:], ('fxn', i), ss[i][:, 1:2], ('fss2', i), h2T, 'fh2T', tt, 0, 1)
                    S.barrier()
                with ExitStack() as e2:
                    sb2 = lambda n, s, d: self.sb(e2, n, s, d)
                    gfo = sb2('fgfo', [128, D], F32)
                    S.dma('sp', gfo[:], self.gfpost[l].partition_broadcast(128), writes=['fgfo'], sem='c')
                    aT = sb2('faT', [128, 44, 512], BF16)
                    wf = [sb2('fwf%d' % i, [128, 16, 128], BF16) for i in range(4)]
                    wd = [sb2('fwd%d' % i, [128, 11, 512], BF16) for i in range(3)]
                    Ub = [sb2('fU%d' % i, [128, 514], F32) for i in range(2)]
                    ca = [sb2('fca%d' % i, [128, 512], F32) for i in range(2)]
                    cv = [sb2('fcv%d' % i, [128, 512], F32) for i in range(2)]
                    xo = [sb2('gxo%d' % i, [128, D], F32) for i in range(4)]
                    xt = [sb2('gxt%d' % i, [128, D], F32) for i in range(1)]
                    jk = sb2('gjk', [128, D], BF16)
                    ss = [sb2('gss%d' % i, [128, 4], F32) for i in range(2)]
                    if tb % 4 == 0:
                        S.op('pool', lambda e: e.memset(H2[:], 0.0), [], ['fH2'])
                    r_wf, r_U = Ring('wf', 4), Ring('U', 2)
                    for jp in range(44):
                        res = []
                        for half in range(2):
                            nt = jp + 44 * half
                            iw = r_wf.next()
                            S.dma('sp', wf[iw][:], self.WF_up[nt].rearrange("p (kt c) -> p kt c", kt=16),
                                  reads=['wscr'], writes=[('fwf', iw)], sem='fwf%d' % iw)
                            bank = (jp % 2) * 2 + half
                            pk = ('ps', bank)
                            for kt in range(16):
                                self.mm(PS[bank][:, :], wf[iw][:, kt, :], h2T[:, kt, :], kt == 0, kt == 15, [('fwf', iw), 'fh2T'], [pk])
                            iu = r_U.next()
                            Ut, uk = Ub[iu], ('fU', iu)
                            self.act(Ut[:, 2:514], PS[bank][:, :], AF.Copy, [pk], [uk])
                            S.op('pool', lambda e, Ut=Ut, nt=nt: e.tensor_copy(Ut[:, 0:2], H2[:, nt, :]), ['fH2'], [uk])
                            S.op('pool', lambda e, Ut=Ut, nt=nt: e.tensor_copy(H2[:, nt, :], Ut[:, 512:514]), [uk], ['fH2'])
                            o = (ca if half == 0 else cv)[jp % 2]
                            ok = ('fc', half, jp % 2)
                            S.op('dve', lambda e, Ut=Ut, o=o, nt=nt: e.tensor_scalar(o[:], Ut[:, 2:514], fc[:, nt, 2:3], fb[:, nt:nt + 1],
                                                                                     ALU.mult, ALU.add), [uk, 'ffc', 'ffb'], [ok])
                            for j in (1, 0):
                                S.op('dve', lambda e, Ut=Ut, o=o, nt=nt, j=j: e.scalar_tensor_tensor(
                                    out=o[:], in0=Ut[:, j:j + 512], scalar=fc[:, nt, j:j + 1], in1=o[:], op0=ALU.mult, op1=ALU.add),
                                    [uk, 'ffc', ok], [ok])
                            res.append((o, ok))
                        (oa, ka), (ov, kv) = res
                        self.act(oa[:], oa[:], AF.Gelu_apprx_tanh, [ka], [ka])
                        S.op('pool', lambda e, oa=oa, ov=ov, jp=jp: e.tensor_tensor(aT[:, jp, :], oa[:], ov[:], ALU.mult), [ka, kv], ['faT'])
                    r_wd = Ring('wd', 3)
                    for cb in range(4):
                        for kp in range(4):
                            iw = r_wd.next()
                            S.dma('sp', wd[iw][:], self.WT_dn[cb * 4 + kp].rearrange("p (kt c) -> p kt c", kt=11),
                                  reads=['wscr'], writes=[('fwd', iw)], sem='fwd%d' % iw)
                            for tt in range(4):
                                for kt in range(11):
                                    self.mm(PS[4 + tt][:, :], aT[:, kp * 11 + kt, tt * 128:(tt + 1) * 128], wd[iw][:, kt, :],
                                            kp == 0 and kt == 0, kp == 3 and kt == 10, ['faT', ('fwd', iw)], [('ps', 4 + tt)],
                                            last=(kt == 10))
                        for tt in range(4):
                            if (cb + tt) % 2:
                                self.act(xo[tt][:, cb * 512:(cb + 1) * 512], PS[4 + tt][:, :], AF.Copy, [('ps', 4 + tt)], [('gxo', tt)])
                            else:
                                S.op('dve', lambda e, tt=tt, cb=cb: e.tensor_copy(xo[tt][:, cb * 512:(cb + 1) * 512], PS[4 + tt][:, :]),
                                     [('ps', 4 + tt)], [('gxo', tt)])
                    for tt in range(4):
                        i = tt
                        rows = slice(t0 + tt * 128, t0 + (tt + 1) * 128)
                        S.dma('sp', xt[0][:], self.x1[rows, :], reads=[('x1', tt)], writes=[('gxt', 0)], sem='gxt0')
                        S.op('pool', lambda e, i=i: e.memset(ss[0][:, 0:1], 0.0), [], [('gss', 0)])
                        self.act(jk[:], xo[i][:], AF.Square, [('gxo', i)], ['gjk', ('gss', 0)], accum_out=ss[0][:, 0:1])
                        self.rstd(ss[0][:, 0:1], D, ('gss', 0))
                        S.op('dve', lambda e, i=i: e.scalar_tensor_tensor(out=xo[i][:], in0=xo[i][:], scalar=ss[0][:, 0:1], in1=gfo[:],
                                                                          op0=ALU.mult, op1=ALU.mult),
                             [('gxo', i), ('gss', 0), 'fgfo'], [('gxo', i)])
                        S.op('pool', lambda e, i=i: e.tensor_tensor(xo[i][:], xo[i][:], xt[0][:], ALU.add),
                             [('gxo', i), ('gxt', 0)], [('gxo', i)])
                        S.dma('act', xout[rows, :], xo[i][:], reads=[('gxo', i)], writes=['xout'], sem='gxo%d' % i)
                    S.barrier()
            S.barrier()


def _bucket_table():
    dist = np.arange(0, 4096, dtype=np.int64)
    n = np.maximum(dist, 0)
    nf = np.maximum(n, 1).astype(np.float32)
    large = 16 + (np.log(nf / np.float32(16)) / np.float32(math.log(8.0)) * np.float32(16)).astype(np.int32)
    large = np.minimum(large, 31)
    return np.where(n < 16, n, large).astype(np.int64)


def host_layout(inp):
    f32 = np.float32
    out = {}
    cst = np.zeros((128, 512), f32)
    cst[:, 0:128] = np.eye(128, dtype=f32)
    cst[:, 128:256] = np.triu(np.ones((128, 128), f32))
    cst[:, 256:384] = np.where(np.arange(128)[:, None] > np.arange(128)[None, :], f32(-30000.0), f32(0.0))
    cst[:, 384:512] = 1.0
    out['cst'] = cst
    g = lambda k: np.asarray(inp[k], f32)
    out['gpre'] = np.ascontiguousarray(g('norm_mix_pre').reshape(2, 16, 128).transpose(0, 2, 1))
    out['gffn'] = np.ascontiguousarray(g('norm_ffn_pre').reshape(2, 16, 128).transpose(0, 2, 1))
    out['gpost'] = g('norm_mix_post').reshape(2, 1, D)
    out['gfpost'] = g('norm_ffn_post').reshape(2, 1, D)
    out['mconv'] = np.ascontiguousarray(g('mlstm_conv').reshape(2, 4, 16, 128).transpose(0, 3, 2, 1))
    out['mnorm'] = g('mlstm_norm').reshape(2, 1, 1024)
    out['dnorm'] = g('diff_norm').reshape(2, 1, 1024)
    out['bif'] = g('mlstm_b_if').reshape(2, 1, 8)
    out['lamT'] = np.ascontiguousarray(g('diff_lambda').transpose(0, 2, 1))
    rb = g('rel_bias')
    bt = _bucket_table()
    kl = np.arange(128)[:, None]
    xx = np.arange(1024)[None, :] - 384
    dist = xx - kl
    wide = np.empty((128, 4, 1024), f32)
    for h in range(4):
        vals = rb[bt[np.clip(dist, 0, 4095)], h]
        wide[:, h, :] = np.where(dist >= 0, vals, f32(-30000.0))
    out['wide'] = wide
    out['cfar'] = np.ascontiguousarray(np.broadcast_to(rb[31][None, :], (128, 4)))
    out['fconv'] = np.ascontiguousarray(g('ffn_conv').reshape(2, 3, 88, 128).transpose(0, 3, 2, 1))
    out['fbias'] = np.ascontiguousarray(g('ffn_conv_b').reshape(2, 88, 128).transpose(0, 2, 1))
    out['s5d'] = np.ascontiguousarray(g('s5_d').reshape(2, 8, 128).transpose(0, 2, 1))
    lre, lim, ldt = g('s5_lambda_re'), g('s5_lambda_im'), g('s5_log_dt')
    ldt_full = np.broadcast_to(ldt[:, :, None], (2, 64, 64))
    s5p = np.empty((2, 3, 128, 32), f32)
    s5r = np.empty((2, 3, 128, 32 * 128), f32)
    for i, a in enumerate((lre, lim, ldt_full)):
        a4 = a.reshape(2, 32, 2, 64)
        s5p[:, i] = a4.transpose(0, 2, 3, 1).reshape(2, 128, 32)
        s5r[:, i] = np.broadcast_to(a4.reshape(2, 1, 32 * 128), (2, 128, 32 * 128))
    out['s5p'], out['s5r'] = s5p, s5r
    bT = np.zeros((2, 2, 128, 32, 128), f32)
    cP = np.zeros((2, 2, 128, 32, 128), f32)
    for i, (bk, ck) in enumerate((('s5_b_re', 's5_c_re'), ('s5_b_im', 's5_c_im'))):
        bb = g(bk)
        cc = g(ck)
        for gi in range(64):
            gp_, gg = gi // 2, gi % 2
            r0 = (gi % 8) * 16
            bT[:, i, r0:r0 + 16, gp_, gg * 64:(gg + 1) * 64] = bb[:, gi].transpose(0, 2, 1)
            cP[:, i, gg * 64:(gg + 1) * 64, gp_, r0:r0 + 16] = cc[:, gi].transpose(0, 2, 1)
    out['s5bT'] = bT.reshape(2, 2, 128, 32 * 128)
    out['s5cP'] = cP.reshape(2, 2, 128, 32 * 128)
    for k in ('w_in', 'w_branch', 's5_w_glu', 'w_out', 'w_up', 'w_down'):
        out[k] = np.ascontiguousarray(g(k))
    return out


_PROG = {}
_PER_LAYER = ('gpre', 'gffn', 'gpost', 'gfpost', 'mconv', 'mnorm', 'dnorm', 'bif', 'lamT', 'fconv', 'fbias', 's5d',
              's5p', 's5r', 's5bT', 's5cP', 'w_in', 'w_branch', 's5_w_glu', 'w_out', 'w_up', 'w_down')


def _prog(kind):
    if kind not in _PROG:
        _PROG[kind] = Prog(kind)
    return _PROG[kind]


def _launch(kind, per_core, shared):
    prog = _prog(kind)
    in_maps = []
    for c in range(NCORE):
        m = {}
        for k in prog.inp:
            m[k] = per_core[c][k] if k in per_core[c] else shared[k]
        in_maps.append(m)
    res = run_bass_kernel_spmd(prog.nc, in_maps, core_ids=list(range(NCORE)))
    return [{k: np.asarray(res.results[c][k]) for k in prog.outs} for c in range(NCORE)]


def kernel(**inputs):
    lay = host_layout(inputs)
    lami = np.empty((2, 128, 2), np.float32)
    for l in range(2):
        li = 0.8 - 0.6 * math.exp(-0.3 * l)
        lami[l, :, 0] = 1.0 - li
        lami[l, :, 1] = -li
    lay['lami'] = lami
    x = np.asarray(inputs['x'], np.float32)
    prog = _prog('F')
    in_maps = []
    for c in range(NCORE):
        m = {k: lay[k] for k in prog.inp if k != 'x'}
        m['x'] = np.ascontiguousarray(x[NB * c:NB * (c + 1)].reshape(T, D))
        in_maps.append(m)
    res = run_bass_kernel_spmd(prog.nc, in_maps, core_ids=list(range(NCORE)))
    out = np.empty((16, L, D), np.float32)
    for c in range(NCORE):
        out[NB * c:NB * (c + 1)] = np.asarray(res.results[c]['y'], np.float32).reshape(NB, L, D)
    return out
```

```python
import math
from contextlib import ExitStack
import numpy as np
import concourse.bass as bass
import concourse.mybir as mybir
from concourse.bass_utils import run_bass_kernel_spmd

F32 = mybir.dt.float32
BF16 = mybir.dt.bfloat16
AF = mybir.ActivationFunctionType
ALU = mybir.AluOpType

NCORE = 8
DBG_X1 = False
L = 2048
NB = 2
T = NB * L
D = 2048
NIN = 14344
DFF = 5632
EPS = 1e-6
O_QM, O_KM, O_VM, O_OM, O_I, O_F, O_QD, O_KD, O_VD, O_US, O_G = (
    0, 1024, 2048, 3072, 4096, 4100, 4104, 5128, 6152, 7176, 8200)


class Sync:
    def __init__(self, nc):
        self.nc = nc
        self.eng = dict(pe=nc.tensor, act=nc.scalar, dve=nc.vector, pool=nc.gpsimd, sp=nc.sync)
        self.sems, self.tot, self.isdma = {}, {}, {}
        for e in self.eng:
            self._mk('e_' + e, False)
        self.seen = {e: {} for e in self.eng}
        self.st = {}
        self.pe_pending = []
        self.n_ins = 0

    def _mk(self, sid, isdma):
        if sid not in self.sems:
            self.sems[sid] = self.nc.semaphore(sid).__enter__()
            self.tot[sid] = 0
            self.isdma[sid] = isdma
        return self.sems[sid]

    def _deps(self, reads, writes):
        need = {}
        for k in reads:
            s = self.st.get(k)
            if s:
                for sid, v in s[0].items():
                    if need.get(sid, 0) < v:
                        need[sid] = v
        for k in writes:
            s = self.st.get(k)
            if s:
                for d in s:
                    for sid, v in d.items():
                        if need.get(sid, 0) < v:
                            need[sid] = v
        return need

    def _wait(self, e, need):
        for sid, v in need.items():
            if e == 'pe' and sid == 'e_pe':
                continue
            if self.isdma[sid]:
                v = self.tot[sid]
            if self.seen[e].get(sid, 0) >= v:
                continue
            self.eng[e].wait_ge(self.sems[sid], v)
            self.seen[e][sid] = v
            self.n_ins += 1

    def _record(self, reads, writes, sid, v):
        for k in writes:
            self.st[k] = [{sid: v}, {}]
        for k in reads:
            s = self.st.setdefault(k, [{}, {}])
            if s[1].get(sid, 0) < v:
                s[1][sid] = v

    def op(self, e, fn, reads=(), writes=(), last=True):
        self._wait(e, self._deps(reads, writes))
        ins = fn(self.eng[e])
        self.n_ins += 1
        sid = 'e_' + e
        if e == 'pe' and not last:
            self.pe_pending.append((tuple(reads), tuple(writes)))
            return ins
        self.tot[sid] += 1
        ins.then_inc(self.sems[sid], 1)
        v = self.tot[sid]
        if e == 'pe':
            for r, w in self.pe_pending:
                self._record(r, w, sid, v)
            self.pe_pending = []
        self._record(reads, writes, sid, v)
        return ins

    def dma(self, e, out, in_, reads=(), writes=(), sem='d0', **kw):
        self._wait(e, self._deps(reads, writes))
        sid = 'd_' + sem
        S = self._mk(sid, True)
        self.tot[sid] += 16
        self.eng[e].dma_start(out=out, in_=in_, **kw).then_inc(S, 16)
        self.n_ins += 1
        self._record(reads, writes, sid, self.tot[sid])

    def barrier(self):
        assert not self.pe_pending
        for e in self.eng:
            for sid in self.sems:
                v = self.tot[sid]
                if v == 0 or self.seen[e].get(sid, 0) >= v:
                    continue
                self.eng[e].wait_ge(self.sems[sid], v)
                self.seen[e][sid] = v
                self.n_ins += 1
        self.st = {}


class Ring:
    def __init__(self, name, n):
        self.name, self.n, self.i = name, n, -1

    def next(self):
        self.i += 1
        return self.i % self.n


class L1:
    def __init__(self, ap):
        self.ap = ap

    def __getitem__(self, k):
        if isinstance(k, tuple):
            return self.ap[k[1]] if len(k) == 2 else self.ap[k[1:]]
        return self.ap


class Prog:
    def __init__(self, kind):
        self.kind = kind
        nc = self.nc = bass.Bass("TRN2", target_bir_lowering=False)
        self.S = Sync(nc)
        self.inp = {}
        self.outs = []
        self.build()

    def din(self, name, shape, dt=F32):
        self.inp[name] = (shape, dt)
        return self.nc.dram_tensor(name, list(shape), dt, kind="ExternalInput").ap()

    def dscr(self, name, shape, dt, io=None):
        if io == 'in':
            return self.din(name, shape, dt)
        if io == 'out':
            self.outs.append(name)
        kind = "ExternalOutput" if io == 'out' else "Internal"
        return self.nc.dram_tensor(name, list(shape), dt, kind=kind).ap()

    def sb(self, es, name, shape, dt):
        self._uid = getattr(self, '_uid', 0) + 1
        return es.enter_context(self.nc.sbuf_tensor('%s_%d' % (name, self._uid), list(shape), dt))

    def act(self, out, in_, func, reads, writes, **kw):
        self.S.op('act', lambda e: e.activation(out=out, in_=in_, func=func, **kw), reads, writes)

    def mm(self, out, lhsT, rhs, start, stop, reads, writes, last=None):
        if last is None:
            last = stop
        self.S.op('pe', lambda e: e.matmul(out, lhsT, rhs, start=start, stop=stop), reads, writes, last=last)

    def rstd(self, ss, n, key):
        S = self.S
        S.op('dve', lambda e: e.tensor_scalar(ss, ss, 1.0 / n, EPS, ALU.mult, ALU.add), [key], [key])
        self.act(ss, ss, AF.Ln, [key], [key])
        self.act(ss, ss, AF.Exp, [key], [key], scale=-0.5)

    def build(self):
        nc, S = self.nc, self.S
        kd = self.kind
        fused = kd == 'F'
        din = (lambda n, sh, dt=F32: self.din(n, [2] + list(sh), dt)) if fused else (lambda n, sh, dt=F32: L1(self.din(n, sh, dt)))
        scr = self.dscr
        self.cst = self.din('cst', [128, 512])
        if kd in 'AF':
            self.x_in = self.din('x', [T, D])
            self.w_in = din('w_in', [D, NIN])
            self.gpre = din('gpre', [128, 16])
            self.mconv = din('mconv', [128, 16, 4])
            self.bif = din('bif', [1, 8])
            self.WF_in = scr('WF_in', [88, 128, 16 * 128], BF16)
            self.WT_in = scr('WT_in', [6, 128, 16 * 512], BF16)
        if kd in 'BF':
            self.w_glu = din('s5_w_glu', [1024, 2048])
            self.mnorm = din('mnorm', [1, 1024])
            self.dnorm = din('dnorm', [1, 1024])
            self.lamT = din('lamT', [128, 4])
            self.lami = din('lami', [128, 2])
            self.wide = self.din('wide', [128, 4, 1024])
            self.cfar = self.din('cfar', [128, 4])
            self.s5d = din('s5d', [128, 8])
            self.s5p = din('s5p', [3, 128, 32])
            self.s5r = din('s5r', [3, 128, 32 * 128])
            self.s5bT = din('s5bT', [2, 128, 32 * 128])
            self.s5cP = din('s5cP', [2, 128, 32 * 128])
            self.WF_glu = scr('WF_glu', [16, 128, 8 * 128], BF16)
            self.ycs = scr('ycs', [8, 128, T], BF16)
        if kd in 'CF':
            if not fused:
                self.x_in = self.din('x', [T, D])
            self.y_out = scr('y', [T, D], F32, 'out')
            self.xmid = scr('xmid', [T, D], F32)
            self.w_branch = din('w_branch', [3, 1024, D])
            self.w_out = din('w_out', [D, D])
            self.w_up = din('w_up', [D, 2 * DFF])
            self.w_down = din('w_down', [DFF, D])
            self.gffn = din('gffn', [128, 16])
            self.gpost = din('gpost', [1, D])
            self.gfpost = din('gfpost', [1, D])
            self.fconv = din('fconv', [128, 88, 3])
            self.fbias = din('fbias', [128, 88])
            self.WF_br = scr('WF_br', [48, 128, 8 * 128], BF16)
            self.WT_out = scr('WT_out', [4, 128, 16 * 512], BF16)
            self.WF_up = scr('WF_up', [88, 128, 16 * 128], BF16)
            self.WT_dn = scr('WT_dn', [16, 128, 11 * 512], BF16)
            self.x1 = scr('x1', [T, D], F32, 'out' if DBG_X1 else None)
        ab = {'A': 'out', 'B': 'in'}.get(kd)
        if kd in 'ABF':
            self.qkm = scr('qkm', [16, 128, T], BF16, ab)
            self.qkd = scr('qkd', [16, 128, T], BF16, ab)
            self.uT = scr('uT', [8, 128, T], F32, ab)
            self.vm = scr('vm', [T, 1024], BF16, ab)
            self.om = scr('om', [T, 1024], BF16, ab)
            self.vd = scr('vd', [T, 1024], BF16, ab)
            self.lif = scr('lif', [T, 8], F32, ab)
        if kd in 'ACF':
            self.gT = scr('gT', [48, 128, T], BF16, {'A': 'out', 'C': 'in'}.get(kd))
        if kd in 'BCF':
            self.yT = scr('yT', [24, 128, T], BF16, {'B': 'out', 'C': 'in'}.get(kd))

        self.C = nc.alloc_sbuf_tensor('cst_sb', [128, 512], F32)
        self.IDB = nc.alloc_sbuf_tensor('idb', [128, 128], BF16)
        S.dma('sp', self.C[:], self.cst[:, :], writes=['cst'], sem='c')
        S.op('dve', lambda e: e.tensor_copy(self.IDB[:], self.C[:, 0:128]), ['cst'], ['idb'])
        self.ID32 = self.C[:, 0:128]
        self.U = self.C[:, 128:256]
        self.NEGM = self.C[:, 256:384]
        self.ONES = self.C[:, 384:512]
        self.PS = [nc.alloc_psum_tensor('ps%d' % i, [128, 512], F32) for i in range(8)]
        S.barrier()
        for l in range(2 if fused else 1):
            xin = getattr(self, 'x_in', None) if l == 0 else self.xmid
            xout = getattr(self, 'y_out', None) if (l == 1 or not fused) else self.xmid
            self.phase_prep(l)
            if kd in 'AF':
                self.phase_inproj(l, xin)
            if kd in 'BF':
                self.phase_mlstm(l)
                self.phase_attn(l)
                self.phase_s5(l)
            if kd in 'CF':
                self.phase_merge_ffn(l, xin, xout)
        S.barrier()

    def phase_prep(self, l):
        nc, S = self.nc, self.S
        with ExitStack() as es:
            s32 = [self.sb(es, 'p32_%d' % i, [128, 16, 512], F32) for i in range(2)]
            s16 = [self.sb(es, 'p16_%d' % i, [128, 16 * 512], BF16) for i in range(2)]
            gp = self.sb(es, 'pgp', [128, 16], F32)
            gf = self.sb(es, 'pgf', [128, 16], F32)
            if self.kind in 'AF':
                S.dma('sp', gp[:], self.gpre[l], writes=['pgp'], sem='c')
            if self.kind in 'CF':
                S.dma('sp', gf[:], self.gffn[l], writes=['pgf'], sem='c')
            ring = Ring('p', 2)
            cnt = [0]

            def block(W2d, c0, kt0, KT, gain, gkey, form, dst):
                i = ring.next()
                a, b = s32[i], s16[i]
                for q in range(0, KT, 4):
                    n = min(4, KT - q)
                    src = W2d[(kt0 + q) * 128:(kt0 + q + n) * 128, c0:c0 + 512].rearrange(
                        "(kt p) c -> p kt c", p=128)
                    S.dma('sp', a[:, q:q + n, :], src, writes=[('p32', i, q // 4)], sem='p32_%d' % i)
                r32 = [('p32', i, q) for q in range((KT + 3) // 4)]
                if form == 'F':
                    ov = b[:, 0:4 * KT * 128].rearrange("p (nt kt c) -> p kt nt c", nt=4, kt=KT)
                    iv = a[:, 0:KT, :].rearrange("p kt (nt c) -> p kt nt c", c=128)
                else:
                    ov = b[:, 0:KT * 512].rearrange("p (kt c) -> p kt c", kt=KT)
                    iv = a[:, 0:KT, :]
                if gain is not None:
                    for kt in range(KT):
                        cnt[0] += 1
                        if cnt[0] % 2:
                            self.act(ov[:, kt], iv[:, kt], AF.Copy, r32 + [gkey], [('p16', i)],
                                     scale=gain[:, kt0 + kt:kt0 + kt + 1])
                        else:
                            S.op('dve', lambda e, kt=kt: e.tensor_scalar(
                                ov[:, kt], iv[:, kt], gain[:, kt0 + kt:kt0 + kt + 1], None, ALU.mult),
                                r32 + [gkey], [('p16', i)])
                else:
                    h = KT // 2
                    self.act(ov[:, 0:h], iv[:, 0:h], AF.Copy, r32, [('p16', i)])
                    S.op('dve', lambda e: e.tensor_copy(ov[:, h:KT], iv[:, h:KT]), r32, [('p16', i)])
                if form == 'F':
                    n = dst.shape[0]
                    S.dma('act', dst.rearrange("nt p f -> p nt f"),
                          b[:, 0:n * KT * 128].rearrange("p (nt f) -> p nt f", nt=n),
                          reads=[('p16', i)], writes=['wscr'], sem='p16_%d' % i)
                else:
                    S.dma('act', dst, b[:, 0:KT * 512], reads=[('p16', i)], writes=['wscr'], sem='p16_%d' % i)

            if self.kind in 'AF':
                Win = self.w_in[l]
                fcols = ([O_QM + 512 * j for j in range(4)] + [O_QD + 512 * j for j in range(4)] +
                         [O_US + 512 * j for j in range(2)] + [O_G + 512 * j for j in range(12)])
                for j, c0 in enumerate(fcols):
                    block(Win, c0, 0, 16, gp, 'pgp', 'F', self.WF_in[4 * j:4 * j + 4])
                tcols = [O_VM, O_VM + 512, O_OM, O_OM + 512, O_VD, O_VD + 512]
                for j, c0 in enumerate(tcols):
                    block(Win, c0, 0, 16, gp, 'pgp', 'T', self.WT_in[j])
            if self.kind in 'BF':
                for j in range(4):
                    block(self.w_glu[l], 512 * j, 0, 8, None, None, 'F', self.WF_glu[4 * j:4 * j + 4])
            if self.kind in 'CF':
                for n in range(3):
                    for j in range(4):
                        block(self.w_branch[l, n], 512 * j, 0, 8, None, None, 'F',
                              self.WF_br[n * 16 + 4 * j:n * 16 + 4 * j + 4])
                for j in range(4):
                    block(self.w_out[l], 512 * j, 0, 16, None, None, 'T', self.WT_out[j])
                for j in range(22):
                    block(self.w_up[l], 512 * j, 0, 16, gf, 'pgf', 'F', self.WF_up[4 * j:4 * j + 4])
                for j in range(4):
                    for kp in range(4):
                        block(self.w_down[l], 512 * j, kp * 11, 11, None, None, 'T', self.WT_dn[j * 4 + kp])
            S.barrier()

    def norm_T(self, xt, xkey, xn, xnkey, ss, sskey, hT, hkey, tt, psA, psB):
        S = self.S
        S.op('pool', lambda e: e.memset(ss, 0.0), [], [sskey])
        self.act(xn, xt, AF.Square, [xkey], [xnkey, sskey], accum_out=ss)
        self.rstd(ss, D, sskey)
        S.op('dve', lambda e: e.tensor_scalar(xn, xt, ss, None, ALU.mult), [xkey, sskey], [xnkey])
        for g in range(4):
            ps = psA if g % 2 == 0 else psB
            pk = ('ps', ps)
            for j in range(4):
                kt = g * 4 + j
                self.mm(self.PS[ps][:, j * 128:(j + 1) * 128], xn[:, kt * 128:(kt + 1) * 128], self.IDB[:],
                        True, True, [xnkey, 'idb'], [pk], last=(j == 3))
            dst = hT[:, g * 4:g * 4 + 4, tt * 128:(tt + 1) * 128]
            src = self.PS[ps][:, :].rearrange("p (a b) -> p a b", a=4)
            if g % 2 == 0:
                self.act(dst, src, AF.Copy, [pk], [hkey])
            else:
                S.op('dve', lambda e, dst=dst, src=src: e.tensor_copy(dst, src), [pk], [hkey])

    def phase_inproj(self, l, xin):
        nc, S = self.nc, self.S
        with ExitStack() as es:
            sb = lambda n, s, d: self.sb(es, n, s, d)
            hT = [sb('hT%d' % i, [128, 16, 512], BF16) for i in range(2)]
            xt = [sb('xt%d' % i, [128, D], F32) for i in range(2)]
            xn = [sb('xn%d' % i, [128, D], BF16) for i in range(2)]
            ss = [sb('ss%d' % i, [128, 1], F32) for i in range(2)]
            wf = [sb('wf%d' % i, [128, 16, 128], BF16) for i in range(3)]
            wt = [sb('wt%d' % i, [128, 16, 512], BF16) for i in range(2)]
            stg = [sb('stg%d' % i, [128, 512], BF16) for i in range(4)]
            stf = [sb('stf%d' % i, [128, 512], F32) for i in range(2)]
            Ub = [sb('U%d' % i, [128, 515], F32) for i in range(2)]
            acc = [sb('acc%d' % i, [128, 512], F32) for i in range(2)]
            H = sb('halo', [128, 16, 3], F32)
            wc = sb('wc', [128, 16, 4], F32)
            wif32 = sb('wif32', [128, 16, 8], F32)
            wif = sb('wif', [128, 16, 8], BF16)
            gp = sb('gp', [128, 16], F32)
            bifb = sb('bifb', [128, 8], F32)
            lifs = [sb('lifs%d' % i, [128, 8], F32) for i in range(2)]
            S.dma('sp', wc[:], self.mconv[l], writes=['wc'], sem='c')
            S.dma('sp', gp[:], self.gpre[l], writes=['gp'], sem='c')
            S.dma('sp', bifb[:], self.bif[l].partition_broadcast(128), writes=['bifb'], sem='c')
            S.dma('sp', wif32[:], self.w_in[l][:, O_I:O_I + 8].rearrange("(kt p) c -> p kt c", p=128),
                  writes=['wif32'], sem='c', allow_slow_non_contiguous=True)
            for kt in range(16):
                S.op('dve', lambda e, kt=kt: e.tensor_scalar(wif[:, kt], wif32[:, kt], gp[:, kt:kt + 1], None, ALU.mult),
                     ['wif32', 'gp'], ['wif'])
            r_x, r_wf, r_wt, r_stg, r_stf, r_U, r_ps = (Ring('x', 2), Ring('wf', 3), Ring('wt', 2), Ring('stg', 4),
                                                        Ring('stf', 2), Ring('U', 2), Ring('ps', 4))
            cnt = 0
            for tb in range(8):
                hi = tb % 2
                hk = ('hT', hi)
                t0 = tb * 512
                for tt in range(4):
                    i = r_x.next()
                    S.dma('sp', xt[i][:], xin[t0 + tt * 128:t0 + (tt + 1) * 128, :], writes=[('xt', i)], sem='xt%d' % i)
                    self.norm_T(xt[i][:], ('xt', i), xn[i][:], ('xn', i), ss[i][:], ('ss', i), hT[hi], hk, tt, 4, 5)
                if tb % 4 == 0:
                    S.op('pool', lambda e: e.memset(H[:], 0.0), [], ['halo'])
                for nt in range(88):
                    iw = r_wf.next()
                    S.dma('sp', wf[iw][:], self.WF_in[nt].rearrange("p (kt c) -> p kt c", kt=16),
                          reads=['wscr'], writes=[('wf', iw)], sem='wf%d' % iw)
                    ps = r_ps.next()
                    pk = ('ps', ps)
                    for kt in range(16):
                        self.mm(self.PS[ps][:, :], wf[iw][:, kt, :], hT[hi][:, kt, :], kt == 0, kt == 15,
                                [('wf', iw), hk], [pk])
                    P = self.PS[ps][:, :]
                    cnt += 1
                    if nt < 16:
                        iu = r_U.next()
                        uk = ('U', iu)
                        Ut = Ub[iu]
                        self.act(Ut[:, 3:515], P, AF.Copy, [pk], [uk])
                        S.op('pool', lambda e, Ut=Ut, nt=nt: e.tensor_copy(Ut[:, 0:3], H[:, nt, :]), ['halo'], [uk])
                        S.op('pool', lambda e, Ut=Ut, nt=nt: e.tensor_copy(H[:, nt, :], Ut[:, 512:515]), [uk], ['halo'])
                        ak = ('acc', iu)
                        A = acc[iu]
                        S.op('dve', lambda e, Ut=Ut, A=A, nt=nt: e.tensor_scalar(A[:], Ut[:, 3:515], wc[:, nt, 3:4], None, ALU.mult),
                             [uk, 'wc'], [ak])
                        for j in (2, 1, 0):
                            S.op('dve', lambda e, Ut=Ut, A=A, nt=nt, j=j: e.scalar_tensor_tensor(
                                out=A[:], in0=Ut[:, j:j + 512], scalar=wc[:, nt, j:j + 1], in1=A[:],
                                op0=ALU.mult, op1=ALU.add), [uk, 'wc', ak], [ak])
                        ig = r_stg.next()
                        if nt < 8:
                            self.act(stg[ig][:], A[:], AF.Silu, [ak], [('stg', ig)])
                        else:
                            self.act(A[:], A[:], AF.Silu, [ak], [ak])
                            S.op('pool', lambda e, A=A, ig=ig: e.tensor_scalar(stg[ig][:], A[:], 1.0 / 16, None, ALU.mult),
                                 [ak], [('stg', ig)])
                        S.dma('act', self.qkm[nt][:, t0:t0 + 512], stg[ig][:], reads=[('stg', ig)], writes=['qkm'],
                              sem='stg%d' % ig)
                    elif nt < 32:
                        ig = r_stg.next()
                        if cnt % 2:
                            self.act(stg[ig][:], P, AF.Copy, [pk], [('stg', ig)])
                        else:
                            S.op('dve', lambda e, ig=ig, P=P: e.tensor_copy(stg[ig][:], P), [pk], [('stg', ig)])
                        S.dma('act', self.qkd[nt - 16][:, t0:t0 + 512], stg[ig][:], reads=[('stg', ig)], writes=['qkd'],
                              sem='stg%d' % ig)
                    elif nt < 40:
                        ig = r_stf.next()
                        S.op('dve', lambda e, ig=ig, P=P: e.tensor_copy(stf[ig][:], P), [pk], [('stf', ig)])
                        S.dma('act', self.uT[nt - 32][:, t0:t0 + 512], stf[ig][:], reads=[('stf', ig)], writes=['uT'],
                              sem='stf%d' % ig)
                    else:
                        ig = r_stg.next()
                        self.act(stg[ig][:], P, AF.Sigmoid, [pk], [('stg', ig)])
                        S.dma('act', self.gT[nt - 40][:, t0:t0 + 512], stg[ig][:], reads=[('stg', ig)], writes=['gT'],
                              sem='stg%d' % ig)
                for j in range(6):
                    iw = r_wt.next()
                    for q in range(4):
                        S.dma('sp', wt[iw][:, 4 * q:4 * q + 4, :],
                              self.WT_in[j][:, 4 * q * 512:(4 * q + 4) * 512].rearrange("p (kt c) -> p kt c", kt=4),
                              reads=['wscr'], writes=[('wt', iw, q)], sem='wt%d' % iw)
                    wk = [('wt', iw, q) for q in range(4)]
                    dst, c0 = [(self.vm, 0), (self.vm, 512), (self.om, 0), (self.om, 512), (self.vd, 0), (self.vd, 512)][j]
                    for tt in range(4):
                        ps = r_ps.next()
                        pk = ('ps', ps)
                        for kt in range(16):
                            self.mm(self.PS[ps][:, :], hT[hi][:, kt, tt * 128:(tt + 1) * 128], wt[iw][:, kt, :],
                                    kt == 0, kt == 15, wk + [hk], [pk])
                        ig = r_stg.next()
                        P = self.PS[ps][:, :]
                        if j in (2, 3):
                            self.act(stg[ig][:], P, AF.Sigmoid, [pk], [('stg', ig)])
                        else:
                            S.op('dve', lambda e, ig=ig, P=P: e.tensor_copy(stg[ig][:], P), [pk], [('stg', ig)])
                        S.dma('act', dst[t0 + tt * 128:t0 + (tt + 1) * 128, c0:c0 + 512], stg[ig][:],
                              reads=[('stg', ig)], writes=['tok'], sem='stg%d' % ig)
                for tt in range(4):
                    ps = r_ps.next()
                    pk = ('ps', ps)
                    for kt in range(16):
                        self.mm(self.PS[ps][:, 0:8], hT[hi][:, kt, tt * 128:(tt + 1) * 128], wif[:, kt, :],
                                kt == 0, kt == 15, ['wif', hk], [pk])
                    il = tt % 2
                    lk = ('lifs', il)
                    Lt = lifs[il]
                    S.op('dve', lambda e, Lt=Lt, ps=ps: e.tensor_tensor(Lt[:], self.PS[ps][:, 0:8], bifb[:], ALU.add),
                         [pk, 'bifb'], [lk])
                    self.act(Lt[:, 4:8], Lt[:, 4:8], AF.Exp, [lk], [lk], scale=-1.0)
                    self.act(Lt[:, 4:8], Lt[:, 4:8], AF.Ln, [lk], [lk], bias=1.0)
                    S.op('dve', lambda e, Lt=Lt: e.tensor_scalar(Lt[:, 4:8], Lt[:, 4:8], -1.0, None, ALU.mult), [lk], [lk])
                    S.dma('act', self.lif[t0 + tt * 128:t0 + (tt + 1) * 128, :], Lt[:], reads=[lk], writes=['tok'],
                          sem='lifs%d' % il)
            S.barrier()

    def phase_mlstm(self, l):
        nc, S = self.nc, self.S
        PS = self.PS
        with ExitStack() as es:
            sb = lambda n, s, d: self.sb(es, n, s, d)
            qk = [sb('mqk%d' % i, [128, 16, 512], BF16) for i in range(2)]
            va = [sb('mva%d' % i, [128, 4, 257], BF16) for i in range(2)]
            os_ = [sb('mo%d' % i, [128, 1024], BF16) for i in range(2)]
            lf = [sb('mlf%d' % i, [128, 8], F32) for i in range(2)]
            sm = [sb('msm%d' % i, [128, 40], F32) for i in range(2)]
            LFb = sb('mLFb', [128, 4, 128], F32)
            At = sb('mAt', [128, 128], F32)
            Pt = sb('mPt', [128, 128], BF16)
            isb = sb('misb', [128, 257], F32)
            tot = sb('mtot', [128, 257], F32)
            hh = sb('mhh', [128, 256], F32)
            junk = sb('mjunk', [128, 256], BF16)
            s1 = sb('ms1', [128, 4], F32)
            kw = sb('mkw', [128, 256], BF16)
            Cst = sb('mC', [128, 4, 2, 257], F32)
            Cb = sb('mCb', [128, 4, 2, 257], BF16)
            gno = [sb('mgno%d' % i, [128, 1024], F32) for i in range(2)]
            ya = [sb('mya%d' % i, [128, 1024], BF16) for i in range(2)]
            yst = [sb('myst%d' % i, [128, 8, 128], BF16) for i in range(2)]
            mnb = sb('mnb', [128, 1024], F32)
            S.dma('sp', mnb[:], self.mnorm[l].partition_broadcast(128), writes=['mnb'], sem='c')
            for i in range(2):
                S.op('pool', lambda e, i=i: e.memset(va[i][:, :, 256:257], 1.0), [], [('mva', i)])
            ci = -1
            for b in range(NB):
                S.op('pool', lambda e: e.memset(Cst[:], 0.0), [], ['mC'])
                S.op('pool', lambda e: e.memset(Cb[:], 0.0), [], ['mCb'])
                for c in range(16):
                    ci += 1
                    i2 = ci % 2
                    tok0 = b * L + c * 128
                    if c % 4 == 0:
                        iq = (ci // 4) % 2
                        for q in range(4):
                            S.dma('sp', qk[iq][:, 4 * q:4 * q + 4, :],
                                  self.qkm[4 * q:4 * q + 4, :, tok0:tok0 + 512].rearrange("n p t -> p n t"),
                                  reads=['qkm'], writes=[('mqk', iq, q)], sem='mqk%d' % iq)
                    qkk = [('mqk', iq, q) for q in range(4)]
                    co = (c % 4) * 128
                    S.dma('sp', va[i2][:, :, 0:256], self.vm[tok0:tok0 + 128, :].rearrange("t (h d) -> t h d", h=4),
                          reads=['tok'], writes=[('mva', i2)], sem='mva%d' % i2)
                    S.dma('sp', os_[i2][:], self.om[tok0:tok0 + 128, :], reads=['tok'], writes=[('mo', i2)], sem='mo%d' % i2)
                    S.dma('sp', lf[i2][:], self.lif[tok0:tok0 + 128, :], reads=['tok'], writes=[('mlf', i2)], sem='mlf%d' % i2)
                    lfk, smk, vak = ('mlf', i2), ('msm', i2), ('mva', i2)
                    LF, SM = lf[i2], sm[i2]
                    self.mm(PS[7][:, 0:4], self.U, LF[:, 4:8], True, True, ['cst', lfk], [('ps', 7)], last=False)
                    self.mm(PS[7][:, 4:8], self.ONES, LF[:, 4:8], True, True, ['cst', lfk], [('ps', 7)])
                    self.act(SM[:, 0:8], PS[7][:, 0:8], AF.Copy, [('ps', 7)], [smk])
                    self.act(SM[:, 8:16], SM[:, 0:8], AF.Exp, [smk], [smk])
                    S.op('dve', lambda e, SM=SM: e.tensor_tensor(SM[:, 16:20], SM[:, 4:8], SM[:, 0:4], ALU.subtract), [smk], [smk])
                    S.op('dve', lambda e, SM=SM, LF=LF: e.tensor_tensor(SM[:, 16:20], SM[:, 16:20], LF[:, 0:4], ALU.add), [smk, lfk], [smk])
                    self.act(SM[:, 16:20], SM[:, 16:20], AF.Exp, [smk], [smk])
                    S.op('dve', lambda e, SM=SM, LF=LF: e.tensor_tensor(SM[:, 20:24], LF[:, 0:4], SM[:, 0:4], ALU.subtract), [smk, lfk], [smk])
                    for h in range(4):
                        S.op('pool', lambda e, h=h, LF=LF: e.tensor_scalar(LFb[:, h, :], self.ONES, LF[:, 4 + h:5 + h], None, ALU.mult),
                             ['cst', lfk], ['mLFb'])
                    S.op('pool', lambda e, i2=i2: e.tensor_tensor(gno[i2][:], os_[i2][:], mnb[:], ALU.mult),
                         [('mo', i2), 'mnb'], [('mgno', i2)])
                    for h in range(4):
                        qT = [qk[iq][:, 2 * h + j, co:co + 128] for j in range(2)]
                        kT = [qk[iq][:, 8 + 2 * h + j, co:co + 128] for j in range(2)]
                        self.mm(PS[0][:, 0:128], LFb[:, h, :], self.U, True, False, ['mLFb', 'cst'], [('ps', 0)], last=False)
                        self.mm(PS[0][:, 0:128], self.ID32, self.NEGM, False, True, ['cst'], [('ps', 0)])
                        self.act(At[:], PS[0][:, 0:128], AF.Exp, [('ps', 0), smk], ['mAt'], bias=SM[:, 20 + h:21 + h])
                        for j in range(2):
                            self.mm(PS[1][:, 0:128], kT[j], qT[j], j == 0, j == 1, qkk, [('ps', 1)])
                        S.op('dve', lambda e: e.tensor_tensor(Pt[:], At[:], PS[1][:, 0:128], ALU.mult), ['mAt', ('ps', 1)], ['mPt'])
                        self.mm(PS[2][:, 0:257], Pt[:], va[i2][:, h, :], True, True, ['mPt', vak], [('ps', 2)])
                        for j in range(2):
                            self.mm(PS[3][:, 0:257], qT[j], Cb[:, h, j, :], j == 0, j == 1, qkk + ['mCb'], [('ps', 3)])
                        self.act(isb[:], PS[3][:, 0:257], AF.Copy, [('ps', 3), smk], ['misb'], scale=SM[:, 8 + h:9 + h])
                        S.op('dve', lambda e: e.tensor_tensor(tot[:], isb[:], PS[2][:, 0:257], ALU.add), ['misb', ('ps', 2)], ['mtot'])
                        self.act(s1[:, 0:1], tot[:, 256:257], AF.Abs, ['mtot'], ['ms1'])
                        S.op('dve', lambda e: e.tensor_scalar_max(s1[:, 0:1], s1[:, 0:1], 1.0), ['ms1'], ['ms1'])
                        S.op('dve', lambda e: e.reciprocal(s1[:, 0:1], s1[:, 0:1]), ['ms1'], ['ms1'])
                        S.op('dve', lambda e: e.tensor_scalar(hh[:], tot[:, 0:256], s1[:, 0:1], None, ALU.mult), ['mtot', 'ms1'], ['mhh'])
                        S.op('pool', lambda e: e.memset(s1[:, 1:2], 0.0), [], ['ms1b'])
                        self.act(junk[:], hh[:], AF.Square, ['mhh'], ['mjunk', 'ms1b'], accum_out=s1[:, 1:2])
                        self.rstd(s1[:, 1:2], 256, 'ms1b')
                        S.op('dve', lambda e, h=h, i2=i2: e.scalar_tensor_tensor(
                            out=ya[i2][:, h * 256:(h + 1) * 256], in0=hh[:], scalar=s1[:, 1:2],
                            in1=gno[i2][:, h * 256:(h + 1) * 256], op0=ALU.mult, op1=ALU.mult),
                            ['mhh', 'ms1b', ('mgno', i2)], [('mya', i2)])
                        for j in range(2):
                            self.mm(PS[4][:, j * 128:(j + 1) * 128], kT[j], self.IDB[:], True, True, qkk + ['idb'], [('ps', 4)],
                                    last=(j == 1))
                        self.act(kw[:], PS[4][:, 0:256], AF.Copy, [('ps', 4), smk], ['mkw'], scale=SM[:, 16 + h:17 + h])
                        for j in range(2):
                            self.mm(PS[5 + j][:, 0:257], kw[:, j * 128:(j + 1) * 128], va[i2][:, h, :], True, True,
                                    ['mkw', vak], [('ps', 5 + j)])
                            S.op('dve', lambda e, h=h, j=j, SM=SM: e.scalar_tensor_tensor(
                                out=Cst[:, h, j, :], in0=Cst[:, h, j, :], scalar=SM[:, 12 + h:13 + h], in1=PS[5 + j][:, 0:257],
                                op0=ALU.mult, op1=ALU.add), ['mC', smk, ('ps', 5 + j)], ['mC'])
                            S.op('pool', lambda e, h=h, j=j: e.tensor_copy(Cb[:, h, j, :], Cst[:, h, j, :]), ['mC'], ['mCb'])
                    for g in range(2):
                        for j in range(4):
                            self.mm(PS[7][:, j * 128:(j + 1) * 128], ya[i2][:, (g * 4 + j) * 128:(g * 4 + j + 1) * 128], self.IDB[:],
                                    True, True, [('mya', i2), 'idb'], [('ps', 7)], last=(j == 3))
                        self.act(yst[i2][:, g * 4:g * 4 + 4, :], PS[7][:, :].rearrange("p (a b) -> p a b", a=4), AF.Copy,
                                 [('ps', 7)], [('myst', i2)])
                    S.dma('act', self.yT[0:8, :, tok0:tok0 + 128].rearrange("n p t -> p n t"), yst[i2][:],
                          reads=[('myst', i2)], writes=['yT'], sem='myst%d' % i2)
            S.barrier()

    def phase_attn(self, l):
        nc, S = self.nc, self.S
        PS = self.PS
        scale = 128 ** -0.5
        with ExitStack() as es:
            sb = lambda n, s, d: self.sb(es, n, s, d)
            wide = sb('awide', [128, 4, 1024], F32)
            cfar = sb('acfar', [128, 4], F32)
            lamT = sb('alamT', [128, 4], F32)
            lm = sb('alm', [128, 4], F32)
            dnb = sb('adnb', [128, 1024], F32)
            kseq = [sb('akseq%d' % i, [128, L], BF16) for i in range(2)]
            qblk = [sb('aqblk%d' % i, [128, 512], BF16) for i in range(2)]
            vaug = sb('avaug', [128, 16, 257], BF16)
            tmp = [sb('atmp%d' % i, [128, 512], F32) for i in range(2)]
            pT = [sb('apT%d' % i, [128, 512], BF16) for i in range(3)]
            s4 = sb('as4', [128, 4], F32)
            o0 = sb('ao0', [128, 4, 256], F32)
            oo = sb('aoo', [128, 256], F32)
            junk = sb('ajunk', [128, 256], BF16)
            yb = sb('ayb', [128, 256], BF16)
            yst = [sb('ayst%d' % i, [128, 2, 128], BF16) for i in range(2)]
            S.dma('sp', wide[:], self.wide[:, :, :], writes=['awide'], sem='c')
            S.dma('sp', cfar[:], self.cfar[:, :], writes=['acfar'], sem='c')
            S.dma('sp', lamT[:], self.lamT[l], writes=['alamT'], sem='c')
            S.dma('sp', dnb[:], self.dnorm[l].partition_broadcast(128), writes=['adnb'], sem='c')
            lami = sb('alami', [128, 2], F32)
            S.dma('sp', lami[:], self.lami[l], writes=['alami'], sem='c')
            S.op('pool', lambda e: e.tensor_scalar(dnb[:], dnb[:], lami[:, 0:1], None, ALU.mult), ['adnb', 'alami'], ['adnb'])
            S.op('dve', lambda e: e.tensor_tensor(lm[:, 0:1], lamT[:, 0:1], lamT[:, 1:2], ALU.mult), ['alamT'], ['alm'])
            S.op('dve', lambda e: e.tensor_tensor(lm[:, 1:2], lamT[:, 2:3], lamT[:, 3:4], ALU.mult), ['alamT', 'alm'], ['alm'])
            self.mm(PS[7][:, 0:2], self.ONES, lm[:, 0:2], True, True, ['cst', 'alm'], [('ps', 7)])
            self.act(lm[:, 2:4], PS[7][:, 0:2], AF.Exp, [('ps', 7)], ['alm'])
            S.op('dve', lambda e: e.tensor_tensor(lm[:, 0:1], lm[:, 3:4], lm[:, 2:3], ALU.subtract), ['alm'], ['alm'])
            S.op('dve', lambda e: e.tensor_scalar(lm[:, 0:1], lm[:, 0:1], lami[:, 1:2], None, ALU.add), ['alm', 'alami'], ['alm'])
            S.op('pool', lambda e: e.memset(vaug[:, :, 256:257], 1.0), [], ['avaug'])
            r_q, r_tmp, r_pT, r_st, r_y = Ring('q', 2), Ring('tmp', 2), Ring('pT', 3), Ring('st', 2), Ring('y', 2)
            for b in range(NB):
                for h in range(4):
                    S.dma('sp', vaug[:, :, 0:256],
                          self.vd[b * L:(b + 1) * L, h * 256:(h + 1) * 256].rearrange("(kt p) d -> p kt d", p=128),
                          reads=['tok'], writes=['avaug'], sem='avaug')
                    for m in range(2):
                        S.dma('sp', kseq[m][:], self.qkd[8 + 2 * h + m][:, b * L:(b + 1) * L], reads=['qkd'],
                              writes=[('akseq', m)], sem='akseq%d' % m)
                    for I in range(4):
                        for m in range(2):
                            iq = r_q.next()
                            S.dma('sp', qblk[iq][:], self.qkd[2 * h + m][:, b * L + I * 512:b * L + (I + 1) * 512],
                                  reads=['qkd'], writes=[('aqblk', iq)], sem='aqblk%d' % iq)
                            for j in range(4 * I + 4):
                                ist = r_st.next()
                                pk = ('ps', 4 + ist)
                                self.mm(PS[4 + ist][:, :], kseq[m][:, j * 128:(j + 1) * 128], qblk[iq][:], True, True,
                                        [('akseq', m), ('aqblk', iq)], [pk])
                                ip = r_pT.next()
                                ppk = ('apT', ip)
                                if j <= 4 * I - 2:
                                    self.act(pT[ip][:], PS[4 + ist][:, :], AF.Exp, [pk, 'acfar'], [ppk], scale=scale,
                                             bias=cfar[:, h:h + 1])
                                else:
                                    x0 = (4 * I - j) * 128 + 384
                                    it = r_tmp.next()
                                    S.op('dve', lambda e, it=it, ist=ist, x0=x0, h=h: e.scalar_tensor_tensor(
                                        out=tmp[it][:], in0=PS[4 + ist][:, :], scalar=scale, in1=wide[:, h, x0:x0 + 512],
                                        op0=ALU.mult, op1=ALU.add), [pk, 'awide'], [('atmp', it)])
                                    self.act(pT[ip][:], tmp[it][:], AF.Exp, [('atmp', it)], [ppk])
                                for qi in range(4):
                                    if 4 * I + qi < j:
                                        continue
                                    self.mm(PS[qi][:, 0:257], pT[ip][:, qi * 128:(qi + 1) * 128], vaug[:, j, :],
                                            j == 0, j == 4 * I + qi, [ppk, 'avaug'], [('ps', qi)])
                            for qi in range(4):
                                acc = PS[qi][:, 0:257]
                                ak_ = ('ps', qi)
                                S.op('dve', lambda e, acc=acc: e.reciprocal(s4[:, 0:1], acc[:, 256:257]), [ak_], ['as4'])
                                if m == 0:
                                    self.act(o0[:, qi, :], acc[:, 0:256], AF.Copy, [ak_, 'as4'], [('ao0', qi)], scale=s4[:, 0:1])
                                    continue
                                S.op('dve', lambda e: e.tensor_tensor(s4[:, 1:2], s4[:, 0:1], lm[:, 0:1], ALU.mult), ['as4', 'alm'], ['as4'])
                                S.op('dve', lambda e, acc=acc, qi=qi: e.scalar_tensor_tensor(
                                    out=oo[:], in0=acc[:, 0:256], scalar=s4[:, 1:2], in1=o0[:, qi, :], op0=ALU.mult, op1=ALU.add),
                                    [ak_, 'as4', ('ao0', qi)], ['aoo'])
                                S.op('pool', lambda e: e.memset(s4[:, 2:3], 0.0), [], ['as4b'])
                                self.act(junk[:], oo[:], AF.Square, ['aoo'], ['ajunk', 'as4b'], accum_out=s4[:, 2:3])
                                self.rstd(s4[:, 2:3], 256, 'as4b')
                                S.op('dve', lambda e, h=h: e.scalar_tensor_tensor(out=yb[:], in0=oo[:], scalar=s4[:, 2:3],
                                                                                  in1=dnb[:, h * 256:(h + 1) * 256],
                                                                                  op0=ALU.mult, op1=ALU.mult),
                                     ['aoo', 'as4b', 'adnb'], ['ayb'])
                                iy = r_y.next()
                                for j in range(2):
                                    self.mm(PS[7][:, j * 128:(j + 1) * 128], yb[:, j * 128:(j + 1) * 128], self.IDB[:], True, True,
                                            ['ayb', 'idb'], [('ps', 7)], last=(j == 1))
                                self.act(yst[iy][:], PS[7][:, 0:256].rearrange("p (a b) -> p a b", a=2), AF.Copy, [('ps', 7)],
                                         [('ayst', iy)])
                                tok0 = b * L + I * 512 + qi * 128
                                S.dma('act', self.yT[8 + 2 * h:8 + 2 * h + 2, :, tok0:tok0 + 128].rearrange("n p t -> p n t"),
                                      yst[iy][:], reads=[('ayst', iy)], writes=['yT'], sem='ayst%d' % iy)
            S.barrier()

    def phase_s5(self, l):
        nc, S = self.nc, self.S
        PS = self.PS
        with ExitStack() as es:
            sb = lambda n, s, d: self.sb(es, n, s, d)
            bbr = sb('sbbr', [128, 32, 128], F32)
            bbi = sb('sbbi', [128, 32, 128], F32)
            cpr = sb('scpr', [128, 32, 128], F32)
            cpi = sb('scpi', [128, 32, 128], F32)
            pw = sb('spw', [128, 12, 2, 32], F32)
            d8 = sb('sd8', [128, 8], F32)
            S.dma('sp', d8[:], self.s5d[l], writes=['sd8'], sem='c')
            def zcalc(es2, tag, n, srcs, want_z):
                sb2 = lambda nm, s_, d_: self.sb(es2, nm, s_, d_)
                lr, li_, ld = (sb2('sz%s%d' % (tag, i), [128, n], F32) for i in range(3))
                t = [sb2('szt%s%d' % (tag, i), [128, n], F32) for i in range(6)]
                k = 'sz' + tag
                for i, dst in enumerate((lr, li_, ld)):
                    S.dma('sp', dst[:], srcs[i], writes=[k], sem='c')
                V = lambda fn: S.op('dve', fn, [k], [k])
                A = lambda out, in_, f, **kw: self.act(out, in_, f, [k], [k], **kw)
                A(ld[:], ld[:], AF.Exp)
                V(lambda e: e.tensor_tensor(t[0][:], lr[:], ld[:], ALU.mult))
                A(t[0][:], t[0][:], AF.Exp, scale=1.0 / 32)
                V(lambda e: e.tensor_tensor(t[1][:], li_[:], ld[:], ALU.mult))
                A(t[2][:], t[1][:], AF.Sin, scale=1.0 / 32)
                V(lambda e: e.tensor_scalar(t[1][:], t[1][:], 1.0 / 32, math.pi / 2, ALU.mult, ALU.add))
                A(t[1][:], t[1][:], AF.Sin)
                V(lambda e: e.tensor_tensor(t[1][:], t[1][:], t[0][:], ALU.mult))
                V(lambda e: e.tensor_tensor(t[2][:], t[2][:], t[0][:], ALU.mult))
                for _ in range(5):
                    V(lambda e: e.tensor_tensor(t[3][:], t[1][:], t[1][:], ALU.mult))
                    V(lambda e: e.tensor_tensor(t[4][:], t[2][:], t[2][:], ALU.mult))
                    V(lambda e: e.tensor_tensor(t[2][:], t[1][:], t[2][:], ALU.mult))
                    V(lambda e: e.tensor_scalar(t[2][:], t[2][:], 2.0, None, ALU.mult))
                    V(lambda e: e.tensor_tensor(t[1][:], t[3][:], t[4][:], ALU.subtract))
                are, aim = t[1], t[2]
                if not want_z:
                    return are, aim, k
                V(lambda e: e.tensor_tensor(t[3][:], lr[:], lr[:], ALU.mult))
                V(lambda e: e.tensor_tensor(t[4][:], li_[:], li_[:], ALU.mult))
                V(lambda e: e.tensor_tensor(t[3][:], t[3][:], t[4][:], ALU.add))
                V(lambda e: e.reciprocal(t[3][:], t[3][:]))
                V(lambda e: e.tensor_scalar(t[0][:], are[:], -1.0, None, ALU.add))
                V(lambda e: e.tensor_tensor(t[4][:], t[0][:], lr[:], ALU.mult))
                V(lambda e: e.tensor_tensor(t[5][:], aim[:], li_[:], ALU.mult))
                V(lambda e: e.tensor_tensor(t[4][:], t[4][:], t[5][:], ALU.add))
                V(lambda e: e.tensor_tensor(t[4][:], t[4][:], t[3][:], ALU.mult))
                V(lambda e: e.tensor_tensor(t[5][:], aim[:], lr[:], ALU.mult))
                V(lambda e: e.tensor_tensor(t[0][:], t[0][:], li_[:], ALU.mult))
                V(lambda e: e.tensor_tensor(t[5][:], t[5][:], t[0][:], ALU.subtract))
                V(lambda e: e.tensor_tensor(t[5][:], t[5][:], t[3][:], ALU.mult))
                return t[4], t[5], k

            bbrf = bbr[:].rearrange("p a b -> p (a b)")
            bbif = bbi[:].rearrange("p a b -> p (a b)")
            for q in range(4):
                cs = slice(q * 1024, (q + 1) * 1024)
                with ExitStack() as es2:
                    zre, zim, zk = zcalc(es2, 'r', 1024, [self.s5r[l, i][:, cs] for i in range(3)], True)
                    bT = [self.sb(es2, 'sbT%d' % i, [128, 1024], F32) for i in range(2)]
                    t6 = self.sb(es2, 'st6', [128, 1024], F32)
                    for i in range(2):
                        S.dma('sp', bT[i][:], self.s5bT[l, i][:, cs], writes=['sbT'], sem='c')
                    V = lambda fn, w: S.op('dve', fn, [zk, 'sbT', 'st6', 'sbb'], w)
                    V(lambda e: e.tensor_tensor(bbrf[:, cs], zre[:], bT[0][:], ALU.mult), ['sbb'])
                    V(lambda e: e.tensor_tensor(t6[:], zim[:], bT[1][:], ALU.mult), ['st6'])
                    V(lambda e: e.tensor_tensor(bbrf[:, cs], bbrf[:, cs], t6[:], ALU.subtract), ['sbb'])
                    V(lambda e: e.tensor_tensor(bbif[:, cs], zre[:], bT[1][:], ALU.mult), ['sbb'])
                    V(lambda e: e.tensor_tensor(t6[:], zim[:], bT[0][:], ALU.mult), ['st6'])
                    V(lambda e: e.tensor_tensor(bbif[:, cs], bbif[:, cs], t6[:], ALU.add), ['sbb'])
                    S.barrier()
            with ExitStack() as es2:
                are, aim, ak = zcalc(es2, 'p', 32, [self.s5p[l, i] for i in range(3)], False)
                S.op('dve', lambda e: e.tensor_copy(pw[:, 0, 0, :], are[:]), [ak], ['spw'])
                S.op('dve', lambda e: e.tensor_copy(pw[:, 0, 1, :], aim[:]), [ak], ['spw'])
                tq = self.sb(es2, 'stq', [128, 2, 32], F32)
                for k in range(1, 11):
                    V = lambda fn, w: S.op('dve', fn, ['spw', 'stq'], w)
                    V(lambda e, k=k: e.tensor_tensor(tq[:, 0, :], pw[:, k - 1, 0, :], pw[:, k - 1, 0, :], ALU.mult), ['stq'])
                    V(lambda e, k=k: e.tensor_tensor(tq[:, 1, :], pw[:, k - 1, 1, :], pw[:, k - 1, 1, :], ALU.mult), ['stq'])
                    V(lambda e, k=k: e.tensor_tensor(pw[:, k, 0, :], tq[:, 0, :], tq[:, 1, :], ALU.subtract), ['spw'])
                    V(lambda e, k=k: e.tensor_tensor(tq[:, 0, :], pw[:, k - 1, 0, :], pw[:, k - 1, 1, :], ALU.mult), ['stq'])
                    V(lambda e, k=k: e.tensor_scalar(pw[:, k, 1, :], tq[:, 0, :], 2.0, None, ALU.mult), ['spw'])
                S.dma('sp', cpr[:].rearrange("p a b -> p (a b)"), self.s5cP[l, 0], writes=['scp'], sem='c')
                S.dma('sp', cpi[:].rearrange("p a b -> p (a b)"), self.s5cP[l, 1], writes=['scp'], sem='c')
                S.op('pool', lambda e: e.tensor_scalar(cpi[:], cpi[:], -1.0, None, ALU.mult), ['scp'], ['scp'])
                S.barrier()
            pj = sb('spj', [128, 16, 3, 32], F32)
            S.op('dve', lambda e: e.tensor_copy(pj[:, 0, 0:2, :], pw[:, 0, :, :]), ['spw'], ['spj'])
            with ExitStack() as es2:
                tj = self.sb(es2, 'stj', [128, 2, 32], F32)
                for j in range(1, 16):
                    V = lambda fn, w: S.op('dve', fn, ['spw', 'spj', 'stj'], w)
                    V(lambda e, j=j: e.tensor_tensor(tj[:, 0, :], pj[:, j - 1, 0, :], pw[:, 0, 0, :], ALU.mult), ['stj'])
                    V(lambda e, j=j: e.tensor_tensor(tj[:, 1, :], pj[:, j - 1, 1, :], pw[:, 0, 1, :], ALU.mult), ['stj'])
                    V(lambda e, j=j: e.tensor_tensor(pj[:, j, 0, :], tj[:, 0, :], tj[:, 1, :], ALU.subtract), ['spj'])
                    V(lambda e, j=j: e.tensor_tensor(tj[:, 0, :], pj[:, j - 1, 0, :], pw[:, 0, 1, :], ALU.mult), ['stj'])
                    V(lambda e, j=j: e.tensor_tensor(tj[:, 1, :], pj[:, j - 1, 1, :], pw[:, 0, 0, :], ALU.mult), ['stj'])
                    V(lambda e, j=j: e.tensor_tensor(pj[:, j, 1, :], tj[:, 0, :], tj[:, 1, :], ALU.add), ['spj'])
                S.op('dve', lambda e: e.tensor_scalar(pj[:, :, 2, :], pj[:, :, 1, :], -1.0, None, ALU.mult), ['spj'], ['spj'])
                S.barrier()
            ER = [sb('sER%d' % i, [128, NB, 128], F32) for i in range(2)]
            EI = [sb('sEI%d' % i, [128, NB, 128], F32) for i in range(2)]
            npw = sb('snpw', [128, 11, 32], F32)
            S.op('dve', lambda e: e.tensor_scalar(npw[:], pw[:, 0:11, 1, :], -1.0, None, ALU.mult), ['spw'], ['snpw'])
            ut = sb('sut', [128, NB, L], F32)
            XR = [sb('sXR%d' % i, [128, NB, L], F32) for i in range(2)]
            XI = [sb('sXI%d' % i, [128, NB, L], F32) for i in range(2)]
            Yacc = sb('sYacc', [128, NB, L], F32)
            yg = sb('syg', [128, NB, L], BF16)
            for ct in range(8):
                S.dma('sp', ut[:, 0, :], self.uT[ct][:, 0:L], reads=['uT'], writes=['sut'], sem='sut')
                S.dma('sp', ut[:, 1, :], self.uT[ct][:, L:2 * L], reads=['uT'], writes=['sut'], sem='sut')
                for g4 in range(4):
                    gp_ = ct * 4 + g4
                    n = 0
                    for b in range(NB):
                        for tb in range(4):
                            for ri, (bb, X) in enumerate(((bbr, XR[0]), (bbi, XI[0]))):
                                bank = n % 4
                                n += 1
                                self.mm(PS[bank][:, :], bb[:, gp_, :], ut[:, b, tb * 512:(tb + 1) * 512], True, True,
                                        ['sbb', 'sut'], [('ps', bank)])
                                key = ('sX', ri, 0)
                                if n % 2:
                                    self.act(X[:, b, tb * 512:(tb + 1) * 512], PS[bank][:, :], AF.Copy, [('ps', bank)], [key])
                                else:
                                    S.op('dve', lambda e, X=X, b=b, tb=tb, bank=bank: e.tensor_copy(
                                        X[:, b, tb * 512:(tb + 1) * 512], PS[bank][:, :]), [('ps', bank)], [key])
                    cur = 0
                    bv = lambda X: X[:].rearrange("p b (n j) -> p b n j", j=16)
                    for k in range(4):
                        d = 1 << k
                        nx = 1 - cur
                        pr, pi_, npi = pw[:, k, 0, gp_:gp_ + 1], pw[:, k, 1, gp_:gp_ + 1], npw[:, k, gp_:gp_ + 1]
                        R0, I0, R1, I1 = bv(XR[cur]), bv(XI[cur]), bv(XR[nx]), bv(XI[nx])
                        kr0, ki0, kr1, ki1 = ('sX', 0, cur), ('sX', 1, cur), ('sX', 0, nx), ('sX', 1, nx)
                        S.op('dve', lambda e, R0=R0, R1=R1, d=d, pr=pr: e.scalar_tensor_tensor(
                            out=R1[:, :, :, d:], in0=R0[:, :, :, 0:16 - d], scalar=pr, in1=R0[:, :, :, d:], op0=ALU.mult, op1=ALU.add),
                            [kr0, 'spw'], [kr1])
                        S.op('dve', lambda e, I0=I0, R1=R1, d=d, npi=npi: e.scalar_tensor_tensor(
                            out=R1[:, :, :, d:], in0=I0[:, :, :, 0:16 - d], scalar=npi, in1=R1[:, :, :, d:], op0=ALU.mult, op1=ALU.add),
                            [ki0, 'snpw', kr1], [kr1])
                        self.act(R1[:, :, :, 0:d], R0[:, :, :, 0:d], AF.Copy, [kr0], [kr1])
                        S.op('dve', lambda e, I0=I0, I1=I1, d=d, pr=pr: e.scalar_tensor_tensor(
                            out=I1[:, :, :, d:], in0=I0[:, :, :, 0:16 - d], scalar=pr, in1=I0[:, :, :, d:], op0=ALU.mult, op1=ALU.add),
                            [ki0, 'spw'], [ki1])
                        S.op('dve', lambda e, R0=R0, I1=I1, d=d, pi_=pi_: e.scalar_tensor_tensor(
                            out=I1[:, :, :, d:], in0=R0[:, :, :, 0:16 - d], scalar=pi_, in1=I1[:, :, :, d:], op0=ALU.mult, op1=ALU.add),
                            [kr0, 'spw', ki1], [ki1])
                        self.act(I1[:, :, :, 0:d], I0[:, :, :, 0:d], AF.Copy, [ki0], [ki1])
                        cur = nx
                    XRv, XIv = bv(XR[cur]), bv(XI[cur])
                    kxr, kxi = ('sX', 0, cur), ('sX', 1, cur)
                    self.act(ER[0][:], XRv[:, :, :, 15], AF.Copy, [kxr], [('sE', 0, 0)])
                    self.act(EI[0][:], XIv[:, :, :, 15], AF.Copy, [kxi], [('sE', 1, 0)])
                    ec = 0
                    for k2 in range(7):
                        d = 1 << k2
                        k = 4 + k2
                        nx = 1 - ec
                        pr, pi_, npi = pw[:, k, 0, gp_:gp_ + 1], pw[:, k, 1, gp_:gp_ + 1], npw[:, k, gp_:gp_ + 1]
                        R0, I0, R1, I1 = ER[ec], EI[ec], ER[nx], EI[nx]
                        kr0, ki0, kr1, ki1 = ('sE', 0, ec), ('sE', 1, ec), ('sE', 0, nx), ('sE', 1, nx)
                        S.op('dve', lambda e, R0=R0, R1=R1, d=d, pr=pr: e.scalar_tensor_tensor(
                            out=R1[:, :, d:], in0=R0[:, :, 0:128 - d], scalar=pr, in1=R0[:, :, d:], op0=ALU.mult, op1=ALU.add),
                            [kr0, 'spw'], [kr1])
                        S.op('dve', lambda e, I0=I0, R1=R1, d=d, npi=npi: e.scalar_tensor_tensor(
                            out=R1[:, :, d:], in0=I0[:, :, 0:128 - d], scalar=npi, in1=R1[:, :, d:], op0=ALU.mult, op1=ALU.add),
                            [ki0, 'snpw', kr1], [kr1])
                        self.act(R1[:, :, 0:d], R0[:, :, 0:d], AF.Copy, [kr0], [kr1])
                        S.op('dve', lambda e, I0=I0, I1=I1, d=d, pr=pr: e.scalar_tensor_tensor(
                            out=I1[:, :, d:], in0=I0[:, :, 0:128 - d], scalar=pr, in1=I0[:, :, d:], op0=ALU.mult, op1=ALU.add),
                            [ki0, 'spw'], [ki1])
                        S.op('dve', lambda e, R0=R0, I1=I1, d=d, pi_=pi_: e.scalar_tensor_tensor(
                            out=I1[:, :, d:], in0=R0[:, :, 0:128 - d], scalar=pi_, in1=I1[:, :, d:], op0=ALU.mult, op1=ALU.add),
                            [kr0, 'spw', ki1], [ki1])
                        self.act(I1[:, :, 0:d], I0[:, :, 0:d], AF.Copy, [ki0], [ki1])
                        ec = nx
                    FR, FI = ER[ec][:, :, 0:127], EI[ec][:, :, 0:127]
                    kfr, kfi = ('sE', 0, ec), ('sE', 1, ec)
                    for j in range(16):
                        ar, ai, nai = pj[:, j, 0, gp_:gp_ + 1], pj[:, j, 1, gp_:gp_ + 1], pj[:, j, 2, gp_:gp_ + 1]
                        xr, xi = XRv[:, :, 1:, j], XIv[:, :, 1:, j]
                        S.op('dve', lambda e, xr=xr, ar=ar: e.scalar_tensor_tensor(out=xr, in0=FR, scalar=ar, in1=xr, op0=ALU.mult, op1=ALU.add),
                             [kfr, 'spj', kxr], [kxr])
                        S.op('dve', lambda e, xr=xr, nai=nai: e.scalar_tensor_tensor(out=xr, in0=FI, scalar=nai, in1=xr, op0=ALU.mult, op1=ALU.add),
                             [kfi, 'spj', kxr], [kxr])
                        S.op('dve', lambda e, xi=xi, ar=ar: e.scalar_tensor_tensor(out=xi, in0=FI, scalar=ar, in1=xi, op0=ALU.mult, op1=ALU.add),
                             [kfi, 'spj', kxi], [kxi])
                        S.op('dve', lambda e, xi=xi, ai=ai: e.scalar_tensor_tensor(out=xi, in0=FR, scalar=ai, in1=xi, op0=ALU.mult, op1=ALU.add),
                             [kfr, 'spj', kxi], [kxi])
                    for b in range(NB):
                        for tb in range(4):
                            bank = 4 + (tb % 2)
                            sl = slice(tb * 512, (tb + 1) * 512)
                            self.mm(PS[bank][:, :], cpr[:, gp_, :], XR[cur][:, b, sl], True, False, ['scp', ('sX', 0, cur)],
                                    [('ps', bank)], last=False)
                            self.mm(PS[bank][:, :], cpi[:, gp_, :], XI[cur][:, b, sl], False, True, ['scp', ('sX', 1, cur)],
                                    [('ps', bank)])
                            if g4 == 0:
                                self.act(Yacc[:, b, sl], PS[bank][:, :], AF.Copy, [('ps', bank)], ['sYacc'])
                            else:
                                S.op('dve', lambda e, b=b, sl=sl, bank=bank: e.tensor_tensor(
                                    Yacc[:, b, sl], Yacc[:, b, sl], PS[bank][:, :], ALU.add), [('ps', bank), 'sYacc'], ['sYacc'])
                S.op('dve', lambda e, ct=ct: e.scalar_tensor_tensor(out=Yacc[:], in0=ut[:], scalar=d8[:, ct:ct + 1], in1=Yacc[:],
                                                                    op0=ALU.mult, op1=ALU.add), ['sut', 'sd8', 'sYacc'], ['sYacc'])
                self.act(yg[:], Yacc[:], AF.Gelu_apprx_tanh, ['sYacc'], ['syg'])
                S.dma('act', self.ycs[ct].rearrange("p (b t) -> p b t", b=NB), yg[:], reads=['syg'], writes=['ycs'], sem='syg')
            S.barrier()
        with ExitStack() as es:
            sb = lambda n, s, d: self.sb(es, n, s, d)
            wg = sb('gwg', [128, 16, 8, 128], BF16)
            yc = [sb('gyc%d' % i, [128, 8, 512], BF16) for i in range(2)]
            sg = [sb('gsg%d' % i, [128, 512], F32) for i in range(2)]
            st = [sb('gst%d' % i, [128, 512], BF16) for i in range(2)]
            for q in range(4):
                S.dma('sp', wg[:, 4 * q:4 * q + 4], self.WF_glu[4 * q:4 * q + 4].rearrange("n p (kt c) -> p n kt c", kt=8),
                      reads=['wscr'], writes=['gwg'], sem='c')
            n = 0
            for tb in range(8):
                i = tb % 2
                S.dma('sp', yc[i][:], self.ycs[0:8, :, tb * 512:(tb + 1) * 512].rearrange("n p t -> p n t"),
                      reads=['ycs'], writes=[('gyc', i)], sem='gyc%d' % i)
                for ft in range(8):
                    for kt in range(8):
                        self.mm(PS[0 + (ft % 2) * 2][:, :], wg[:, ft, kt, :], yc[i][:, kt, :], kt == 0, kt == 7,
                                ['gwg', ('gyc', i)], [('ps', (ft % 2) * 2)])
                    for kt in range(8):
                        self.mm(PS[1 + (ft % 2) * 2][:, :], wg[:, 8 + ft, kt, :], yc[i][:, kt, :], kt == 0, kt == 7,
                                ['gwg', ('gyc', i)], [('ps', 1 + (ft % 2) * 2)])
                    j = n % 2
                    n += 1
                    self.act(sg[j][:], PS[1 + (ft % 2) * 2][:, :], AF.Sigmoid, [('ps', 1 + (ft % 2) * 2)], [('gsg', j)])
                    S.op('dve', lambda e, j=j, ft=ft: e.tensor_tensor(st[j][:], sg[j][:], PS[(ft % 2) * 2][:, :], ALU.mult),
                         [('gsg', j), ('ps', (ft % 2) * 2)], [('gst', j)])
                    S.dma('act', self.yT[16 + ft][:, tb * 512:(tb + 1) * 512], st[j][:], reads=[('gst', j)], writes=['yT'],
                          sem='gst%d' % j)
            S.barrier()

    def phase_merge_ffn(self, l, xin, xout):
        nc, S = self.nc, self.S
        PS = self.PS
        with ExitStack() as es:
            sb = lambda n, s, d: self.sb(es, n, s, d)
            H2 = sb('fH2', [128, 88, 2], F32)
            fc = sb('ffc', [128, 88, 3], F32)
            fb = sb('ffb', [128, 88], F32)
            S.dma('sp', fc[:], self.fconv[l], writes=['ffc'], sem='c')
            S.dma('sp', fb[:], self.fbias[l], writes=['ffb'], sem='c')
            h2T = sb('fh2T', [128, 16, 512], BF16)
            for tb in range(8):
                t0 = tb * 512
                with ExitStack() as e2:
                    sb2 = lambda n, s, d: self.sb(e2, n, s, d)
                    gpo = sb2('fgpo', [128, D], F32)
                    S.dma('sp', gpo[:], self.gpost[l].partition_broadcast(128), writes=['fgpo'], sem='c')
                    yTb = sb2('fyT', [128, 24, 512], BF16)
                    mT = sb2('fmT', [128, 16, 512], BF16)
                    gt = [sb2('fgt%d' % i, [128, 3, 512], BF16) for i in range(2)]
                    wb = [sb2('fwb%d' % i, [128, 3, 8, 128], BF16) for i in range(2)]
                    wo = [sb2('fwo%d' % i, [128, 16, 512], BF16) for i in range(2)]
                    m1 = [sb2('fm1%d' % i, [128, 512], F32) for i in range(2)]
                    m2 = [sb2('fm2%d' % i, [128, 512], F32) for i in range(2)]
                    xt = [sb2('fxt%d' % i, [128, D], F32) for i in range(2)]
                    xo = [sb2('fxo%d' % i, [128, D], F32) for i in range(2)]
                    xn = [sb2('fxn%d' % i, [128, D], BF16) for i in range(2)]
                    ss = [sb2('fss%d' % i, [128, 8], F32) for i in range(2)]
                    for q in range(6):
                        S.dma('sp', yTb[:, 4 * q:4 * q + 4, :], self.yT[4 * q:4 * q + 4, :, t0:t0 + 512].rearrange("n p t -> p n t"),
                              reads=['yT'], writes=[('fyT', q)], sem='fyT')
                    yk = [('fyT', q) for q in range(6)]
                    for ft in range(16):
                        i = ft % 2
                        S.dma('sp', wb[i][:], self.WF_br[ft:48:16].rearrange("n p (kt c) -> p n kt c", kt=8),
                              reads=['wscr'], writes=[('fwb', i)], sem='fwb%d' % i)
                        S.dma('sp', gt[i][:], self.gT[ft:48:16, :, t0:t0 + 512].rearrange("n p t -> p n t"),
                              reads=['gT'], writes=[('fgt', i)], sem='fgt%d' % i)
                        for n in range(3):
                            for kt in range(8):
                                self.mm(PS[n + 3 * i][:, :], wb[i][:, n, kt, :], yTb[:, n * 8 + kt, :], kt == 0, kt == 7,
                                        [('fwb', i)] + yk, [('ps', n + 3 * i)])
                        S.op('dve', lambda e, i=i: e.tensor_tensor(m1[i][:], gt[i][:, 0, :], PS[0 + 3 * i][:, :], ALU.mult),
                             [('fgt', i), ('ps', 3 * i)], [('fm1', i)])
                        S.op('dve', lambda e, i=i: e.tensor_tensor(m2[i][:], gt[i][:, 1, :], PS[1 + 3 * i][:, :], ALU.mult),
                             [('fgt', i), ('ps', 1 + 3 * i)], [('fm2', i)])
                        S.op('pool', lambda e, i=i: e.tensor_tensor(m1[i][:], m1[i][:], m2[i][:], ALU.add),
                             [('fm1', i), ('fm2', i)], [('fm1', i)])
                        S.op('dve', lambda e, i=i: e.tensor_tensor(m2[i][:], gt[i][:, 2, :], PS[2 + 3 * i][:, :], ALU.mult),
                             [('fgt', i), ('ps', 2 + 3 * i)], [('fm2', i)])
                        S.op('pool', lambda e, i=i, ft=ft: e.tensor_tensor(mT[:, ft, :], m1[i][:], m2[i][:], ALU.add),
                             [('fm1', i), ('fm2', i)], ['fmT'])
                    for cb in range(4):
                        S.dma('sp', wo[cb % 2][:, 0:8, :], self.WT_out[cb][:, 0:8 * 512].rearrange("p (kt c) -> p kt c", kt=8),
                              reads=['wscr'], writes=[('fwo', cb % 2, 0)], sem='fwo%d' % (cb % 2))
                        S.dma('sp', wo[cb % 2][:, 8:16, :], self.WT_out[cb][:, 8 * 512:16 * 512].rearrange("p (kt c) -> p kt c", kt=8),
                              reads=['wscr'], writes=[('fwo', cb % 2, 1)], sem='fwo%d' % (cb % 2))
                        wk = [('fwo', cb % 2, 0), ('fwo', cb % 2, 1)]
                        for tt in range(4):
                            bank = (cb * 4 + tt) % 2 + 6
                            for kt in range(16):
                                self.mm(PS[bank][:, :], mT[:, kt, tt * 128:(tt + 1) * 128], wo[cb % 2][:, kt, :], kt == 0, kt == 15,
                                        ['fmT'] + wk, [('ps', bank)])
                            i = (cb * 4 + tt) % 2
                            self.act(m1[i][:], PS[bank][:, :], AF.Copy, [('ps', bank)], [('fm1', i)])
                            S.dma('act', self.x1[t0 + tt * 128:t0 + (tt + 1) * 128, cb * 512:(cb + 1) * 512], m1[i][:],
                                  reads=[('fm1', i)], writes=[('x1', tt)], sem='fm1%d' % i)
                    for tt in range(4):
                        i = tt % 2
                        rows = slice(t0 + tt * 128, t0 + (tt + 1) * 128)
                        S.dma('sp', xo[i][:], self.x1[rows, :], reads=[('x1', tt)], writes=[('fxo', i)], sem='fxo%d' % i)
                        S.dma('sp', xt[i][:], xin[rows, :], reads=['xin'], writes=[('fxt', i)], sem='fxt%d' % i)
                        S.op('pool', lambda e, i=i: e.memset(ss[i][:, 0:1], 0.0), [], [('fss', i)])
                        self.act(xn[i][:], xo[i][:], AF.Square, [('fxo', i)], [('fxn', i), ('fss', i)], accum_out=ss[i][:, 0:1])
                        self.rstd(ss[i][:, 0:1], D, ('fss', i))
                        S.op('dve', lambda e, i=i: e.scalar_tensor_tensor(out=xo[i][:], in0=xo[i][:], scalar=ss[i][:, 0:1], in1=gpo[:],
                                                                          op0=ALU.mult, op1=ALU.mult),
                             [('fxo', i), ('fss', i), 'fgpo'], [('fxo', i)])
                        S.op('pool', lambda e, i=i: e.tensor_tensor(xt[i][:], xt[i][:], xo[i][:], ALU.add),
                             [('fxt', i), ('fxo', i)], [('fxt', i)])
                        S.dma('act', self.x1[rows, :], xt[i][:], reads=[('fxt', i)], writes=[('x1', tt)], sem='fxt%d' % i)
                        self.norm_T(xt[i][:], ('fxt', i), xn[i][:], ('fxn', i), ss[i][:, 1:2], ('fss2', i), h2T, 'fh2T', tt, 0, 1)
                    S.barrier()
                with ExitStack() as e2:
                    sb2 = lambda n, s, d: self.sb(e2, n, s, d)
                    gfo = sb2('fgfo', [128, D], F32)
                    S.dma('sp', gfo[:], self.gfpost[l].partition_broadcast(128), writes=['fgfo'], sem='c')
                    aT = sb2('faT', [128, 44, 512], BF16)
                    wf = [sb2('fwf%d' % i, [128, 16, 128], BF16) for i in range(4)]
                    wd = [sb2('fwd%d' % i, [128, 11, 512], BF16) for i in range(3)]
                    Ub = [sb2('fU%d' % i, [128, 514], F32) for i in range(2)]
                    ca = [sb2('fca%d' % i, [128, 512], F32) for i in range(2)]
                    cv = [sb2('fcv%d' % i, [128, 512], F32) for i in range(2)]
                    xo = [sb2('gxo%d' % i, [128, D], F32) for i in range(4)]
                    xt = [sb2('gxt%d' % i, [128, D], F32) for i in range(1)]
                    jk = sb2('gjk', [128, D], BF16)
                    ss = [sb2('gss%d' % i, [128, 4], F32) for i in range(2)]
                    if tb % 4 == 0:
                        S.op('pool', lambda e: e.memset(H2[:], 0.0), [], ['fH2'])
                    r_wf, r_U = Ring('wf', 4), Ring('U', 2)
                    for jp in range(44):
                        res = []
                        for half in range(2):
                            nt = jp + 44 * half
                            iw = r_wf.next()
                            S.dma('sp', wf[iw][:], self.WF_up[nt].rearrange("p (kt c) -> p kt c", kt=16),
                                  reads=['wscr'], writes=[('fwf', iw)], sem='fwf%d' % iw)
                            bank = (jp % 2) * 2 + half
                            pk = ('ps', bank)
                            for kt in range(16):
                                self.mm(PS[bank][:, :], wf[iw][:, kt, :], h2T[:, kt, :], kt == 0, kt == 15, [('fwf', iw), 'fh2T'], [pk])
                            iu = r_U.next()
                            Ut, uk = Ub[iu], ('fU', iu)
                            self.act(Ut[:, 2:514], PS[bank][:, :], AF.Copy, [pk], [uk])
                            S.op('pool', lambda e, Ut=Ut, nt=nt: e.tensor_copy(Ut[:, 0:2], H2[:, nt, :]), ['fH2'], [uk])
                            S.op('pool', lambda e, Ut=Ut, nt=nt: e.tensor_copy(H2[:, nt, :], Ut[:, 512:514]), [uk], ['fH2'])
                            o = (ca if half == 0 else cv)[jp % 2]
                            ok = ('fc', half, jp % 2)
                            S.op('dve', lambda e, Ut=Ut, o=o, nt=nt: e.tensor_scalar(o[:], Ut[:, 2:514], fc[:, nt, 2:3], fb[:, nt:nt + 1],
                                                                                     ALU.mult, ALU.add), [uk, 'ffc', 'ffb'], [ok])
                            for j in (1, 0):
                                S.op('dve', lambda e, Ut=Ut, o=o, nt=nt, j=j: e.scalar_tensor_tensor(
                                    out=o[:], in0=Ut[:, j:j + 512], scalar=fc[:, nt, j:j + 1], in1=o[:], op0=ALU.mult, op1=ALU.add),
                                    [uk, 'ffc', ok], [ok])
                            res.append((o, ok))
                        (oa, ka), (ov, kv) = res
                        self.act(oa[:], oa[:], AF.Gelu_apprx_tanh, [ka], [ka])
                        S.op('pool', lambda e, oa=oa, ov=ov, jp=jp: e.tensor_tensor(aT[:, jp, :], oa[:], ov[:], ALU.mult), [ka, kv], ['faT'])
                    r_wd = Ring('wd', 3)
                    for cb in range(4):
                        for kp in range(4):
                            iw = r_wd.next()
                            S.dma('sp', wd[iw][:], self.WT_dn[cb * 4 + kp].rearrange("p (kt c) -> p kt c", kt=11),
                                  reads=['wscr'], writes=[('fwd', iw)], sem='fwd%d' % iw)
                            for tt in range(4):
                                for kt in range(11):
                                    self.mm(PS[4 + tt][:, :], aT[:, kp * 11 + kt, tt * 128:(tt + 1) * 128], wd[iw][:, kt, :],
                                            kp == 0 and kt == 0, kp == 3 and kt == 10, ['faT', ('fwd', iw)], [('ps', 4 + tt)],
                                            last=(kt == 10))
                        for tt in range(4):
                            if (cb + tt) % 2:
                                self.act(xo[tt][:, cb * 512:(cb + 1) * 512], PS[4 + tt][:, :], AF.Copy, [('ps', 4 + tt)], [('gxo', tt)])
                            else:
                                S.op('dve', lambda e, tt=tt, cb=cb: e.tensor_copy(xo[tt][:, cb * 512:(cb + 1) * 512], PS[4 + tt][:, :]),
                                     [('ps', 4 + tt)], [('gxo', tt)])
                    for tt in range(4):
                        i = tt
                        rows = slice(t0 + tt * 128, t0 + (tt + 1) * 128)
                        S.dma('sp', xt[0][:], self.x1[rows, :], reads=[('x1', tt)], writes=[('gxt', 0)], sem='gxt0')
                        S.op('pool', lambda e, i=i: e.memset(ss[0][:, 0:1], 0.0), [], [('gss', 0)])
                        self.act(jk[:], xo[i][:], AF.Square, [('gxo', i)], ['gjk', ('gss', 0)], accum_out=ss[0][:, 0:1])
                        self.rstd(ss[0][:, 0:1], D, ('gss', 0))
                        S.op('dve', lambda e, i=i: e.scalar_tensor_tensor(out=xo[i][:], in0=xo[i][:], scalar=ss[0][:, 0:1], in1=gfo[:],
                                                                          op0=ALU.mult, op1=ALU.mult),
                             [('gxo', i), ('gss', 0), 'fgfo'], [('gxo', i)])
                        S.op('pool', lambda e, i=i: e.tensor_tensor(xo[i][:], xo[i][:], xt[0][:], ALU.add),
                             [('gxo', i), ('gxt', 0)], [('gxo', i)])
                        S.dma('act', xout[rows, :], xo[i][:], reads=[('gxo', i)], writes=['xout'], sem='gxo%d' % i)
                    S.barrier()
            S.barrier()


def _bucket_table():
    dist = np.arange(0, 4096, dtype=np.int64)
    n = np.maximum(dist, 0)
    nf = np.maximum(n, 1).astype(np.float32)
    large = 16 + (np.log(nf / np.float32(16)) / np.float32(math.log(8.0)) * np.float32(16)).astype(np.int32)
    large = np.minimum(large, 31)
    return np.where(n < 16, n, large).astype(np.int64)


def host_layout(inp):
    f32 = np.float32
    out = {}
    cst = np.zeros((128, 512), f32)
    cst[:, 0:128] = np.eye(128, dtype=f32)
    cst[:, 128:256] = np.triu(np.ones((128, 128), f32))
    cst[:, 256:384] = np.where(np.arange(128)[:, None] > np.arange(128)[None, :], f32(-30000.0), f32(0.0))
    cst[:, 384:512] = 1.0
    out['cst'] = cst
    g = lambda k: np.asarray(inp[k], f32)
    out['gpre'] = np.ascontiguousarray(g('norm_mix_pre').reshape(2, 16, 128).transpose(0, 2, 1))
    out['gffn'] = np.ascontiguousarray(g('norm_ffn_pre').reshape(2, 16, 128).transpose(0, 2, 1))
    out['gpost'] = g('norm_mix_post').reshape(2, 1, D)
    out['gfpost'] = g('norm_ffn_post').reshape(2, 1, D)
    out['mconv'] = np.ascontiguousarray(g('mlstm_conv').reshape(2, 4, 16, 128).transpose(0, 3, 2, 1))
    out['mnorm'] = g('mlstm_norm').reshape(2, 1, 1024)
    out['dnorm'] = g('diff_norm').reshape(2, 1, 1024)
    out['bif'] = g('mlstm_b_if').reshape(2, 1, 8)
    out['lamT'] = np.ascontiguousarray(g('diff_lambda').transpose(0, 2, 1))
    rb = g('rel_bias')
    bt = _bucket_table()
    kl = np.arange(128)[:, None]
    xx = np.arange(1024)[None, :] - 384
    dist = xx - kl
    wide = np.empty((128, 4, 1024), f32)
    for h in range(4):
        vals = rb[bt[np.clip(dist, 0, 4095)], h]
        wide[:, h, :] = np.where(dist >= 0, vals, f32(-30000.0))
    out['wide'] = wide
    out['cfar'] = np.ascontiguousarray(np.broadcast_to(rb[31][None, :], (128, 4)))
    out['fconv'] = np.ascontiguousarray(g('ffn_conv').reshape(2, 3, 88, 128).transpose(0, 3, 2, 1))
    out['fbias'] = np.ascontiguousarray(g('ffn_conv_b').reshape(2, 88, 128).transpose(0, 2, 1))
    out['s5d'] = np.ascontiguousarray(g('s5_d').reshape(2, 8, 128).transpose(0, 2, 1))
    lre, lim, ldt = g('s5_lambda_re'), g('s5_lambda_im'), g('s5_log_dt')
    ldt_full = np.broadcast_to(ldt[:, :, None], (2, 64, 64))
    s5p = np.empty((2, 3, 128, 32), f32)
    s5r = np.empty((2, 3, 128, 32 * 128), f32)
    for i, a in enumerate((lre, lim, ldt_full)):
        a4 = a.reshape(2, 32, 2, 64)
        s5p[:, i] = a4.transpose(0, 2, 3, 1).reshape(2, 128, 32)
        s5r[:, i] = np.broadcast_to(a4.reshape(2, 1, 32 * 128), (2, 128, 32 * 128))
    out['s5p'], out['s5r'] = s5p, s5r
    bT = np.zeros((2, 2, 128, 32, 128), f32)
    cP = np.zeros((2, 2, 128, 32, 128), f32)
    for i, (bk, ck) in enumerate((('s5_b_re', 's5_c_re'), ('s5_b_im', 's5_c_im'))):
        bb = g(bk)
        cc = g(ck)
        for gi in range(64):
            gp_, gg = gi // 2, gi % 2
            r0 = (gi % 8) * 16
            bT[:, i, r0:r0 + 16, gp_, gg * 64:(gg + 1) * 64] = bb[:, gi].transpose(0, 2, 1)
            cP[:, i, gg * 64:(gg + 1) * 64, gp_, r0:r0 + 16] = cc[:, gi].transpose(0, 2, 1)
    out['s5bT'] = bT.reshape(2, 2, 128, 32 * 128)
    out['s5cP'] = cP.reshape(2, 2, 128, 32 * 128)
    for k in ('w_in', 'w_branch', 's5_w_glu', 'w_out', 'w_up', 'w_down'):
        out[k] = np.ascontiguousarray(g(k))
    return out


_PROG = {}
_PER_LAYER = ('gpre', 'gffn', 'gpost', 'gfpost', 'mconv', 'mnorm', 'dnorm', 'bif', 'lamT', 'fconv', 'fbias', 's5d',
              's5p', 's5r', 's5bT', 's5cP', 'w_in', 'w_branch', 's5_w_glu', 'w_out', 'w_up', 'w_down')


def _prog(kind):
    if kind not in _PROG:
        _PROG[kind] = Prog(kind)
    return _PROG[kind]


def _launch(kind, per_core, shared):
    prog = _prog(kind)
    in_maps = []
    for c in range(NCORE):
        m = {}
        for k in prog.inp:
            m[k] = per_core[c][k] if k in per_core[c] else shared[k]
        in_maps.append(m)
    res = run_bass_kernel_spmd(prog.nc, in_maps, core_ids=list(range(NCORE)))
    return [{k: np.asarray(res.results[c][k]) for k in prog.outs} for c in range(NCORE)]


def kernel(**inputs):
    lay = host_layout(inputs)
    lami = np.empty((2, 128, 2), np.float32)
    for l in range(2):
        li = 0.8 - 0.6 * math.exp(-0.3 * l)
        lami[l, :, 0] = 1.0 - li
        lami[l, :, 1] = -li
    lay['lami'] = lami
    x = np.asarray(inputs['x'], np.float32)
    prog = _prog('F')
    in_maps = []
    for c in range(NCORE):
        m = {k: lay[k] for k in prog.inp if k != 'x'}
        m['x'] = np.ascontiguousarray(x[NB * c:NB * (c + 1)].reshape(T, D))
        in_maps.append(m)
    res = run_bass_kernel_spmd(prog.nc, in_maps, core_ids=list(range(NCORE)))
    out = np.empty((16, L, D), np.float32)
    for c in range(NCORE):
        out[NB * c:NB * (c + 1)] = np.asarray(res.results[c]['y'], np.float32).reshape(NB, L, D)
    return out
```
